# Optimizing a Trainium2 kernel written in Bass

```python
import jax, jax.numpy as jnp
from jax import lax
import numpy as np


D_MODEL = 1024
BATCH = 8
SEQ = 2048
DEPTH = 2

GRID_W = 64
ROPE_THETA = 10000.0
EPS = 1e-6
Q_BLOCK = 128
A_HEADS = 8
A_KV_HEADS = 2
A_HEAD_DIM = 64
B_HEADS = 8
B_NOPE_DIM = 64
B_ROPE_DIM = 32
B_V_DIM = 64
B_Q_RANK = 256
B_KV_RANK = 256
C_HEADS = 4
C_DK = 128
C_DV = 128
C_GATE_RANK = 16
C_TAU = 16.0
C_CHUNK = 64
N_BRANCH = 3
D_FF = 2816
N_EXPERTS = 8
TOP_K = 2
D_FF_EXPERT = 3584
N_DENSE = (DEPTH + 1) // 2
N_MOE = DEPTH // 2
IN_SIZES = (A_HEADS * A_HEAD_DIM, A_KV_HEADS * A_HEAD_DIM, A_KV_HEADS * A_HEAD_DIM,
            B_Q_RANK, B_KV_RANK, B_ROPE_DIM,
            C_HEADS * C_DK, C_HEADS * C_DK, C_HEADS * C_DV, C_HEADS * C_DV, C_GATE_RANK, C_GATE_RANK,
            N_BRANCH * D_MODEL)
IN_WIDTH = sum(IN_SIZES)

kernel_name = 'hybrid_gated_gqa_mla_gla_moe_encoder'


def rmsnorm(x, g):
    xf = x.astype(jnp.float32)
    y = xf * lax.rsqrt(jnp.mean(xf * xf, axis=-1, keepdims=True) + EPS)
    return (y * g.astype(jnp.float32)).astype(x.dtype)


def axial_angles(T, rot_dim):
    rows = T // GRID_W
    row = jnp.repeat(jnp.arange(rows, dtype=jnp.float32), GRID_W)
    col = jnp.tile(jnp.arange(GRID_W, dtype=jnp.float32), rows)
    n_freq = rot_dim // 4
    inv_freq = ROPE_THETA ** (-jnp.arange(n_freq, dtype=jnp.float32) / n_freq)
    return row[:, None] * inv_freq, col[:, None] * inv_freq


def rope_1d(x, ang):
    x1, x2 = jnp.split(x, 2, axis=-1)
    c, s = jnp.cos(ang), jnp.sin(ang)
    return jnp.concatenate([x1 * c - x2 * s, x2 * c + x1 * s], axis=-1)


def axial_rope(x, ang_row, ang_col):
    xf = x.astype(jnp.float32)
    half = x.shape[-1] // 2
    y = jnp.concatenate([rope_1d(xf[..., :half], ang_row), rope_1d(xf[..., half:], ang_col)], axis=-1)
    return y.astype(x.dtype)


def heads(t, n):
    Bsz, T, w = t.shape
    return t.reshape(Bsz, T, n, w // n).transpose(0, 2, 1, 3)


def merge_heads(t):
    Bsz, n, T, d = t.shape
    return t.transpose(0, 2, 1, 3).reshape(Bsz, T, n * d)


def blocked_attention(q, k, v, scale):
    Bsz, Hk, G, T, dq = q.shape
    n_blk = T // Q_BLOCK
    qb = jnp.moveaxis(q.reshape(Bsz, Hk, G, n_blk, Q_BLOCK, dq), 3, 0)

    def one_block(q_blk):
        s = jnp.einsum('bhgqd,bhkd->bhgqk', q_blk, k).astype(jnp.float32) * scale
        p = jax.nn.softmax(s, axis=-1).astype(v.dtype)
        return jnp.einsum('bhgqk,bhkv->bhgqv', p, v)

    out = lax.map(one_block, qb)
    return jnp.moveaxis(out, 0, 3).reshape(Bsz, Hk, G, T, v.shape[-1])


def gla_chunked(q, k, v, log_a):
    Bsz, H, T, dk = q.shape
    dv = v.shape[-1]
    n = T // C_CHUNK
    shp = (Bsz, H, n, C_CHUNK)
    q = q.reshape(shp + (dk,))
    k = k.reshape(shp + (dk,))
    v = v.reshape(shp + (dv,))
    b = jnp.cumsum(log_a.reshape(shp + (dk,)), axis=3)
    q_t = q * jnp.exp(b)
    k_t = k * jnp.exp(-b)
    causal = jnp.tril(jnp.ones((C_CHUNK, C_CHUNK), dtype=bool))
    scores = jnp.where(causal, jnp.einsum('bhncd,bhnsd->bhncs', q_t, k_t), 0.0)
    intra = jnp.einsum('bhncs,bhnsv->bhncv', scores, v)
    b_last = b[:, :, :, -1:, :]
    chunk_kv = jnp.einsum('bhncd,bhncv->bhndv', k * jnp.exp(b_last - b), v)
    chunk_decay = jnp.exp(b_last[:, :, :, 0, :])

    def step(state, inp):
        dec, kv = inp
        return dec[..., None] * state + kv, state

    init = jnp.zeros((Bsz, H, dk, dv), q.dtype)
    _, prev = lax.scan(step, init, (jnp.moveaxis(chunk_decay, 2, 0), jnp.moveaxis(chunk_kv, 2, 0)))
    prev = jnp.moveaxis(prev, 0, 2)
    inter = jnp.einsum('bhncd,bhndv->bhncv', q_t, prev)
    return (intra + inter).reshape(Bsz, H, T, dv)


def bidir_gla(q, k, v, log_a_fwd, log_a_bwd):
    flip = lambda t: jnp.flip(t, axis=2)
    fwd = gla_chunked(q, k, v, log_a_fwd)
    bwd = flip(gla_chunked(flip(q), flip(k), flip(v), flip(log_a_bwd)))
    return fwd + bwd


def token_mixer(h, w_in, b_gate, g_a_q, g_a_k, g_b_q, w_b_q_up, g_b_kv, w_b_kv_up,
                w_c_af_up, b_c_af, w_c_ab_up, b_c_ab, g_c_out, w_pa, w_pb, w_pc, w_out, ang_a, ang_b):
    Bsz, T, _ = h.shape
    f32 = jnp.float32
    proj = h @ w_in
    offs = []
    acc = 0
    for s in IN_SIZES[:-1]:
        acc += s
        offs.append(acc)
    (a_q, a_k, a_v, b_cq, b_ckv, b_kr, c_q, c_k, c_v, c_g, c_af, c_ab, gates) = jnp.split(proj, offs, axis=-1)

    qa = axial_rope(rmsnorm(heads(a_q, A_HEADS), g_a_q), *ang_a)
    ka = axial_rope(rmsnorm(heads(a_k, A_KV_HEADS), g_a_k), *ang_a)
    va = heads(a_v, A_KV_HEADS)
    qa = qa.reshape(Bsz, A_KV_HEADS, A_HEADS // A_KV_HEADS, T, A_HEAD_DIM)
    ya = blocked_attention(qa, ka, va, A_HEAD_DIM ** -0.5).reshape(Bsz, A_HEADS, T, A_HEAD_DIM)
    ya = merge_heads(ya)

    qb = heads(rmsnorm(b_cq, g_b_q) @ w_b_q_up, B_HEADS)
    q_nope, q_rope = qb[..., :B_NOPE_DIM], qb[..., B_NOPE_DIM:]
    kvb = heads(rmsnorm(b_ckv, g_b_kv) @ w_b_kv_up, B_HEADS)
    k_nope, vb = kvb[..., :B_NOPE_DIM], kvb[..., B_NOPE_DIM:]
    k_rope = axial_rope(b_kr[:, None], *ang_b)
    qb = jnp.concatenate([q_nope, axial_rope(q_rope, *ang_b)], axis=-1)
    kb = jnp.concatenate([k_nope, jnp.broadcast_to(k_rope, k_nope.shape[:-1] + (B_ROPE_DIM,))], axis=-1)
    yb = blocked_attention(qb[:, :, None], kb, vb, (B_NOPE_DIM + B_ROPE_DIM) ** -0.5)[:, :, 0]
    yb = merge_heads(yb)

    qc = heads(c_q, C_HEADS).astype(f32) * (C_DK ** -0.5)
    kc = heads(c_k, C_HEADS).astype(f32)
    vc = heads(c_v, C_HEADS).astype(f32)
    la_f = heads(jax.nn.log_sigmoid((c_af @ w_c_af_up + b_c_af).astype(f32)) / C_TAU, C_HEADS)
    la_b = heads(jax.nn.log_sigmoid((c_ab @ w_c_ab_up + b_c_ab).astype(f32)) / C_TAU, C_HEADS)
    oc = bidir_gla(qc, kc, vc, la_f, la_b)
    yc = merge_heads(rmsnorm(oc, g_c_out)).astype(h.dtype) * jax.nn.silu(c_g)

    gb = jax.nn.sigmoid(gates + b_gate).reshape(Bsz, T, N_BRANCH, D_MODEL)
    m = gb[:, :, 0] * (ya @ w_pa) + gb[:, :, 1] * (yb @ w_pb) + gb[:, :, 2] * (yc @ w_pc)
    return m @ w_out


def swiglu(h, wg, wu, wd):
    return (jax.nn.silu(h @ wg) * (h @ wu)) @ wd


def moe_ffn(h, w_router, w_e_gate, w_e_up, w_e_down):
    logits = jnp.einsum('btd,de->bte', h.astype(jnp.float32), w_router.astype(jnp.float32))
    top_v, top_i = lax.top_k(logits, TOP_K)
    top_w = jax.nn.softmax(top_v, axis=-1)
    gate = jnp.einsum('btk,btke->bte', top_w, jax.nn.one_hot(top_i, N_EXPERTS, dtype=jnp.float32)).astype(h.dtype)
    out = jnp.zeros_like(h)
    for e in range(N_EXPERTS):
        out = out + gate[..., e:e + 1] * swiglu(h, w_e_gate[e], w_e_up[e], w_e_down[e])
    return out


def setup_inputs(seed: int = 0) -> dict:
    key = jax.random.key(seed)
    keys = jax.random.split(key, 28)

    def nrm(i, shape, scale):
        return scale * jax.random.normal(keys[i], shape, dtype=jnp.float32)

    def gain(i, shape):
        return 1.0 + nrm(i, shape, 0.01)

    return {
        'x': nrm(0, (BATCH, SEQ, D_MODEL), 1.0),
        'w_in': nrm(1, (DEPTH, D_MODEL, IN_WIDTH), D_MODEL ** -0.5),
        'b_gate': nrm(2, (DEPTH, N_BRANCH * D_MODEL), 0.02),
        'g_mix': gain(3, (DEPTH, D_MODEL)),
        'g_a_q': gain(4, (DEPTH, A_HEAD_DIM)),
        'g_a_k': gain(5, (DEPTH, A_HEAD_DIM)),
        'g_b_q': gain(6, (DEPTH, B_Q_RANK)),
        'w_b_q_up': nrm(7, (DEPTH, B_Q_RANK, B_HEADS * (B_NOPE_DIM + B_ROPE_DIM)), B_Q_RANK ** -0.5),
        'g_b_kv': gain(8, (DEPTH, B_KV_RANK)),
        'w_b_kv_up': nrm(9, (DEPTH, B_KV_RANK, B_HEADS * (B_NOPE_DIM + B_V_DIM)), B_KV_RANK ** -0.5),
        'w_c_af_up': nrm(10, (DEPTH, C_GATE_RANK, C_HEADS * C_DK), C_GATE_RANK ** -0.5),
        'b_c_af': nrm(11, (DEPTH, C_HEADS * C_DK), 0.1),
        'w_c_ab_up': nrm(12, (DEPTH, C_GATE_RANK, C_HEADS * C_DK), C_GATE_RANK ** -0.5),
        'b_c_ab': nrm(13, (DEPTH, C_HEADS * C_DK), 0.1),
        'g_c_out': gain(14, (DEPTH, C_DV)),
        'w_pa': nrm(15, (DEPTH, A_HEADS * A_HEAD_DIM, D_MODEL), (A_HEADS * A_HEAD_DIM) ** -0.5),
        'w_pb': nrm(16, (DEPTH, B_HEADS * B_V_DIM, D_MODEL), (B_HEADS * B_V_DIM) ** -0.5),
        'w_pc': nrm(17, (DEPTH, C_HEADS * C_DV, D_MODEL), (C_HEADS * C_DV) ** -0.5),
        'w_out': nrm(18, (DEPTH, D_MODEL, D_MODEL), D_MODEL ** -0.5),
        'g_ffn': gain(19, (DEPTH, D_MODEL)),
        'w_ff_gate': nrm(20, (N_DENSE, D_MODEL, D_FF), D_MODEL ** -0.5),
        'w_ff_up': nrm(21, (N_DENSE, D_MODEL, D_FF), D_MODEL ** -0.5),
        'w_ff_down': nrm(22, (N_DENSE, D_FF, D_MODEL), D_FF ** -0.5),
        'w_router': nrm(23, (N_MOE, D_MODEL, N_EXPERTS), D_MODEL ** -0.5),
        'w_e_gate': nrm(24, (N_MOE, N_EXPERTS, D_MODEL, D_FF_EXPERT), D_MODEL ** -0.5),
        'w_e_up': nrm(25, (N_MOE, N_EXPERTS, D_MODEL, D_FF_EXPERT), D_MODEL ** -0.5),
        'w_e_down': nrm(26, (N_MOE, N_EXPERTS, D_FF_EXPERT, D_MODEL), D_FF_EXPERT ** -0.5),
        'g_final': gain(27, (D_MODEL,)),
    }


def reference(x, w_in, b_gate, g_mix, g_a_q, g_a_k, g_b_q, w_b_q_up, g_b_kv, w_b_kv_up,
              w_c_af_up, b_c_af, w_c_ab_up, b_c_ab, g_c_out, w_pa, w_pb, w_pc, w_out, g_ffn,
              w_ff_gate, w_ff_up, w_ff_down, w_router, w_e_gate, w_e_up, w_e_down, g_final):
    T = x.shape[1]
    ang_a = axial_angles(T, A_HEAD_DIM)
    ang_b = axial_angles(T, B_ROPE_DIM)
    for i in range(DEPTH):
        h = rmsnorm(x, g_mix[i])
        x = x + token_mixer(h, w_in[i], b_gate[i], g_a_q[i], g_a_k[i], g_b_q[i], w_b_q_up[i],
                            g_b_kv[i], w_b_kv_up[i], w_c_af_up[i], b_c_af[i], w_c_ab_up[i], b_c_ab[i],
                            g_c_out[i], w_pa[i], w_pb[i], w_pc[i], w_out[i], ang_a, ang_b)
        h = rmsnorm(x, g_ffn[i])
        j = i // 2
        if i % 2 == 0:
            x = x + swiglu(h, w_ff_gate[j], w_ff_up[j], w_ff_down[j])
        else:
            x = x + moe_ffn(h, w_router[j], w_e_gate[j], w_e_up[j], w_e_down[j])
    return rmsnorm(x, g_final)
```

```python
import numpy as np
import concourse.bass as bass
import concourse.mybir as mybir
from concourse.bass_utils import run_bass_kernel_spmd
from contextlib import ExitStack

F32 = mybir.dt.float32
BF16 = mybir.dt.bfloat16
AF = mybir.ActivationFunctionType
ALU = mybir.AluOpType
AX = mybir.AxisListType

T = 2048
D = 1024
NT = 16
NB = 4
DEPTH = 2
DFF = 2816
NEXP = 8
DFFE = 3584
EPS = 1e-6
ENGS = ("sp", "act", "dve", "pool", "pe")
SEM_EPOCH = 30000
SKIP = ''
STRICT = True
HLS = (0, 1, 2, 3)
DEBUG_OUT = {}


class Sched:
    def __init__(self, nc, es):
        self.nc = nc
        self.es = es
        self.ops = []
        self.last_writer = {}
        self.readers = {}
        self.dma_sems = {}
        self.eng_ops = {e: [] for e in ENGS}
        self.bar_start = 0

    def op(self, eng, fn, reads=(), writes=(), dma=None, dma_batch=False, extra=()):
        deps = set(extra)
        raw = set(extra)
        for k in reads:
            w = self.last_writer.get(k)
            if w is not None:
                deps.add(w)
                raw.add(w)
            if isinstance(k, tuple) and k and k[0] in ("ps", "plg"):
                for r in self.readers.get(k, ()):
                    if self.ops[r]["eng"] != eng:
                        deps.add(r)
                        raw.add(r)
        for k in writes:
            w = self.last_writer.get(k)
            if w is not None:
                deps.add(w)
            for r in self.readers.get(k, ()):
                deps.add(r)
        idx = len(self.ops)
        o = dict(eng=eng, fn=fn, deps=deps, dma=dma, needed=False, idx=idx, raw=raw)
        if dma is not None:
            d = self.dma_sems.setdefault(dma, dict(total=0, batch=dma_batch))
            d["total"] += 16
            o["dma_val"] = d["total"]
        self.ops.append(o)
        self.eng_ops[eng].append(idx)
        for k in reads:
            self.readers.setdefault(k, []).append(idx)
        for k in writes:
            self.last_writer[k] = idx
            self.readers[k] = []
        return idx

    def barrier(self):
        last = [self.eng_ops[e][-1] for e in ENGS if self.eng_ops[e] and not self.ops[self.eng_ops[e][-1]].get("bar")]
        dmas = [i for i in range(self.bar_start, len(self.ops)) if self.ops[i]["dma"] is not None]
        ex = set(last + dmas)
        for e in ENGS:
            i_ = self.op(e, lambda eng: eng.nop(), extra=ex)
            self.ops[i_]["bar"] = True
        self.bar_start = len(self.ops)
        self.last_writer = {}
        self.readers = {}

    def emit(self, final_waits=()):
        nc = self.nc
        ops = self.ops
        for o in ops:
            nd = set()
            for d in o["deps"]:
                p = ops[d]
                if p["dma"] is None and p["eng"] == o["eng"] and o["dma"] is None:
                    if o["eng"] == "pe":
                        continue
                    if d not in o["raw"] and not STRICT:
                        continue
                nd.add(d)
            o["deps"] = nd
            for d in nd:
                ops[d]["needed"] = True
        cnt = {e: 0 for e in ENGS}
        for o in ops:
            if o["dma"] is None and o["needed"]:
                cnt[o["eng"]] += 1
                o["sig"] = cnt[o["eng"]]
        self.cnt = cnt
        eng_sems = {}
        for e in ENGS:
            n_ep = max(cnt[e] - 1, 0) // SEM_EPOCH + 1
            eng_sems[e] = [self.es.enter_context(nc.semaphore(f"s_{e}{i}")) for i in range(n_ep)]
        dsem = {}
        for name in self.dma_sems:
            dsem[name] = self.es.enter_context(nc.semaphore(f"d_{name}"))

        def event(o):
            if o["dma"] is not None:
                d = self.dma_sems[o["dma"]]
                v = d["total"] if d["batch"] else o["dma_val"]
                return (("d", o["dma"]), dsem[o["dma"]], v)
            s = o["sig"]
            ep = (s - 1) // SEM_EPOCH
            return (("e", o["eng"], ep), eng_sems[o["eng"]][ep], s - ep * SEM_EPOCH)

        block = self.es.enter_context(nc.Block())

        def run_engine(ename):
            def body(eng):
                wm = {}
                for idx in self.eng_ops[ename]:
                    o = ops[idx]
                    need = {}
                    for d in o["deps"]:
                        key, sem, val = event(ops[d])
                        if val > need.get(key, (None, 0))[1]:
                            need[key] = (sem, val)
                    for key, (sem, val) in need.items():
                        if key[0] == "e":
                            if any(k[0] == "e" and k[1] == key[1] and k[2] > key[2] for k in wm):
                                continue
                        if wm.get(key, 0) >= val:
                            continue
                        eng.wait_ge(sem, val)
                        wm[key] = val
                    ins = o["fn"](eng)
                    if o["dma"] is not None:
                        ins.then_inc(dsem[o["dma"]], 16)
                    elif o["needed"]:
                        key, sem, val = event(o)
                        ins.then_inc(sem, 1)
                for (e2, name) in final_waits:
                    if e2 == ename and name in dsem:
                        eng.wait_ge(dsem[name], self.dma_sems[name]["total"])
            return body

        block.sync(run_engine("sp"))
        block.scalar(run_engine("act"))
        block.vector(run_engine("dve"))
        block.gpsimd(run_engine("pool"))
        block.tensor(run_engine("pe"))


IN_SIZES = (512, 128, 128, 256, 256, 32, 512, 512, 512, 512, 16, 16, 3072)
OFF = np.concatenate([[0], np.cumsum(IN_SIZES)]).astype(int)
(O_AQ, O_AK, O_AV, O_BCQ, O_BCKV, O_BKR, O_CQ, O_CK, O_CV, O_CG, O_CAF, O_CAB, O_GATE) = OFF[:13]


def lhsT(W, cols):
    W = np.asarray(W)
    kc = W.shape[0] // 128
    sub = W[:, cols] if cols is not None else W
    return np.ascontiguousarray(sub.reshape(kc, 128, sub.shape[1]).transpose(1, 0, 2))


def partner(n):
    h = n // 2
    return np.concatenate([np.arange(h, n), np.arange(0, h)])


def rope_tables():
    GRID_W = 64
    rows = T // GRID_W
    row = np.repeat(np.arange(rows, dtype=np.float32), GRID_W)
    col = np.tile(np.arange(GRID_W, dtype=np.float32), rows)

    def tabs(rot_dim):
        nf = rot_dim // 4
        inv = (10000.0 ** (-np.arange(nf, dtype=np.float32) / nf)).astype(np.float32)
        ar = row[:, None] * inv
        ac = col[:, None] * inv
        half = rot_dim // 2
        cos = np.concatenate([np.cos(ar), np.cos(ar), np.cos(ac), np.cos(ac)], axis=1)
        sin = np.concatenate([-np.sin(ar), np.sin(ar), -np.sin(ac), np.sin(ac)], axis=1)
        return cos.T.astype(np.float32), sin.T.astype(np.float32)
    ca, sa = tabs(64)
    cb, sb = tabs(32)
    cosA = np.concatenate([ca, ca], 0)
    sinA = np.concatenate([sa, sa], 0)
    cosB = np.concatenate([np.ones((64, T), np.float32), cb, np.zeros((32, T), np.float32)], 0)
    sinB = np.concatenate([np.zeros((64, T), np.float32), sb, np.zeros((32, T), np.float32)], 0)
    return (np.ascontiguousarray(cosA), np.ascontiguousarray(sinA),
            np.ascontiguousarray(cosB), np.ascontiguousarray(sinB))


def rope_partner_cols(n_rot):
    h = n_rot // 2
    p = partner(h)
    return np.concatenate([p, h + p])


def consts_pack():
    c = {}
    c["ident"] = np.eye(128, dtype=np.float32)
    blk = np.zeros((128, 128), np.float32)
    blk[:64, :64] = 1.0 / 64
    blk[64:, 64:] = 1.0 / 64
    c["blk64"] = blk
    pa = rope_partner_cols(64)
    full = np.concatenate([pa, 64 + pa])
    P = np.zeros((128, 128), np.float32)
    P[full, np.arange(128)] = 1.0
    c["permA"] = P
    s = np.arange(128)
    same = (s[:, None] // 64) == (s[None, :] // 64)
    c["trif"] = (same & (s[:, None] <= s[None, :])).astype(np.float32)
    c["trib"] = (same & (s[:, None] >= s[None, :])).astype(np.float32)
    c["uf"] = (same & (s[:, None] > s[None, :])).astype(np.float32)
    c["ub"] = (same & (s[:, None] < s[None, :])).astype(np.float32)
    names = ["ident", "blk64", "permA", "trif", "trib", "uf", "ub"]
    return np.ascontiguousarray(np.stack([c[n] for n in names], 1)), names


CONST_NAMES = ["ident", "blk64", "permA", "trif", "trib", "uf", "ub"]
V_GMIX, V_GFFN, V_GAQ, V_GAK, V_GBQ, V_GBKV, V_GCO, V_BG = 0, 8, 16, 17, 18, 20, 22, 23
NVEC = 23 + 24


def pack_inputs(inp):
    out = {}
    cst, _ = consts_pack()
    out["cst"] = cst
    cosA, sinA, cosB, sinB = rope_tables()
    out["ropeA"] = np.ascontiguousarray(np.stack([cosA, sinA], 1))
    out["ropeB"] = np.ascontiguousarray(np.stack([cosB, sinB], 1))
    pk96 = rope_partner_cols(32)
    for l in range(DEPTH):
        w_in = np.asarray(inp["w_in"][l])
        vec = np.zeros((128, NVEC), np.float32)
        vec[:, V_GMIX:V_GMIX + 8] = np.asarray(inp["g_mix"][l]).reshape(8, 128).T
        vec[:, V_GFFN:V_GFFN + 8] = np.asarray(inp["g_ffn"][l]).reshape(8, 128).T
        vec[:, V_GAQ] = np.tile(np.asarray(inp["g_a_q"][l]), 2)
        vec[:, V_GAK] = np.tile(np.asarray(inp["g_a_k"][l]), 2)
        vec[:, V_GBQ:V_GBQ + 2] = np.asarray(inp["g_b_q"][l]).reshape(2, 128).T
        vec[:, V_GBKV:V_GBKV + 2] = np.asarray(inp["g_b_kv"][l]).reshape(2, 128).T
        vec[:, V_GCO] = np.asarray(inp["g_c_out"][l])
        vec[:, V_BG:V_BG + 24] = np.asarray(inp["b_gate"][l]).reshape(24, 128).T
        out[f"vec{l}"] = vec
        wa = []
        for g in range(2):
            kg = lhsT(w_in, O_AK + np.arange(g * 64, g * 64 + 64))
            zz = np.zeros_like(kg)
            wa.append(np.concatenate([lhsT(w_in, O_AQ + np.arange(2 * g * 128, (2 * g + 2) * 128)), kg, zz, zz, kg,
                                      lhsT(w_in, O_AV + np.arange(g * 64, g * 64 + 64))], axis=2))
        out[f"wA{l}"] = np.ascontiguousarray(np.stack(wa, 0))
        z64 = np.zeros((128, 8, 64), np.float32)
        kr = lhsT(w_in, O_BKR + np.arange(32))
        krp = lhsT(w_in, O_BKR + pk96)
        out[f"wB1{l}"] = np.ascontiguousarray(np.concatenate(
            [lhsT(w_in, O_BCQ + np.arange(256)), lhsT(w_in, O_BCKV + np.arange(256)), z64, kr, z64, krp], axis=2))
        wq = np.asarray(inp["w_b_q_up"][l])
        qcols, qpcols = [], []
        for h in range(8):
            base = h * 96
            qcols.append(base + np.arange(96))
            qpcols.append(np.concatenate([base + np.arange(64), base + 64 + pk96]))
        wq_l = np.stack([np.stack([lhsT(wq, qcols[h]), lhsT(wq, qpcols[h])], 2) for h in range(8)], 2)
        wkv = np.asarray(inp["w_b_kv_up"][l])
        wk_l = np.stack([lhsT(wkv, h * 128 + np.arange(64)) for h in range(8)], 2)
        wv_l = np.stack([lhsT(wkv, h * 128 + 64 + np.arange(64)) for h in range(8)], 2)
        out[f"wB2{l}"] = np.ascontiguousarray(np.concatenate(
            [wq_l.reshape(128, -1), wk_l.reshape(128, -1), wv_l.reshape(128, -1)], axis=1))
        wc = []
        for h in range(4):
            sl = np.arange(h * 128, (h + 1) * 128)
            a = np.stack([lhsT(w_in, O_CQ + sl), lhsT(w_in, O_CK + sl), lhsT(w_in, O_CG + sl)], 2)
            b = np.concatenate([lhsT(w_in, O_CK + sl), lhsT(w_in, O_CV + sl)], 2)
            wc.append(np.concatenate([a.reshape(128, -1), b.reshape(128, -1)], 1))
        out[f"wC{l}"] = np.ascontiguousarray(np.stack(wc, 0))
        z16 = np.zeros((128, 8, 16), np.float32)
        out[f"wCg{l}"] = np.ascontiguousarray(np.concatenate(
            [lhsT(w_in, O_CAF + np.arange(16)), z16, lhsT(w_in, O_CAB + np.arange(16)), z16], 2))
        up = np.zeros((128, 4, 2, 128), np.float32)
        waf = np.asarray(inp["w_c_af_up"][l]); wab = np.asarray(inp["w_c_ab_up"][l])
        baf = np.asarray(inp["b_c_af"][l]); bab = np.asarray(inp["b_c_ab"][l])
        for h in range(4):
            sl = slice(h * 128, (h + 1) * 128)
            up[0:16, h, 0] = waf[:, sl]
            up[48, h, 0] = baf[sl]
            up[32:48, h, 1] = wab[:, sl]
            up[48, h, 1] = bab[sl]
        out[f"wCup{l}"] = np.ascontiguousarray(up.reshape(128, -1))
        wps = [np.asarray(inp[n][l]) for n in ("w_pa", "w_pb", "w_pc")]
        wm = []
        for j in range(8):
            cj = np.arange(j * 128, (j + 1) * 128)
            for br in range(3):
                a = lhsT(wps[br], cj).reshape(128, -1)
                gts = lhsT(w_in, O_GATE + br * 1024 + cj).reshape(128, -1)
                wm.append(np.concatenate([a, gts], 1))
        out[f"wM{l}"] = np.ascontiguousarray(np.stack(wm, 0))
        wo = np.asarray(inp["w_out"][l])
        out[f"wO{l}"] = np.ascontiguousarray(np.stack([lhsT(wo, np.arange(j * 128, (j + 1) * 128)).reshape(128, -1) for j in range(8)], 0))

    def ffn_pack(wg, wu, wd):
        dff = wg.shape[1]
        ng = dff // 256
        blocks = []
        for g in range(ng):
            parts = []
            for w in (wg, wu):
                parts.append(np.stack([lhsT(w, np.arange((2 * g + c) * 128, (2 * g + c + 1) * 128)) for c in range(2)], 1).reshape(128, -1))
            dd = np.stack([np.asarray(wd)[(2 * g + c) * 128:(2 * g + c + 1) * 128, :] for c in range(2)], 1).reshape(128, -1)
            parts.append(dd)
            blocks.append(np.concatenate(parts, 1))
        return np.ascontiguousarray(np.stack(blocks, 0))
    out["wF0"] = ffn_pack(np.asarray(inp["w_ff_gate"][0]), np.asarray(inp["w_ff_up"][0]), np.asarray(inp["w_ff_down"][0]))
    we = [ffn_pack(np.asarray(inp["w_e_gate"][0][e]), np.asarray(inp["w_e_up"][0][e]), np.asarray(inp["w_e_down"][0][e])) for e in range(NEXP)]
    out["wE"] = np.ascontiguousarray(np.concatenate(we, 0))
    out["wR"] = lhsT(np.asarray(inp["w_router"][0]), None)
    gf = np.asarray(inp["g_final"]).reshape(8, 128).T
    out["gfin"] = np.ascontiguousarray(gf)
    return out


class Ctx:
    pass


def build_program(shapes, stop_after=None, dbg=None):
    nc = bass.Bass("TRN2", target_bir_lowering=False)
    dr = {}
    for name, shp in shapes.items():
        dr[name] = nc.dram_tensor(name, list(shp), F32, kind="ExternalInput").ap()
    y_out = nc.dram_tensor("y", [T, D], F32, kind="ExternalOutput").ap()
    dbg_out = {}
    if dbg:
        for name, shp in dbg.items():
            dbg_out[name] = nc.dram_tensor("dbg_" + name, list(shp), F32, kind="ExternalOutput").ap()
    es = ExitStack()
    with es:
        S = Sched(nc, es)
        uid = [0]

        def sbt(stack, name, shape, dt):
            uid[0] += 1
            return stack.enter_context(nc.sbuf_tensor(f"{name}_{uid[0]}", list(shape), dt))

        P = lambda name, shape, dt=F32: sbt(es, name, shape, dt)
        ps = [es.enter_context(nc.psum_tensor(f"ps{i}", [128, 512], F32)) for i in range(8)]
        rr = {}

        def psb(pool, banks):
            i = rr.get(pool, 0)
            rr[pool] = i + 1
            b = banks[i % len(banks)]
            return ps[b], ("ps", b)

        ALLB = [0, 1, 2, 3, 4, 5, 6, 7]
        PJ = [2, 3, 4, 5, 6, 7]

        xT = P("xT", [128, 8, T])
        hT = P("hT", [128, 8, T], BF16)
        cst = P("cst", [128, 7, 128])
        cstb = P("cstb", [128, 7, 128], BF16)
        vec = [P(f"vec{l}", [128, NVEC]) for l in range(DEPTH)]
        gfin = P("gfin", [128, 8])
        epsb = P("epsb", [128, 1])
        lnsc = P("lnsc", [128, 1])
        onesm = P("onesm", [128, 128])
        ones256 = P("ones256", [128, 128], BF16)
        ones128 = P("ones128", [128, 128])
        ones1 = P("ones1", [128, 128])
        CI = {n: i for i, n in enumerate(CONST_NAMES)}
        ident = cst[:, CI["ident"], :]

        def cload(eng, out, in_, key, sem="const"):
            S.op(eng, lambda e: e.dma_start(out=out, in_=in_), writes=[key], dma=sem, dma_batch=True)

        cload("sp", cst[:], dr["cst"][:, :, :], "cst")
        for l in range(DEPTH):
            cload("sp", vec[l][:], dr[f"vec{l}"][:, :], ("vec", l))
        cload("sp", gfin[:], dr["gfin"][:, :], "gfin")
        S.op("dve", lambda e: e.tensor_copy(out=cstb[:], in_=cst[:]), reads=["cst"], writes=["cstb"])
        S.op("dve", lambda e: e.memset(epsb[:], EPS), writes=["eps"])
        S.op("dve", lambda e: e.memset(lnsc[:], float(np.log(128.0 ** -0.5))), writes=["lnsc"])
        S.op("dve", lambda e: e.memset(onesm[:], 1.0 / 1024), writes=["onesm"])
        S.op("dve", lambda e: e.memset(ones256[:], 1.0 / 256), writes=["ones256"])
        S.op("dve", lambda e: e.memset(ones128[:], 1.0 / 128), writes=["ones128"])
        S.op("dve", lambda e: e.memset(ones1[:], 1.0), writes=["ones1"])

        def mm(out, pairs, reads, writes):
            def fn(e):
                n = len(pairs)
                ins = None
                for i, (l_, r_) in enumerate(pairs):
                    ins = e.matmul(out, l_, r_, start=(i == 0), stop=(i == n - 1))
                return ins
            return S.op("pe", fn, reads=reads, writes=writes)

        def xk(c, tb):
            return ("xT", c, tb)

        def hk(tb):
            return ("hT", tb)

        def TB(tb):
            return slice(tb * 512, (tb + 1) * 512)

        def dump(name, src_ap, key_reads=None):
            if name in dbg_out:
                S.barrier()
                if src_ap.dtype != F32:
                    with ExitStack() as ph2:
                        tmp = sbt(ph2, "dbgtmp", list(src_ap.shape), F32)
                        S.op("dve", lambda e: e.tensor_copy(out=tmp[:], in_=src_ap), writes=["dbgtmp"])
                        S.op("sp", lambda e: e.dma_start(out=dbg_out[name], in_=tmp[:]), reads=["dbgtmp"], dma="dbg")
                        S.barrier()
                else:
                    S.op("sp", lambda e: e.dma_start(out=dbg_out[name], in_=src_ap), dma="dbg")
                    S.barrier()

        with ExitStack() as ph:
            stage = [sbt(ph, "stg", [128, D], F32) for _ in range(2)]
            for tt in range(NT):
                st = stage[tt % 2]
                S.op("sp", lambda e, st=st, tt=tt: e.dma_start(out=st[:], in_=dr["x"][tt * 128:(tt + 1) * 128, :]),
                     writes=[("stg", tt % 2)], dma=f"xin{tt % 2}")
                for half in range(2):
                    p, pk = psb("all", ALLB)

                    def tr(e, st=st, half=half, p=p):
                        ins = None
                        for j in range(4):
                            c = half * 4 + j
                            ins = e.transpose(p[:, j * 128:(j + 1) * 128], st[:, c * 128:(c + 1) * 128], ident)
                        return ins
                    S.op("pe", tr, reads=[("stg", tt % 2), "cst"], writes=[pk])
                    S.op("dve" if half == 0 else "act",
                         (lambda e, half=half, p=p, tt=tt: e.tensor_copy(out=xT[:, half * 4:(half + 1) * 4, tt * 128:(tt + 1) * 128],
                                                                        in_=p[:].rearrange("p (j t) -> p j t", j=4))) if half == 0 else
                         (lambda e, half=half, p=p, tt=tt: e.copy(out=xT[:, half * 4:(half + 1) * 4, tt * 128:(tt + 1) * 128],
                                                                  in_=p[:].rearrange("p (j t) -> p j t", j=4))),
                         reads=[pk], writes=[xk(c, tt // 4) for c in range(half * 4, half * 4 + 4)])
            S.barrier()

        def rmsnorm_to_h(gcol_ap_fn, gkey, router=None):
            with ExitStack() as ph:
                sq = [sbt(ph, "sq", [128, 512], F32) for _ in range(2)]
                rs = [sbt(ph, "rs", [128, 512], F32) for _ in range(2)]
                if router is not None:
                    h32 = sbt(ph, "h32", [128, 8, 512], F32)
                for tb in range(NB):
                    p, pk = psb("n7", [0, 1, 2, 3, 4, 5, 6])
                    for c in range(8):
                        s_ = sq[c % 2]
                        S.op("act", lambda e, s_=s_, c=c, tb=tb: e.activation(out=s_[:], in_=xT[:, c, TB(tb)], func=AF.Square),
                             reads=[xk(c, tb)], writes=[("sq", c % 2)])
                        S.op("pe", lambda e, s_=s_, c=c, p=p: e.matmul(p[:], onesm[:], s_[:], start=(c == 0), stop=(c == 7)),
                             reads=[("sq", c % 2), "onesm"], writes=[pk])
                    r_ = rs[tb % 2]
                    S.op("act", lambda e, p=p, r_=r_: e.activation(out=r_[:], in_=p[:], func=AF.Ln, bias=epsb[:, 0:1], scale=1.0),
                         reads=[pk, "eps"], writes=[("rs", tb % 2)])
                    S.op("act", lambda e, r_=r_: e.activation(out=r_[:], in_=r_[:], func=AF.Exp, scale=-0.5), reads=[("rs", tb % 2)], writes=[("rs", tb % 2)])
                    for c in range(8):
                        S.op("dve",
                             lambda e, c=c, tb=tb, r_=r_: e.scalar_tensor_tensor(out=hT[:, c, TB(tb)], in0=xT[:, c, TB(tb)], scalar=gcol_ap_fn(c),
                                                                               in1=r_[:], op0=ALU.mult, op1=ALU.mult),
                             reads=[xk(c, tb), ("rs", tb % 2), gkey], writes=[("hT", c, tb)])
                    if router is not None:
                        for c in range(8):
                            S.op("dve", lambda e, c=c, tb=tb, r_=r_: e.scalar_tensor_tensor(out=h32[:, c, :], in0=xT[:, c, TB(tb)], scalar=gcol_ap_fn(c),
                                                                                        in1=r_[:], op0=ALU.mult, op1=ALU.mult),
                                 reads=[xk(c, tb), ("rs", tb % 2), gkey], writes=[("h32", c)])
                        router(tb, h32)
                S.barrier()

        def h_reads(tb):
            return [("hT", c, tb) for c in range(8)]

        def h_reads_all():
            return [("hT", c, tb) for c in range(8) for tb in range(NB)]

        C = Ctx()
        C.__dict__.update(locals())
        for l in range(DEPTH):
            g0 = V_GMIX
            rmsnorm_to_h(lambda c, l=l: vec[l][:, V_GMIX + c:V_GMIX + c + 1], ("vec", l))
            dump(f"h{l}", hT[:], None)
            if stop_after == f"norm{l}":
                break
            stop = False
            with ExitStack() as mixph:
                C.yT = [sbt(mixph, f"y{b}T", [128, 4, T], BF16) for b in range(3)]
                for nm, fn in (("B", mixer_B), ("A", mixer_A), ("C", mixer_C), ("M", merge_out)):
                    if nm in SKIP:
                        continue
                    fn(C, l)
                    if stop_after == f"{nm}{l}":
                        stop = True
                        break
                if stop:
                    S.barrier()
            if stop:
                break
            if l == 0:
                rmsnorm_to_h(lambda c, l=l: vec[l][:, V_GFFN + c:V_GFFN + c + 1], ("vec", l))
                ffn_dense(C)
            else:
                moe(C, l)
            if stop_after == f"F{l}":
                break
        final_out(C)
        waits = [("sp", n) for n in S.dma_sems if n.startswith("yout") or n == "dbg"]
        S.emit(final_waits=waits)
    return nc


def attn_core(C, k_ap, q_ap, v_ap, par, out_ap, scale, kkeys, qkeys, vkeys, okey_fn, PT, rec):
    S, ps, psb = C.S, C.ps, C.psb
    vr = slice(par * 64, par * 64 + 64)
    sr = slice((1 - par) * 64, (1 - par) * 64 + 64)
    for qb in range(NB):
        acc, acck = psb("acc", [0, 1])
        pend = []

        def issue_pv(item, last):
            kt, pt_i = item
            S.op("pe", lambda e, kt=kt, pt_i=pt_i, acc=acc: e.matmul(acc[:], v_ap(kt), PT[pt_i][:], start=(kt == 0), stop=(kt == NT - 1)),
                 reads=[("PT", pt_i)] + vkeys, writes=[acck])
        for kt in range(NT):
            sp_, spk = psb("s", [2, 3, 4])
            S.op("pe", lambda e, kt=kt, qb=qb, sp_=sp_: e.matmul(sp_[:], k_ap(kt), q_ap(qb), start=True, stop=True),
                 reads=kkeys + qkeys, writes=[spk])
            pt_i = C.pt_rr[0] % len(PT)
            C.pt_rr[0] += 1
            S.op("act", lambda e, sp_=sp_, pt_i=pt_i: e.activation(out=PT[pt_i][:], in_=sp_[:], func=AF.Exp, scale=scale),
                 reads=[spk], writes=[("PT", pt_i)])
            pend.append((kt, pt_i))
            if len(pend) > 1:
                issue_pv(pend.pop(0), False)
        while pend:
            issue_pv(pend.pop(0), True)
        r_i = C.pt_rr[1] % len(rec)
        C.pt_rr[1] += 1
        S.op("dve", lambda e, acc=acc, r_i=r_i: e.reciprocal(out=rec[r_i][vr, :], in_=acc[sr, :]),
             reads=[acck], writes=[("rec", r_i)])
        S.op("dve", lambda e, acc=acc, r_i=r_i, qb=qb: e.tensor_tensor(out=out_ap(qb), in0=acc[vr, :], in1=rec[r_i][vr, :], op=ALU.mult),
             reads=[acck, ("rec", r_i)], writes=[okey_fn(qb)])


def mixer_A(C, l):
    S, nc, dr, hT, vec, cstb, psb, sbt = C.S, C.nc, C.dr, C.hT, C.vec, C.cstb, C.psb, C.sbt
    CI, TB, epsb = C.CI, C.TB, C.epsb
    yaT = C.yT[0]
    PJ = C.PJ
    with ExitStack() as ph:
        wA = sbt(ph, "wA", [128, 8, 576], BF16)
        qT = sbt(ph, "qTa", [128, 2, T], BF16)
        kT = sbt(ph, "kTa", [128, 2, T], BF16)
        Va = [sbt(ph, "Va", [128, NT, 128], BF16) for _ in range(2)]
        PT = [sbt(ph, "PT", [128, 512], BF16) for _ in range(3)]
        rec = [sbt(ph, "rec", [128, 512], F32) for _ in range(1)]
        rope = [sbt(ph, "ropeA", [128, 2, 512], F32) for _ in range(1)]
        sqb = [sbt(ph, "sqb", [128, 512], BF16) for _ in range(1)]
        ub = [sbt(ph, "ub", [128, 512], BF16) for _ in range(1)]
        rsb = [sbt(ph, "rsb", [128, 512], F32) for _ in range(1)]
        t1 = [sbt(ph, "t1", [128, 512], F32) for _ in range(1)]
        t2 = [sbt(ph, "t2", [128, 512], F32) for _ in range(1)]
        onesA = sbt(ph, "onesA", [128, 512], F32)
        S.op("dve", lambda e: e.memset(onesA[:], 1.0), writes=["onesA"])
        C.pt_rr = [0, 0]
        blk = cstb[:, CI["blk64"], :]
        perm = cstb[:, CI["permA"], :]
        for g in range(2):
            S.op("pool", lambda e, g=g: e.dma_start(out=wA[:], in_=dr[f"wA{l}"][g]), writes=["wA"], dma="wA")
            S.op("pool", lambda e: e.memset(Va[0][:], 1.0), writes=["Va"])
            S.op("pool", lambda e: e.memset(Va[1][:], 1.0), writes=["Va1"])
            bi = 0
            for tb in range(NB):
                rp = rope[0]
                S.op("sp", lambda e, rp=rp, tb=tb: e.dma_start(out=rp[:], in_=dr["ropeA"][:, :, TB(tb)]),
                     writes=[("ropeA", 0)], dma="ropeA0")
                for blkid in range(4):
                    col0 = blkid * 128
                    gcol = vec[l][:, V_GAQ:V_GAQ + 1] if blkid < 2 else vec[l][:, V_GAK:V_GAK + 1]
                    i = 0
                    p1, p1k = psb("pj", PJ)
                    C.mm(p1[:], [(wA[:, k, col0:col0 + 128], hT[:, k, TB(tb)]) for k in range(8)],
                         reads=["wA"] + C.h_reads(tb), writes=[p1k])
                    S.op("act", lambda e, p1=p1, i=i: e.activation(out=sqb[i][:], in_=p1[:], func=AF.Square),
                         reads=[p1k], writes=[("sqb", i)])
                    S.op("dve", lambda e, p1=p1, i=i, gcol=gcol: e.scalar_tensor_tensor(out=ub[i][:], in0=p1[:], scalar=gcol, in1=onesA[:], op0=ALU.mult, op1=ALU.mult),
                         reads=[p1k, ("vec", l), "onesA"], writes=[("ub", i)])
                    p2, p2k = psb("pj", PJ)
                    C.mm(p2[:], [(blk, sqb[i][:])], reads=["cstb", ("sqb", i)], writes=[p2k])
                    p3, p3k = psb("pj", PJ)
                    C.mm(p3[:], [(perm, ub[i][:])], reads=["cstb", ("ub", i)], writes=[p3k])
                    S.op("act", lambda e, p2=p2, i=i: e.activation(out=rsb[i][:], in_=p2[:], func=AF.Ln, bias=epsb[:, 0:1], scale=1.0),
                         reads=[p2k, "eps"], writes=[("rsb", i)])
                    S.op("act", lambda e, i=i: e.activation(out=rsb[i][:], in_=rsb[i][:], func=AF.Exp, scale=-0.5), reads=[("rsb", i)], writes=[("rsb", i)])
                    S.op("dve", lambda e, i=i, rp=rp: e.tensor_tensor(out=t1[i][:], in0=ub[i][:], in1=rp[:, 0, :], op=ALU.mult),
                         reads=[("ub", i), ("ropeA", 0)], writes=[("t1", i)])
                    S.op("dve", lambda e, i=i, rp=rp, p3=p3: e.tensor_tensor(out=t2[i][:], in0=p3[:], in1=rp[:, 1, :], op=ALU.mult),
                         reads=[p3k, ("ropeA", 0)], writes=[("t2", i)])
                    S.op("pool", lambda e, i=i: e.tensor_tensor(out=t1[i][:], in0=t1[i][:], in1=t2[i][:], op=ALU.add),
                         reads=[("t1", i), ("t2", i)], writes=[("t1", i)])
                    dst = qT[:, blkid, TB(tb)] if blkid < 2 else kT[:, blkid - 2, TB(tb)]
                    dkey = ("qTa", blkid, tb) if blkid < 2 else ("kTa", blkid - 2, tb)
                    S.op("dve", lambda e, i=i, dst=dst: e.tensor_tensor(out=dst, in0=t1[i][:], in1=rsb[i][:], op=ALU.mult),
                         reads=[("t1", i), ("rsb", i)], writes=[dkey])
            for tg in range(4):
                pv, pvk = psb("pj", PJ)

                def vfn(e, tg=tg, pv=pv):
                    ins = None
                    for j in range(4):
                        tt = tg * 4 + j
                        for k in range(8):
                            ins = e.matmul(pv[:, j * 64:(j + 1) * 64], hT[:, k, tt * 128:(tt + 1) * 128], wA[:, k, 512:576],
                                           start=(k == 0), stop=(k == 7))
                    return ins
                S.op("pe", vfn, reads=["wA"] + C.h_reads(tg), writes=[pvk])
                S.op("dve", lambda e, tg=tg, pv=pv: e.tensor_copy(out=Va[0][:, tg * 4:(tg + 1) * 4, 0:64],
                                                                  in_=pv[:, 0:256].rearrange("p (j d) -> p j d", j=4)),
                     reads=[pvk, "Va"], writes=[("Vav", tg)])
                S.op("act", lambda e, tg=tg, pv=pv: e.copy(out=Va[1][:, tg * 4:(tg + 1) * 4, 64:128],
                                                          in_=pv[:, 0:256].rearrange("p (j d) -> p j d", j=4)),
                     reads=[pvk, "Va1"], writes=[("Vav1", tg)])
            vkeys = [("Vav", tg) for tg in range(4)] + ["Va", "Va1"] + [("Vav1", tg) for tg in range(4)]
            for hl in HLS:
                cl, par = hl // 2, hl % 2
                pr = slice(par * 64, par * 64 + 64)
                chunk = 2 * g + cl
                attn_core(C,
                          k_ap=lambda kt, par=par: kT[:, par, kt * 128:(kt + 1) * 128],
                          q_ap=lambda qb, cl=cl: qT[:, cl, TB(qb)],
                          v_ap=(lambda kt: Va[0][:, kt, :]) if par == 0 else (lambda kt: Va[1][:, kt, :]),
                          par=par,
                          out_ap=lambda qb, pr=pr, chunk=chunk: yaT[pr, chunk, TB(qb)],
                          scale=64.0 ** -0.5,
                          kkeys=[("kTa", par, tb) for tb in range(NB)], qkeys=[("qTa", cl, tb) for tb in range(NB)], vkeys=vkeys,
                          okey_fn=lambda qb, chunk=chunk, par=par: ("y0T", chunk, par, qb), PT=PT, rec=rec)
        S.barrier()
    C.dump(f"ya{l}", yaT[:])


def mixer_B(C, l):
    S, nc, dr, hT, vec, cstb, psb, sbt = C.S, C.nc, C.dr, C.hT, C.vec, C.cstb, C.psb, C.sbt
    CI, TB, epsb, ones256 = C.CI, C.TB, C.epsb, C.ones256
    ybT = C.yT[1]
    PJ = C.PJ
    with ExitStack() as ph:
        cqT = sbt(ph, "cqT", [128, 2, T], BF16)
        ckvT = sbt(ph, "ckvT", [128, 2, T], BF16)
        kh = sbt(ph, "kh", [128, T], BF16)
        rope = [sbt(ph, "ropeB", [128, 2, 512], F32) for _ in range(2)]
        t1 = [sbt(ph, "t1b", [128, 512], F32) for _ in range(1)]
        t2 = [sbt(ph, "t2b", [128, 512], F32) for _ in range(1)]
        with ExitStack() as ph1:
            w1 = sbt(ph1, "wB1", [128, 8, 704], BF16)
            sqb = [sbt(ph1, "sqbB", [128, 512], BF16) for _ in range(4)]
            rsb = [sbt(ph1, "rsbB", [128, 512], F32) for _ in range(2)]
            S.op("pool", lambda e: e.dma_start(out=w1[:], in_=dr[f"wB1{l}"][:, :, :]), writes=["wB1"], dma="wB1")
            ni = 0
            for tb in range(NB):
                rp = rope[0]
                S.op("sp", lambda e, rp=rp, tb=tb: e.dma_start(out=rp[:], in_=dr["ropeB"][:, :, TB(tb)]),
                     writes=[("ropeB", 0)], dma="ropeB0")
                for which in range(2):
                    dstT = cqT if which == 0 else ckvT
                    gbase = V_GBQ if which == 0 else V_GBKV
                    pp = []
                    for c in range(2):
                        p1, p1k = psb("pj", PJ)
                        col0 = which * 256 + c * 128
                        C.mm(p1[:], [(w1[:, k, col0:col0 + 128], hT[:, k, TB(tb)]) for k in range(8)],
                             reads=["wB1"] + C.h_reads(tb), writes=[p1k])
                        si = (ni * 2 + c) % 4
                        S.op("act", lambda e, p1=p1, si=si: e.activation(out=sqb[si][:], in_=p1[:], func=AF.Square),
                             reads=[p1k], writes=[("sqbB", si)])
                        pp.append((p1, p1k, si))
                    p2, p2k = psb("pj", PJ)
                    C.mm(p2[:], [(ones256[:], sqb[pp[0][2]][:]), (ones256[:], sqb[pp[1][2]][:])],
                         reads=["ones256", ("sqbB", pp[0][2]), ("sqbB", pp[1][2])], writes=[p2k])
                    ri = ni % 2
                    ni += 1
                    S.op("act", lambda e, p2=p2, ri=ri: e.activation(out=rsb[ri][:], in_=p2[:], func=AF.Ln, bias=epsb[:, 0:1], scale=1.0),
                         reads=[p2k, "eps"], writes=[("rsbB", ri)])
                    S.op("act", lambda e, ri=ri: e.activation(out=rsb[ri][:], in_=rsb[ri][:], func=AF.Exp, scale=-0.5), reads=[("rsbB", ri)], writes=[("rsbB", ri)])
                    for c in range(2):
                        p1, p1k, si = pp[c]
                        S.op("dve", lambda e, p1=p1, c=c, ri=ri, dstT=dstT, gbase=gbase, tb=tb: e.scalar_tensor_tensor(
                            out=dstT[:, c, TB(tb)], in0=p1[:], scalar=vec[l][:, gbase + c:gbase + c + 1], in1=rsb[ri][:], op0=ALU.mult, op1=ALU.mult),
                            reads=[p1k, ("rsbB", ri), ("vec", l)], writes=[("lat", which, c, tb)])
                pa, pak = psb("pj", PJ)
                C.mm(pa[0:96, :], [(w1[:, k, 512:608], hT[:, k, TB(tb)]) for k in range(8)], reads=["wB1"] + C.h_reads(tb), writes=[pak])
                pb, pbk = psb("pj", PJ)
                C.mm(pb[0:96, :], [(w1[:, k, 608:704], hT[:, k, TB(tb)]) for k in range(8)], reads=["wB1"] + C.h_reads(tb), writes=[pbk])
                i = 0
                rr_ = slice(64, 96)
                S.op("dve", lambda e, pa=pa, rp=rp, i=i: e.tensor_tensor(out=t1[i][rr_, :], in0=pa[rr_, :], in1=rp[rr_, 0, :], op=ALU.mult),
                     reads=[pak, ("ropeB", 0)], writes=[("t1b", i)])
                S.op("dve", lambda e, pb=pb, rp=rp, i=i: e.tensor_tensor(out=t2[i][rr_, :], in0=pb[rr_, :], in1=rp[rr_, 1, :], op=ALU.mult),
                     reads=[pbk, ("ropeB", 0)], writes=[("t2b", i)])
                S.op("pool", lambda e, i=i, tb=tb: e.tensor_tensor(out=kh[rr_, TB(tb)], in0=t1[i][rr_, :], in1=t2[i][rr_, :], op=ALU.add),
                     reads=[("t1b", i), ("t2b", i)], writes=[("krT", tb)])
            S.barrier()
        C.dump(f"cq{l}", cqT[:])
        w2 = sbt(ph, "wB2", [128, 5120], BF16)
        qh = sbt(ph, "qh", [128, T], BF16)
        Vb = sbt(ph, "Vb", [128, NT, 128], BF16)
        PT = [sbt(ph, "PTb", [128, 512], BF16) for _ in range(2)]
        rec = [sbt(ph, "recb", [128, 512], F32) for _ in range(1)]
        C.pt_rr = [0, 0]
        S.op("pool", lambda e: e.dma_start(out=w2[:], in_=dr[f"wB2{l}"][:, :]), writes=["wB2"], dma="wB2")
        wq = w2[:, 0:3072].rearrange("p (k h v m) -> p k h v m", k=2, h=8, v=2)
        wk = w2[:, 3072:4096].rearrange("p (k h m) -> p k h m", k=2, h=8)
        wv = w2[:, 4096:5120].rearrange("p (k h m) -> p k h m", k=2, h=8)
        latq = [("lat", 0, c, tb) for c in range(2) for tb in range(NB)]
        latkv = [("lat", 1, c, tb) for c in range(2) for tb in range(NB)]
        for h in range(8):
            par = h % 2
            for tb in range(NB):
                ri_ = (h * NB + tb) % 2
                rp = rope[ri_]
                S.op("sp", lambda e, rp=rp, tb=tb: e.dma_start(out=rp[:], in_=dr["ropeB"][:, :, TB(tb)]),
                     writes=[("ropeB", ri_)], dma=f"ropeB{ri_}")
                pa, pak = psb("pj", PJ)
                C.mm(pa[0:96, :], [(wq[:, k, h, 0, :], cqT[:, k, TB(tb)]) for k in range(2)], reads=["wB2"] + latq, writes=[pak])
                pb, pbk = psb("pj", PJ)
                C.mm(pb[0:96, :], [(wq[:, k, h, 1, :], cqT[:, k, TB(tb)]) for k in range(2)], reads=["wB2"] + latq, writes=[pbk])
                i = 0
                r96 = slice(0, 96)
                S.op("dve", lambda e, pa=pa, rp=rp, i=i: e.tensor_tensor(out=t1[i][r96, :], in0=pa[r96, :], in1=rp[r96, 0, :], op=ALU.mult),
                     reads=[pak, ("ropeB", ri_)], writes=[("t1b", i)])
                S.op("dve", lambda e, pb=pb, rp=rp, i=i: e.tensor_tensor(out=t2[i][r96, :], in0=pb[r96, :], in1=rp[r96, 1, :], op=ALU.mult),
                     reads=[pbk, ("ropeB", ri_)], writes=[("t2b", i)])
                S.op("pool", lambda e, i=i, tb=tb: e.tensor_tensor(out=qh[r96, TB(tb)], in0=t1[i][r96, :], in1=t2[i][r96, :], op=ALU.add),
                     reads=[("t1b", i), ("t2b", i)], writes=[("qh", tb)])
                pc, pck = psb("pj", PJ)
                C.mm(pc[0:64, :], [(wk[:, k, h, :], ckvT[:, k, TB(tb)]) for k in range(2)], reads=["wB2"] + latkv, writes=[pck])
                S.op("act", lambda e, pc=pc, tb=tb: e.copy(out=kh[0:64, TB(tb)], in_=pc[0:64, :]), reads=[pck], writes=[("kh", tb)])
            voff = 0 if par == 0 else 64
            S.op("pool", lambda e, ooff=64 - voff: e.memset(Vb[:, :, ooff:ooff + 64], 1.0), writes=["Vb1"])
            for tg in range(4):
                pv, pvk = psb("pj", PJ)

                def vfn(e, tg=tg, pv=pv, h=h):
                    ins = None
                    for j in range(4):
                        tt = tg * 4 + j
                        for k in range(2):
                            ins = e.matmul(pv[:, j * 64:(j + 1) * 64], ckvT[:, k, tt * 128:(tt + 1) * 128], wv[:, k, h, :],
                                           start=(k == 0), stop=(k == 1))
                    return ins
                S.op("pe", vfn, reads=["wB2"] + latkv, writes=[pvk])
                S.op("dve", lambda e, tg=tg, pv=pv, voff=voff: e.tensor_copy(out=Vb[:, tg * 4:(tg + 1) * 4, voff:voff + 64],
                                                                            in_=pv[:, 0:256].rearrange("p (j d) -> p j d", j=4)),
                     reads=[pvk], writes=[("Vbv", tg)])
            pr = slice(par * 64, par * 64 + 64)
            chunk = h // 2
            attn_core(C,
                      k_ap=lambda kt: kh[0:96, kt * 128:(kt + 1) * 128],
                      q_ap=lambda qb: qh[0:96, TB(qb)],
                      v_ap=lambda kt: Vb[:, kt, :],
                      par=par,
                      out_ap=lambda qb, pr=pr, chunk=chunk: ybT[pr, chunk, TB(qb)],
                      scale=96.0 ** -0.5,
                      kkeys=[("kh", tb) for tb in range(NB)], qkeys=[("qh", tb) for tb in range(NB)],
                      vkeys=[("Vbv", tg) for tg in range(4)] + ["Vb1"],
                      okey_fn=lambda qb, chunk=chunk, par=par: ("y1T", chunk, par, qb), PT=PT, rec=rec)
        S.barrier()
    C.dump(f"yb{l}", ybT[:])


def mixer_C(C, l):
    S, nc, dr, hT, vec, cst, cstb, sbt, ps = C.S, C.nc, C.dr, C.hT, C.vec, C.cst, C.cstb, C.sbt, C.ps
    CI, TB, epsb, ones128, lnsc = C.CI, C.TB, C.epsb, C.ones128, C.lnsc
    ycT = C.yT[2]
    with ExitStack() as ph:
        cT = sbt(ph, "cT", [128, T], BF16)
        with ExitStack() as ph0:
            wg_ = sbt(ph0, "wCg", [128, 8, 64], BF16)
            S.op("pool", lambda e: e.dma_start(out=wg_[:], in_=dr[f"wCg{l}"][:, :, :]), writes=["wCg"], dma="wCg")
            S.op("dve", lambda e: e.memset(cT[:], 1.0), writes=["cT"])
            for tb in range(NB):
                p, pk = C.psb("all", C.ALLB)
                C.mm(p[0:64, :], [(wg_[:, k, :], hT[:, k, TB(tb)]) for k in range(8)], reads=["wCg"] + C.h_reads(tb), writes=[pk])
                S.op("act", lambda e, p=p, tb=tb: e.copy(out=cT[0:48, TB(tb)], in_=p[0:48, :]), reads=[pk, "cT"], writes=[("cTv", tb)])
            S.barrier()
        wup = sbt(ph, "wup", [128, 2, 128], BF16)
        wh = sbt(ph, "wCh", [128, 5120], BF16)
        qT = sbt(ph, "qTc", [128, T], BF16)
        kT = sbt(ph, "kTc", [128, T], BF16)
        ktok = sbt(ph, "ktok", [128, NT, 128], BF16)
        vtok = sbt(ph, "vtok", [128, NT, 128], BF16)
        oT = sbt(ph, "oT", [128, T], F32)
        sp_ = sbt(ph, "sp", [128, 4, 128], F32)
        e1 = sp_[:].rearrange("p a b -> p (a b)")
        eb = sbt(ph, "eb", [128, 512], F32)
        enb = eb
        eg = sbt(ph, "eg", [128, 4, 128], F32)
        kt_ = sbt(ph, "kt", [128, 512], BF16)
        qt2 = [sbt(ph, "qt", [128, 512], BF16) for _ in range(2)]
        kd2 = [[sbt(ph, "kd", [128, 4, 128], BF16) for _ in range(2)] for _ in range(2)]
        ms2 = [sbt(ph, "ms", [128, 4, 128], BF16) for _ in range(2)]
        dec2 = [sbt(ph, "dec", [128, 8], F32) for _ in range(2)]
        Sst = [sbt(ph, "Sst", [128, 128], F32) for _ in range(2)]
        Sbf = [sbt(ph, "Sbf", [128, 128], BF16) for _ in range(4)]
        sqo = sp_[:].rearrange("p a b -> p (a b)")
        rso = eb
        sgo = eg[:].rearrange("p a b -> p (a b)")
        ckeys = []
        tri = {0: cst[:, CI["trif"], :], 1: cst[:, CI["trib"], :]}
        trib16 = {0: cstb[:, CI["trif"], :], 1: cstb[:, CI["trib"], :]}
        uu = {0: cst[:, CI["uf"], :], 1: cst[:, CI["ub"], :]}
        wl = wh[:, 0:3072].rearrange("p (k v m) -> p k v m", k=8, v=3)
        wr = wh[:, 3072:5120].rearrange("p (k m) -> p k m", k=8)
        si = [0]
        for h in range(4):
            S.op("pool", lambda e, h=h: e.dma_start(out=wh[:], in_=dr[f"wC{l}"][h]), writes=["wCh"], dma="wCh")
            S.op("pool", lambda e, h=h: e.dma_start(out=wup[:].rearrange("p b c -> p (b c)"), in_=dr[f"wCup{l}"][:, h * 256:(h + 1) * 256]),
                 writes=["wup"], dma="wCup")
            for gi in range(NB):
                for which, dst, dk in ((0, qT, "qTc"), (1, kT, "kTc")):
                    p, pk = C.psb("all", C.ALLB)
                    C.mm(p[:], [(wl[:, k, which, :], hT[:, k, TB(gi)]) for k in range(8)], reads=["wCh"] + C.h_reads(gi), writes=[pk])
                    S.op("act", lambda e, p=p, dst=dst, gi=gi: e.copy(out=dst[:, TB(gi)], in_=p[:]), reads=[pk], writes=[(dk, gi)])
                for half in range(2):
                    p, pk = C.psb("all", C.ALLB)

                    def kvfn(e, p=p, gi=gi, half=half):
                        ins = None
                        for j in range(2):
                            tt = gi * 4 + half * 2 + j
                            for k in range(8):
                                ins = e.matmul(p[:, j * 256:(j + 1) * 256], hT[:, k, tt * 128:(tt + 1) * 128], wr[:, k, :], start=(k == 0), stop=(k == 7))
                        return ins
                    S.op("pe", kvfn, reads=["wCh"] + C.h_reads(gi), writes=[pk])
                    t0 = gi * 4 + half * 2
                    pv = p[:].rearrange("p (j w) -> p j w", j=2)
                    S.op("dve", lambda e, pv=pv, t0=t0: e.tensor_copy(out=ktok[:, t0:t0 + 2, :], in_=pv[:, :, 0:128]), reads=[pk], writes=[("ktok", t0 // 2)])
                    S.op("act", lambda e, pv=pv, t0=t0: e.copy(out=vtok[:, t0:t0 + 2, :], in_=pv[:, :, 128:256]), reads=[pk], writes=[("vtok", t0 // 2)])
            def prologue(d, gi, pb_):
                qt, kd, ms, dec = qt2[pb_], kd2[pb_], ms2[pb_], dec2[pb_]
                pspk = ("ps", 0)

                def spfn(e, gi=gi, d=d):
                    ins = None
                    for j in range(4):
                        tt = gi * 4 + j
                        ins = e.matmul(ps[0][:, j * 128:(j + 1) * 128], cT[:, tt * 128:(tt + 1) * 128], wup[:, d, :], start=True, stop=True)
                    return ins
                S.op("pe", spfn, reads=ckeys + ["wup"], writes=[pspk])
                S.op("act", lambda e: e.activation(out=e1, in_=ps[0][:], func=AF.Exp, scale=-1.0), reads=[pspk], writes=["sp"])
                S.op("act", lambda e: e.activation(out=sp_[:].rearrange("p a b -> p (a b)"), in_=e1, func=AF.Ln, bias=C.ones1[:, 0:1], scale=1.0),
                     reads=["sp", "ones1"], writes=["sp"])

                def cfn(e, d=d):
                    ins = None
                    for j in range(4):
                        ins = e.matmul(ps[1][:, j * 128:(j + 1) * 128], sp_[:, j, :], tri[d], start=True, stop=True)
                    return ins
                S.op("pe", cfn, reads=["sp", "cst"], writes=[("ps", 1)])
                S.op("act", lambda e: e.activation(out=eb[:], in_=ps[1][:], func=AF.Exp, scale=-1.0 / 16, bias=lnsc[:, 0:1]),
                     reads=[("ps", 1), "lnsc"], writes=["eb"])
                S.op("dve", lambda e, gi=gi: e.tensor_tensor(out=qt[:], in0=qT[:, TB(gi)], in1=eb[:], op=ALU.mult), reads=[("qTc", gi), "eb"], writes=[("qt", pb_)])
                S.op("act", lambda e: e.activation(out=enb[:], in_=ps[1][:], func=AF.Exp, scale=1.0 / 16), reads=[("ps", 1)], writes=["eb"])
                S.op("pool", lambda e, gi=gi: e.tensor_tensor(out=kt_[:], in0=kT[:, TB(gi)], in1=enb[:], op=ALU.mult), reads=[("kTc", gi), "eb"], writes=["kt"])
                pb3 = ps[1][:].rearrange("p (c s) -> p c s", c=8)
                col = 63 if d == 0 else 0
                S.op("act", lambda e, pb3=pb3, col=col: e.activation(out=dec[:], in_=pb3[:, :, col], func=AF.Exp, scale=-1.0 / 16),
                     reads=[("ps", 1)], writes=[("dec", pb_)])

                def gfn(e, d=d):
                    ins = None
                    for j in range(4):
                        ins = e.matmul(ps[2][:, j * 128:(j + 1) * 128], uu[d], sp_[:, j, :], start=True, stop=True)
                    return ins
                S.op("pe", gfn, reads=["sp", "cst"], writes=[("ps", 2)])
                S.op("act", lambda e: e.activation(out=eg[:].rearrange("p a b -> p (a b)"), in_=ps[2][:], func=AF.Exp, scale=-1.0 / 16),
                     reads=[("ps", 2)], writes=["eg"])
                for ch_ in range(2):
                    rmask = cst[:, CI["trif"], 63:64] if ch_ == 0 else cst[:, CI["trib"], 64:65]
                    S.op("dve", lambda e, gi=gi, ch_=ch_, rmask=rmask: e.scalar_tensor_tensor(out=kd[ch_][:], in0=eg[:], scalar=rmask, in1=ktok[:, gi * 4:gi * 4 + 4, :],
                                                                                          op0=ALU.mult, op1=ALU.mult),
                         reads=[("ktok", gi * 2), ("ktok", gi * 2 + 1), "eg", "cst"], writes=[("kd", pb_, ch_)])

                def sfn(e):
                    ins = None
                    for j in range(4):
                        ins = e.matmul(ps[3][:, j * 128:(j + 1) * 128], kt_[:, j * 128:(j + 1) * 128], qt[:, j * 128:(j + 1) * 128], start=True, stop=True)
                    return ins
                S.op("pe", sfn, reads=["kt", ("qt", pb_)], writes=[("ps", 3)])
                S.op("dve", lambda e, d=d: e.tensor_tensor(out=ms[:], in0=ps[3][:].rearrange("p (j c) -> p j c", j=4),
                                                          in1=trib16[d].unsqueeze(1).to_broadcast([128, 4, 128]), op=ALU.mult),
                     reads=[("ps", 3), "cstb"], writes=[("ms", pb_)])

            def chain(d, gi, pb_, first):
                qt, kd, ms, dec = qt2[pb_], kd2[pb_], ms2[pb_], dec2[pb_]
                if first:
                    cur = si[0] % 2
                    S.op("dve", lambda e, cur=cur: e.memset(Sst[cur][:], 0.0), writes=[("Sst", cur)])
                    sb_i = si[0] % 4
                    S.op("pool", lambda e, sb_i=sb_i: e.memset(Sbf[sb_i][:], 0.0), writes=[("Sbf", sb_i)])
                tiles = list(range(gi * 4, gi * 4 + 4))
                ob = 4 + (si[0] % 2)
                po, pok = ps[ob], ("ps", ob)
                torder = tiles if d == 0 else tiles[::-1]
                for tt in torder:
                    j = tt - gi * 4
                    corder = (0, 1) if d == 0 else (1, 0)
                    S.op("pe", lambda e, tt=tt, j=j, po=po: e.matmul(po[:, j * 128:(j + 1) * 128], vtok[:, tt, :], ms[:, j, :], start=True, stop=False),
                         reads=[("vtok", tt // 2), ("ms", pb_)], writes=[pok])
                    for ci, ch in enumerate(corder):
                        cur = si[0] % 2
                        sb_i = si[0] % 4
                        csl = slice(j * 128 + ch * 64, j * 128 + ch * 64 + 64)
                        S.op("pe", lambda e, po=po, csl=csl, sb_i=sb_i, last=(ci == 1): e.matmul(po[:, csl], Sbf[sb_i][:], qt[:, csl], start=False, stop=last),
                             reads=[("Sbf", sb_i), ("qt", pb_)], writes=[pok])
                        kb = 6 + (si[0] % 2)
                        S.op("pe", lambda e, kb=kb, ch=ch, j=j, tt=tt: e.matmul(ps[kb][:, 0:128], kd[ch][:, j, :], vtok[:, tt, :], start=True, stop=True),
                             reads=[("kd", pb_, ch), ("vtok", tt // 2)], writes=[("ps", kb)])
                        nxt = 1 - cur
                        dcol = j * 2 + ch
                        nb_ = (si[0] + 1) % 4
                        S.op("dve", lambda e, kb=kb, cur=cur, nb_=nb_, dcol=dcol: e.scalar_tensor_tensor(
                            out=Sbf[nb_][:], in0=Sst[cur][:], scalar=dec[:, dcol:dcol + 1], in1=ps[kb][:, 0:128], op0=ALU.mult, op1=ALU.add),
                            reads=[("Sst", cur), ("dec", pb_), ("ps", kb)], writes=[("Sbf", nb_)])
                        S.op("dve", lambda e, kb=kb, cur=cur, nxt=nxt, dcol=dcol: e.scalar_tensor_tensor(
                            out=Sst[nxt][:], in0=Sst[cur][:], scalar=dec[:, dcol:dcol + 1], in1=ps[kb][:, 0:128], op0=ALU.mult, op1=ALU.add),
                            reads=[("Sst", cur), ("dec", pb_), ("ps", kb)], writes=[("Sst", nxt)])
                        si[0] += 1
                if d == 0:
                    S.op("act", lambda e, po=po, gi=gi: e.copy(out=oT[:, TB(gi)], in_=po[:]), reads=[pok], writes=[("oT", gi)])
                else:
                    S.op("dve", lambda e, po=po, gi=gi: e.tensor_tensor(out=oT[:, TB(gi)], in0=oT[:, TB(gi)], in1=po[:], op=ALU.add),
                         reads=[pok, ("oT", gi)], writes=[("oT", gi)])

            steps = [(0, gi, gi == 0) for gi in range(NB)] + [(1, gi, gi == NB - 1) for gi in range(NB - 1, -1, -1)]
            prologue(steps[0][0], steps[0][1], 0)
            for k_, (d, gi, first) in enumerate(steps):
                if k_ + 1 < len(steps):
                    prologue(steps[k_ + 1][0], steps[k_ + 1][1], (k_ + 1) % 2)
                chain(d, gi, k_ % 2, first)
            for gi in range(NB):
                S.op("act", lambda e, gi=gi: e.activation(out=sqo, in_=oT[:, TB(gi)], func=AF.Square), reads=[("oT", gi)], writes=["sp"])
                C.mm(ps[0][:], [(ones128[:], sqo)], reads=["ones128", "sp"], writes=[("ps", 0)])
                S.op("act", lambda e: e.activation(out=rso[:], in_=ps[0][:], func=AF.Ln, bias=epsb[:, 0:1], scale=1.0), reads=[("ps", 0), "eps"], writes=["eb"])
                S.op("act", lambda e: e.activation(out=rso[:], in_=rso[:], func=AF.Exp, scale=-0.5), reads=["eb"], writes=["eb"])
                C.mm(ps[1][:], [(wl[:, k, 2, :], hT[:, k, TB(gi)]) for k in range(8)], reads=["wCh"] + C.h_reads(gi), writes=[("ps", 1)])
                S.op("act", lambda e: e.activation(out=sgo, in_=ps[1][:], func=AF.Silu), reads=[("ps", 1)], writes=["eg"])
                S.op("dve", lambda e, gi=gi: e.scalar_tensor_tensor(out=oT[:, TB(gi)], in0=oT[:, TB(gi)], scalar=vec[l][:, V_GCO:V_GCO + 1], in1=rso[:],
                                                                 op0=ALU.mult, op1=ALU.mult), reads=[("oT", gi), "eb", ("vec", l)], writes=[("oT", gi)])
                S.op("dve", lambda e, gi=gi, h=h: e.tensor_tensor(out=ycT[:, h, TB(gi)], in0=oT[:, TB(gi)], in1=sgo, op=ALU.mult),
                     reads=[("oT", gi), "eg"], writes=[("y2T", h, gi)])
            if h == 0:
                C.dump(f"oc{l}", oT[:])
        S.barrier()
    C.dump(f"yc{l}", ycT[:])


def merge_out(C, l):
    S, dr, hT, vec, sbt, psb, TB, xT = C.S, C.dr, C.hT, C.vec, C.sbt, C.psb, C.TB, C.xT
    yT = C.yT
    with ExitStack() as ph:
        mT = sbt(ph, "mT", [128, 8, T], BF16)
        with ExitStack() as ph1:
            wm = [sbt(ph1, "wm", [128, 1536], BF16) for _ in range(3)]
            sg = [sbt(ph1, "sg", [128, 512], BF16) for _ in range(2)]
            acc = sbt(ph1, "macc", [128, NB, 512], F32)
            tm = [sbt(ph1, "tm", [128, 512], F32) for _ in range(2)]
            ykeys = [[k_ for k_ in S.last_writer if isinstance(k_, tuple) and k_[0] == f"y{br}T"] for br in range(3)]
            pi = 0
            si = 0
            for j in range(8):
                for br in range(3):
                    w_i = pi % 3
                    pi += 1
                    S.op("pool", lambda e, w_i=w_i, j=j, br=br: e.dma_start(out=wm[w_i][:], in_=dr[f"wM{l}"][j * 3 + br]),
                         writes=[("wm", w_i)], dma=f"wm{w_i}")
                    wp = wm[w_i][:, 0:512].rearrange("p (k m) -> p k m", k=4)
                    wgt = wm[w_i][:, 512:1536].rearrange("p (k m) -> p k m", k=8)
                    bcol = V_BG + br * 8 + j
                    for tb in range(NB):
                        pg, pgk = psb("mg", [0, 1, 2, 3])
                        C.mm(pg[:], [(wgt[:, k, :], hT[:, k, TB(tb)]) for k in range(8)], reads=[("wm", w_i)] + C.h_reads(tb), writes=[pgk])
                        pp, ppk = psb("mp", [4, 5, 6, 7])
                        C.mm(pp[:], [(wp[:, k, :], yT[br][:, k, TB(tb)]) for k in range(4)], reads=[("wm", w_i)] + ykeys[br], writes=[ppk])
                        s_i = si % 2
                        si += 1
                        S.op("act", lambda e, pg=pg, s_i=s_i, bcol=bcol: e.activation(out=sg[s_i][:], in_=pg[:], func=AF.Sigmoid,
                                                                                     bias=vec[l][:, bcol:bcol + 1], scale=1.0),
                             reads=[pgk, ("vec", l)], writes=[("sg", s_i)])
                        if br == 0:
                            S.op("dve", lambda e, pp=pp, s_i=s_i, tb=tb: e.tensor_tensor(out=acc[:, tb, :], in0=pp[:], in1=sg[s_i][:], op=ALU.mult),
                                 reads=[ppk, ("sg", s_i)], writes=[("macc", tb)])
                        else:
                            S.op("dve", lambda e, pp=pp, s_i=s_i: e.tensor_tensor(out=tm[s_i][:], in0=pp[:], in1=sg[s_i][:], op=ALU.mult),
                                 reads=[ppk, ("sg", s_i)], writes=[("tm", s_i)])
                            if br == 1:
                                S.op("pool", lambda e, s_i=s_i, tb=tb: e.tensor_tensor(out=acc[:, tb, :], in0=acc[:, tb, :], in1=tm[s_i][:], op=ALU.add),
                                     reads=[("macc", tb), ("tm", s_i)], writes=[("macc", tb)])
                            else:
                                S.op("pool", lambda e, s_i=s_i, tb=tb, j=j: e.tensor_tensor(out=mT[:, j, TB(tb)], in0=acc[:, tb, :], in1=tm[s_i][:], op=ALU.add),
                                     reads=[("macc", tb), ("tm", s_i)], writes=[("mT", j, tb)])
            S.barrier()
        wo = [sbt(ph, "wo", [128, 1024], BF16) for _ in range(2)]
        for j in range(8):
            w_i = j % 2
            S.op("pool", lambda e, w_i=w_i, j=j: e.dma_start(out=wo[w_i][:], in_=dr[f"wO{l}"][j]), writes=[("wo", w_i)], dma=f"wo{w_i}")
            wov = wo[w_i][:].rearrange("p (k m) -> p k m", k=8)
            for tb in range(NB):
                po, pok = psb("all", C.ALLB)
                C.mm(po[:], [(wov[:, k, :], mT[:, k, TB(tb)]) for k in range(8)], reads=[("wo", w_i)], writes=[pok])
                S.op("dve", lambda e, po=po, j=j, tb=tb: e.tensor_tensor(out=xT[:, j, TB(tb)], in0=xT[:, j, TB(tb)], in1=po[:], op=ALU.add),
                     reads=[pok, C.xk(j, tb)], writes=[C.xk(j, tb)])
        S.barrier()
    C.dump(f"xmix{l}", xT[:])


def ffn_groups(C, wname, g0, ng, gbc=None, gkey=None):
    S, dr, hT, psb, TB, xT = C.S, C.dr, C.hT, C.psb, C.TB, C.xT
    wf, abuf, sgf, tgf = C.wf, C.abuf, C.sgf, C.tgf
    GU = [0, 1, 2, 3]
    OB = [4, 5, 6, 7]
    for g in range(ng):
        w_i = C.ffi[0] % 2
        C.ffi[0] += 1
        S.op("pool", lambda e, w_i=w_i, g=g: e.dma_start(out=wf[w_i][:], in_=dr[wname][g0 + g]), writes=[("wf", w_i)], dma=f"wf{w_i}")
        wgv = wf[w_i][:, 0:2048].rearrange("p (c k m) -> p c k m", c=2, k=8)
        wuv = wf[w_i][:, 2048:4096].rearrange("p (c k m) -> p c k m", c=2, k=8)
        wdv = wf[w_i][:, 4096:6144].rearrange("p (c j m) -> p c j m", c=2, j=8)
        a_i = C.ffi[1] % 2
        C.ffi[1] += 1
        ab = abuf[a_i]
        for c in range(2):
            for tb in range(NB):
                pg, pgk = psb("gu", GU)
                C.mm(pg[:], [(wgv[:, c, k, :], hT[:, k, TB(tb)]) for k in range(8)], reads=[("wf", w_i)] + C.h_reads(tb), writes=[pgk])
                pu, puk = psb("gu", GU)
                C.mm(pu[:], [(wuv[:, c, k, :], hT[:, k, TB(tb)]) for k in range(8)], reads=[("wf", w_i)] + C.h_reads(tb), writes=[puk])
                s_i = C.ffi[2] % 2
                C.ffi[2] += 1
                S.op("act", lambda e, pg=pg, s_i=s_i: e.activation(out=sgf[s_i][:], in_=pg[:], func=AF.Silu), reads=[pgk], writes=[("sgf", s_i)])
                if gbc is None:
                    S.op("dve", lambda e, pu=pu, s_i=s_i, ab=ab, c=c, tb=tb: e.tensor_tensor(out=ab[:, c, TB(tb)], in0=pu[:], in1=sgf[s_i][:], op=ALU.mult),
                         reads=[puk, ("sgf", s_i)], writes=[("ab", a_i, c, tb)])
                else:
                    S.op("pool", lambda e, s_i=s_i, tb=tb: e.tensor_tensor(out=tgf[s_i][:], in0=sgf[s_i][:], in1=gbc[:, TB(tb)], op=ALU.mult),
                         reads=[("sgf", s_i), gkey], writes=[("tgf", s_i)])
                    S.op("dve", lambda e, pu=pu, s_i=s_i, ab=ab, c=c, tb=tb: e.tensor_tensor(out=ab[:, c, TB(tb)], in0=pu[:], in1=tgf[s_i][:], op=ALU.mult),
                         reads=[puk, ("tgf", s_i)], writes=[("ab", a_i, c, tb)])
        for tb in range(NB):
            for j in range(8):
                po, pok = psb("ob", OB)
                C.mm(po[:], [(wdv[:, c, j, :], ab[:, c, TB(tb)]) for c in range(2)],
                     reads=[("wf", w_i)] + [("ab", a_i, c, tb) for c in range(2)], writes=[pok])
                S.op("dve", lambda e, po=po, j=j, tb=tb: e.tensor_tensor(out=xT[:, j, TB(tb)], in0=xT[:, j, TB(tb)], in1=po[:], op=ALU.add),
                     reads=[pok, C.xk(j, tb)], writes=[C.xk(j, tb)])


def ffn_alloc(C, ph):
    sbt = C.sbt
    C.wf = [sbt(ph, "wf", [128, 6144], BF16) for _ in range(2)]
    C.abuf = [sbt(ph, "abuf", [128, 2, T], BF16) for _ in range(2)]
    C.sgf = [sbt(ph, "sgf", [128, 512], F32) for _ in range(2)]
    C.tgf = [sbt(ph, "tgf", [128, 512], F32) for _ in range(2)]
    C.ffi = [0, 0, 0]


def ffn_dense(C):
    with ExitStack() as ph:
        ffn_alloc(C, ph)
        ffn_groups(C, "wF0", 0, DFF // 256)
        C.S.barrier()
    C.dump("xffn0", C.xT[:])


def moe(C, l):
    S, dr, sbt, vec, cst, CI, ps, xT = C.S, C.dr, C.sbt, C.vec, C.cst, C.CI, C.ps, C.xT
    with ExitStack() as ph0:
        gate = sbt(ph0, "gate", [128, NT, NEXP], F32)
        with ExitStack() as ph:
            wr = sbt(ph, "wR", [128, 8, 8], F32)
            lg = sbt(ph, "lg", [128, NT, NEXP], F32)
            lg2 = sbt(ph, "lg2", [128, NT, NEXP], F32)
            mk1 = sbt(ph, "mk1", [128, NT, NEXP], F32)
            mk2 = sbt(ph, "mk2", [128, NT, NEXP], F32)
            m1 = sbt(ph, "m1", [128, NT], F32)
            m2 = sbt(ph, "m2", [128, NT], F32)
            w1 = sbt(ph, "w1", [128, NT], F32)
            w2 = sbt(ph, "w2", [128, NT], F32)
            S.op("sp", lambda e: e.dma_start(out=wr[:], in_=dr["wR"][:, :, :]), writes=["wR"], dma="wR")
            plg = ps[7]

            def router(tb, h32):
                def rfn(e):
                    ins = None
                    for jt in range(4):
                        tt = tb * 4 + jt
                        for c in range(8):
                            ins = e.matmul(plg[:, tt * 8:(tt + 1) * 8], h32[:, c, jt * 128:(jt + 1) * 128], wr[:, c, :], start=(c == 0), stop=(c == 7))
                    return ins
                S.op("pe", rfn, reads=[("h32", c) for c in range(8)] + ["wR"], writes=[("plg", tb)])
            C.rmsnorm_to_h(lambda c: vec[l][:, V_GFFN + c:V_GFFN + c + 1], ("vec", l), router=router)
            bc = lambda a: a[:].unsqueeze(2).to_broadcast([128, NT, NEXP])
            S.op("dve", lambda e: e.tensor_copy(out=lg[:].rearrange("p a b -> p (a b)"), in_=plg[:, 0:NT * NEXP]), writes=["lg"])
            S.op("dve", lambda e: e.tensor_reduce(out=m1[:], in_=lg[:], axis=AX.X, op=ALU.max), reads=["lg"], writes=["m1"])
            S.op("dve", lambda e: e.tensor_tensor(out=mk1[:], in0=lg[:], in1=bc(m1), op=ALU.is_equal), reads=["lg", "m1"], writes=["mk1"])
            S.op("dve", lambda e: e.scalar_tensor_tensor(out=lg2[:], in0=mk1[:], scalar=-1e30, in1=lg[:], op0=ALU.mult, op1=ALU.add),
                 reads=["mk1", "lg"], writes=["lg2"])
            S.op("dve", lambda e: e.tensor_reduce(out=m2[:], in_=lg2[:], axis=AX.X, op=ALU.max), reads=["lg2"], writes=["m2"])
            S.op("dve", lambda e: e.tensor_tensor(out=mk2[:], in0=lg2[:], in1=bc(m2), op=ALU.is_equal), reads=["lg2", "m2"], writes=["mk2"])
            S.op("dve", lambda e: e.tensor_tensor(out=w2[:], in0=m2[:], in1=m1[:], op=ALU.subtract), reads=["m1", "m2"], writes=["w2"])
            S.op("act", lambda e: e.activation(out=w2[:], in_=w2[:], func=AF.Exp), reads=["w2"], writes=["w2"])
            S.op("dve", lambda e: e.tensor_tensor(out=w1[:], in0=w2[:], in1=C.ones1[:, 0:NT], op=ALU.add), reads=["w2", "ones1"], writes=["w1"])
            S.op("dve", lambda e: e.reciprocal(out=w1[:], in_=w1[:]), reads=["w1"], writes=["w1"])
            S.op("dve", lambda e: e.tensor_tensor(out=w2[:], in0=w2[:], in1=w1[:], op=ALU.mult), reads=["w1", "w2"], writes=["w2"])
            S.op("dve", lambda e: e.tensor_tensor(out=mk1[:], in0=mk1[:], in1=bc(w1), op=ALU.mult), reads=["mk1", "w1"], writes=["mk1"])
            S.op("dve", lambda e: e.tensor_tensor(out=mk2[:], in0=mk2[:], in1=bc(w2), op=ALU.mult), reads=["mk2", "w2"], writes=["mk2"])
            S.op("dve", lambda e: e.tensor_tensor(out=gate[:], in0=mk1[:], in1=mk2[:], op=ALU.add), reads=["mk1", "mk2"], writes=["gate"])
            S.barrier()
        C.dump("gate", gate[:].rearrange("p a b -> p (a b)"))
        with ExitStack() as ph:
            ffn_alloc(C, ph)
            gbc = [sbt(ph, "gbc", [128, T], F32) for _ in range(2)]
            dg = [sbt(ph, "dg", [128, 128], F32) for _ in range(2)]
            ident = cst[:, CI["ident"], :]
            di = 0
            for ex in range(NEXP):
                gb = gbc[ex % 2]
                for tg in range(4):
                    pb_, pbk = C.psb("ob", [4, 5, 6, 7])
                    for jt in range(4):
                        tt = tg * 4 + jt
                        d_i = di % 2
                        di += 1
                        S.op("dve", lambda e, d_i=d_i, tt=tt, ex=ex: e.scalar_tensor_tensor(out=dg[d_i][:], in0=ident, scalar=gate[:, tt, ex:ex + 1], in1=C.ones1[:], op0=ALU.mult, op1=ALU.mult),
                             reads=["gate", "cst", "ones1"], writes=[("dg", d_i)])
                        S.op("pe", lambda e, d_i=d_i, jt=jt, pb_=pb_: e.matmul(pb_[:, jt * 128:(jt + 1) * 128], C.ones1[:], dg[d_i][:], start=True, stop=True),
                             reads=[("dg", d_i), "ones1"], writes=[pbk])
                    S.op("act", lambda e, pb_=pb_, gb=gb, tg=tg: e.copy(out=gb[:, tg * 512:(tg + 1) * 512], in_=pb_[:]), reads=[pbk], writes=[("gbc", ex % 2)])
                ffn_groups(C, "wE", ex * (DFFE // 256), DFFE // 256, gbc=gb, gkey=("gbc", ex % 2))
            S.barrier()
    C.dump("xffn1", xT[:])


def final_out(C):
    S, sbt, xT, TB, gfin, epsb, onesm, psb, ident = C.S, C.sbt, C.xT, C.TB, C.gfin, C.epsb, C.onesm, C.psb, C.ident
    with ExitStack() as ph:
        sq = [sbt(ph, "sqf", [128, 512], F32) for _ in range(2)]
        rs = sbt(ph, "rsf", [128, T], F32)
        yt = [sbt(ph, "ytf", [128, 128], F32) for _ in range(4)]
        stage = [sbt(ph, "stgo", [128, D], F32) for _ in range(2)]
        for tb in range(NB):
            p, pk = psb("all", C.ALLB)
            for c in range(8):
                s_ = sq[c % 2]
                S.op("act", lambda e, s_=s_, c=c, tb=tb: e.activation(out=s_[:], in_=xT[:, c, TB(tb)], func=AF.Square),
                     reads=[C.xk(c, tb)], writes=[("sqf", c % 2)])
                S.op("pe", lambda e, s_=s_, c=c, p=p: e.matmul(p[:], onesm[:], s_[:], start=(c == 0), stop=(c == 7)),
                     reads=[("sqf", c % 2), "onesm"], writes=[pk])
            S.op("act", lambda e, p=p, tb=tb: e.activation(out=rs[:, TB(tb)], in_=p[:], func=AF.Ln, bias=epsb[:, 0:1], scale=1.0),
                 reads=[pk, "eps"], writes=[("rsf", tb)])
            S.op("act", lambda e, tb=tb: e.activation(out=rs[:, TB(tb)], in_=rs[:, TB(tb)], func=AF.Exp, scale=-0.5), reads=[("rsf", tb)], writes=[("rsf", tb)])
        yi = 0
        for tt in range(NT):
            tb = tt // 4
            st = stage[tt % 2]
            for half in range(2):
                p, pk = psb("all", C.ALLB)
                for j in range(4):
                    c = half * 4 + j
                    y_i = yi % 4
                    yi += 1
                    S.op("dve", lambda e, y_i=y_i, c=c, tt=tt: e.scalar_tensor_tensor(out=yt[y_i][:], in0=xT[:, c, tt * 128:(tt + 1) * 128], scalar=gfin[:, c:c + 1],
                                                                                 in1=rs[:, tt * 128:(tt + 1) * 128], op0=ALU.mult, op1=ALU.mult),
                         reads=[C.xk(c, tb), ("rsf", tb), "gfin"], writes=[("ytf", y_i)])
                    S.op("pe", lambda e, y_i=y_i, j=j, p=p: e.transpose(p[:, j * 128:(j + 1) * 128], yt[y_i][:], ident),
                         reads=[("ytf", y_i), "cst"], writes=[pk])
                S.op("act", lambda e, p=p, st=st, half=half: e.copy(out=st[:, half * 512:(half + 1) * 512], in_=p[:]),
                     reads=[pk], writes=[("stgo", tt % 2, half)])
            S.op("sp", lambda e, st=st, tt=tt: e.dma_start(out=C.y_out[tt * 128:(tt + 1) * 128, :], in_=st[:]),
                 reads=[("stgo", tt % 2, 0), ("stgo", tt % 2, 1)], dma=f"yout{tt % 2}")


_CACHE = {}


def kernel(**inputs):
    x = np.asarray(inputs["x"], dtype=np.float32)
    packed = pack_inputs(inputs)
    shapes = {k: v.shape for k, v in packed.items()}
    shapes["x"] = (T, D)
    nc = build_program(shapes)
    in_maps = []
    for b in range(8):
        m = dict(packed)
        m["x"] = np.ascontiguousarray(x[b])
        in_maps.append(m)
    res = run_bass_kernel_spmd(nc, in_maps, core_ids=list(range(8)))
    return np.stack([np.asarray(res.results[b]["y"], dtype=np.float32) for b in range(8)], 0)
```

```python
import numpy as np
import concourse.bass as bass
import concourse.mybir as mybir
from concourse.bass_utils import run_bass_kernel_spmd
from contextlib import ExitStack

F32 = mybir.dt.float32
BF16 = mybir.dt.bfloat16
AF = mybir.ActivationFunctionType
ALU = mybir.AluOpType
AX = mybir.AxisListType

T = 2048
D = 1024
NT = 16
NB = 4
DEPTH = 2
DFF = 2816
NEXP = 8
DFFE = 3584
EPS = 1e-6
ENGS = ("sp", "act", "dve", "pool", "pe")
SEM_EPOCH = 30000
SKIP = ''
STRICT = True
HLS = (0, 1, 2, 3)
DEBUG_OUT = {}


class Sched:
    def __init__(self, nc, es):
        self.nc = nc
        self.es = es
        self.ops = []
        self.last_writer = {}
        self.readers = {}
        self.dma_sems = {}
        self.eng_ops = {e: [] for e in ENGS}
        self.bar_start = 0

    def op(self, eng, fn, reads=(), writes=(), dma=None, dma_batch=False, extra=()):
        deps = set(extra)
        raw = set(extra)
        for k in reads:
            w = self.last_writer.get(k)
            if w is not None:
                deps.add(w)
                raw.add(w)
            if isinstance(k, tuple) and k and k[0] in ("ps", "plg"):
                for r in self.readers.get(k, ()):
                    if self.ops[r]["eng"] != eng:
                        deps.add(r)
                        raw.add(r)
        for k in writes:
            w = self.last_writer.get(k)
            if w is not None:
                deps.add(w)
            for r in self.readers.get(k, ()):
                deps.add(r)
        idx = len(self.ops)
        o = dict(eng=eng, fn=fn, deps=deps, dma=dma, needed=False, idx=idx, raw=raw)
        if dma is not None:
            d = self.dma_sems.setdefault(dma, dict(total=0, batch=dma_batch))
            d["total"] += 16
            o["dma_val"] = d["total"]
        self.ops.append(o)
        self.eng_ops[eng].append(idx)
        for k in reads:
            self.readers.setdefault(k, []).append(idx)
        for k in writes:
            self.last_writer[k] = idx
            self.readers[k] = []
        return idx

    def barrier(self):
        last = [self.eng_ops[e][-1] for e in ENGS if self.eng_ops[e] and not self.ops[self.eng_ops[e][-1]].get("bar")]
        dmas = [i for i in range(self.bar_start, len(self.ops)) if self.ops[i]["dma"] is not None]
        ex = set(last + dmas)
        for e in ENGS:
            i_ = self.op(e, lambda eng: eng.nop(), extra=ex)
            self.ops[i_]["bar"] = True
        self.bar_start = len(self.ops)
        self.last_writer = {}
        self.readers = {}

    def emit(self, final_waits=()):
        nc = self.nc
        ops = self.ops
        for o in ops:
            nd = set()
            for d in o["deps"]:
                p = ops[d]
                if p["dma"] is None and p["eng"] == o["eng"] and o["dma"] is None:
                    if o["eng"] == "pe":
                        continue
                    if d not in o["raw"] and not STRICT:
                        continue
                nd.add(d)
            o["deps"] = nd
            for d in nd:
                ops[d]["needed"] = True
        cnt = {e: 0 for e in ENGS}
        for o in ops:
            if o["dma"] is None and o["needed"]:
                cnt[o["eng"]] += 1
                o["sig"] = cnt[o["eng"]]
        self.cnt = cnt
        eng_sems = {}
        for e in ENGS:
            n_ep = max(cnt[e] - 1, 0) // SEM_EPOCH + 1
            eng_sems[e] = [self.es.enter_context(nc.semaphore(f"s_{e}{i}")) for i in range(n_ep)]
        dsem = {}
        for name in self.dma_sems:
            dsem[name] = self.es.enter_context(nc.semaphore(f"d_{name}"))

        def event(o):
            if o["dma"] is not None:
                d = self.dma_sems[o["dma"]]
                v = d["total"] if d["batch"] else o["dma_val"]
                return (("d", o["dma"]), dsem[o["dma"]], v)
            s = o["sig"]
            ep = (s - 1) // SEM_EPOCH
            return (("e", o["eng"], ep), eng_sems[o["eng"]][ep], s - ep * SEM_EPOCH)

        block = self.es.enter_context(nc.Block())

        def run_engine(ename):
            def body(eng):
                wm = {}
                for idx in self.eng_ops[ename]:
                    o = ops[idx]
                    need = {}
                    for d in o["deps"]:
                        key, sem, val = event(ops[d])
                        if val > need.get(key, (None, 0))[1]:
                            need[key] = (sem, val)
                    for key, (sem, val) in need.items():
                        if key[0] == "e":
                            if any(k[0] == "e" and k[1] == key[1] and k[2] > key[2] for k in wm):
                                continue
                        if wm.get(key, 0) >= val:
                            continue
                        eng.wait_ge(sem, val)
                        wm[key] = val
                    ins = o["fn"](eng)
                    if o["dma"] is not None:
                        ins.then_inc(dsem[o["dma"]], 16)
                    elif o["needed"]:
                        key, sem, val = event(o)
                        ins.then_inc(sem, 1)
                for (e2, name) in final_waits:
                    if e2 == ename and name in dsem:
                        eng.wait_ge(dsem[name], self.dma_sems[name]["total"])
            return body

        block.sync(run_engine("sp"))
        block.scalar(run_engine("act"))
        block.vector(run_engine("dve"))
        block.gpsimd(run_engine("pool"))
        block.tensor(run_engine("pe"))


IN_SIZES = (512, 128, 128, 256, 256, 32, 512, 512, 512, 512, 16, 16, 3072)
OFF = np.concatenate([[0], np.cumsum(IN_SIZES)]).astype(int)
(O_AQ, O_AK, O_AV, O_BCQ, O_BCKV, O_BKR, O_CQ, O_CK, O_CV, O_CG, O_CAF, O_CAB, O_GATE) = OFF[:13]


def lhsT(W, cols):
    W = np.asarray(W)
    kc = W.shape[0] // 128
    sub = W[:, cols] if cols is not None else W
    return np.ascontiguousarray(sub.reshape(kc, 128, sub.shape[1]).transpose(1, 0, 2))


def partner(n):
    h = n // 2
    return np.concatenate([np.arange(h, n), np.arange(0, h)])


def rope_tables():
    GRID_W = 64
    rows = T // GRID_W
    row = np.repeat(np.arange(rows, dtype=np.float32), GRID_W)
    col = np.tile(np.arange(GRID_W, dtype=np.float32), rows)

    def tabs(rot_dim):
        nf = rot_dim // 4
        inv = (10000.0 ** (-np.arange(nf, dtype=np.float32) / nf)).astype(np.float32)
        ar = row[:, None] * inv
        ac = col[:, None] * inv
        half = rot_dim // 2
        cos = np.concatenate([np.cos(ar), np.cos(ar), np.cos(ac), np.cos(ac)], axis=1)
        sin = np.concatenate([-np.sin(ar), np.sin(ar), -np.sin(ac), np.sin(ac)], axis=1)
        return cos.T.astype(np.float32), sin.T.astype(np.float32)
    ca, sa = tabs(64)
    cb, sb = tabs(32)
    cosA = np.concatenate([ca, ca], 0)
    sinA = np.concatenate([sa, sa], 0)
    cosB = np.concatenate([np.ones((64, T), np.float32), cb, np.zeros((32, T), np.float32)], 0)
    sinB = np.concatenate([np.zeros((64, T), np.float32), sb, np.zeros((32, T), np.float32)], 0)
    return (np.ascontiguousarray(cosA), np.ascontiguousarray(sinA),
            np.ascontiguousarray(cosB), np.ascontiguousarray(sinB))


def rope_partner_cols(n_rot):
    h = n_rot // 2
    p = partner(h)
    return np.concatenate([p, h + p])


def consts_pack():
    c = {}
    c["ident"] = np.eye(128, dtype=np.float32)
    blk = np.zeros((128, 128), np.float32)
    blk[:64, :64] = 1.0 / 64
    blk[64:, 64:] = 1.0 / 64
    c["blk64"] = blk
    pa = rope_partner_cols(64)
    full = np.concatenate([pa, 64 + pa])
    P = np.zeros((128, 128), np.float32)
    P[full, np.arange(128)] = 1.0
    c["permA"] = P
    s = np.arange(128)
    same = (s[:, None] // 64) == (s[None, :] // 64)
    c["trif"] = (same & (s[:, None] <= s[None, :])).astype(np.float32)
    c["trib"] = (same & (s[:, None] >= s[None, :])).astype(np.float32)
    c["uf"] = (same & (s[:, None] > s[None, :])).astype(np.float32)
    c["ub"] = (same & (s[:, None] < s[None, :])).astype(np.float32)
    names = ["ident", "blk64", "permA", "trif", "trib", "uf", "ub"]
    return np.ascontiguousarray(np.stack([c[n] for n in names], 1)), names


CONST_NAMES = ["ident", "blk64", "permA", "trif", "trib", "uf", "ub"]
V_GMIX, V_GFFN, V_GAQ, V_GAK, V_GBQ, V_GBKV, V_GCO, V_BG = 0, 8, 16, 17, 18, 20, 22, 23
NVEC = 23 + 24


def pack_inputs(inp):
    out = {}
    cst, _ = consts_pack()
    out["cst"] = cst
    cosA, sinA, cosB, sinB = rope_tables()
    out["ropeA"] = np.ascontiguousarray(np.stack([cosA, sinA], 1))
    out["ropeB"] = np.ascontiguousarray(np.stack([cosB, sinB], 1))
    pk96 = rope_partner_cols(32)
    for l in range(DEPTH):
        w_in = np.asarray(inp["w_in"][l])
        vec = np.zeros((128, NVEC), np.float32)
        vec[:, V_GMIX:V_GMIX + 8] = np.asarray(inp["g_mix"][l]).reshape(8, 128).T
        vec[:, V_GFFN:V_GFFN + 8] = np.asarray(inp["g_ffn"][l]).reshape(8, 128).T
        vec[:, V_GAQ] = np.tile(np.asarray(inp["g_a_q"][l]), 2)
        vec[:, V_GAK] = np.tile(np.asarray(inp["g_a_k"][l]), 2)
        vec[:, V_GBQ:V_GBQ + 2] = np.asarray(inp["g_b_q"][l]).reshape(2, 128).T
        vec[:, V_GBKV:V_GBKV + 2] = np.asarray(inp["g_b_kv"][l]).reshape(2, 128).T
        vec[:, V_GCO] = np.asarray(inp["g_c_out"][l])
        vec[:, V_BG:V_BG + 24] = np.asarray(inp["b_gate"][l]).reshape(24, 128).T
        out[f"vec{l}"] = vec
        wa = []
        for g in range(2):
            kg = lhsT(w_in, O_AK + np.arange(g * 64, g * 64 + 64))
            zz = np.zeros_like(kg)
            wa.append(np.concatenate([lhsT(w_in, O_AQ + np.arange(2 * g * 128, (2 * g + 2) * 128)), kg, zz, zz, kg,
                                      lhsT(w_in, O_AV + np.arange(g * 64, g * 64 + 64))], axis=2))
        out[f"wA{l}"] = np.ascontiguousarray(np.stack(wa, 0))
        z64 = np.zeros((128, 8, 64), np.float32)
        kr = lhsT(w_in, O_BKR + np.arange(32))
        krp = lhsT(w_in, O_BKR + pk96)
        out[f"wB1{l}"] = np.ascontiguousarray(np.concatenate(
            [lhsT(w_in, O_BCQ + np.arange(256)), lhsT(w_in, O_BCKV + np.arange(256)), z64, kr, z64, krp], axis=2))
        wq = np.asarray(inp["w_b_q_up"][l])
        qcols, qpcols = [], []
        for h in range(8):
            base = h * 96
            qcols.append(base + np.arange(96))
            qpcols.append(np.concatenate([base + np.arange(64), base + 64 + pk96]))
        wq_l = np.stack([np.stack([lhsT(wq, qcols[h]), lhsT(wq, qpcols[h])], 2) for h in range(8)], 2)
        wkv = np.asarray(inp["w_b_kv_up"][l])
        wk_l = np.stack([lhsT(wkv, h * 128 + np.arange(64)) for h in range(8)], 2)
        wv_l = np.stack([lhsT(wkv, h * 128 + 64 + np.arange(64)) for h in range(8)], 2)
        out[f"wB2{l}"] = np.ascontiguousarray(np.concatenate(
            [wq_l.reshape(128, -1), wk_l.reshape(128, -1), wv_l.reshape(128, -1)], axis=1))
        wc = []
        for h in range(4):
            sl = np.arange(h * 128, (h + 1) * 128)
            a = np.stack([lhsT(w_in, O_CQ + sl), lhsT(w_in, O_CK + sl), lhsT(w_in, O_CG + sl)], 2)
            b = np.concatenate([lhsT(w_in, O_CK + sl), lhsT(w_in, O_CV + sl)], 2)
            wc.append(np.concatenate([a.reshape(128, -1), b.reshape(128, -1)], 1))
        out[f"wC{l}"] = np.ascontiguousarray(np.stack(wc, 0))
        z16 = np.zeros((128, 8, 16), np.float32)
        out[f"wCg{l}"] = np.ascontiguousarray(np.concatenate(
            [lhsT(w_in, O_CAF + np.arange(16)), z16, lhsT(w_in, O_CAB + np.arange(16)), z16], 2))
        up = np.zeros((128, 4, 2, 128), np.float32)
        waf = np.asarray(inp["w_c_af_up"][l]); wab = np.asarray(inp["w_c_ab_up"][l])
        baf = np.asarray(inp["b_c_af"][l]); bab = np.asarray(inp["b_c_ab"][l])
        for h in range(4):
            sl = slice(h * 128, (h + 1) * 128)
            up[0:16, h, 0] = waf[:, sl]
            up[48, h, 0] = baf[sl]
            up[32:48, h, 1] = wab[:, sl]
            up[48, h, 1] = bab[sl]
        out[f"wCup{l}"] = np.ascontiguousarray(up.reshape(128, -1))
        wps = [np.asarray(inp[n][l]) for n in ("w_pa", "w_pb", "w_pc")]
        wm = []
        for j in range(8):
            cj = np.arange(j * 128, (j + 1) * 128)
            for br in range(3):
                a = lhsT(wps[br], cj).reshape(128, -1)
                gts = lhsT(w_in, O_GATE + br * 1024 + cj).reshape(128, -1)
                wm.append(np.concatenate([a, gts], 1))
        out[f"wM{l}"] = np.ascontiguousarray(np.stack(wm, 0))
        wo = np.asarray(inp["w_out"][l])
        out[f"wO{l}"] = np.ascontiguousarray(np.stack([lhsT(wo, np.arange(j * 128, (j + 1) * 128)).reshape(128, -1) for j in range(8)], 0))

    def ffn_pack(wg, wu, wd):
        dff = wg.shape[1]
        ng = dff // 256
        blocks = []
        for g in range(ng):
            parts = []
            for w in (wg, wu):
                parts.append(np.stack([lhsT(w, np.arange((2 * g + c) * 128, (2 * g + c + 1) * 128)) for c in range(2)], 1).reshape(128, -1))
            dd = np.stack([np.asarray(wd)[(2 * g + c) * 128:(2 * g + c + 1) * 128, :] for c in range(2)], 1).reshape(128, -1)
            parts.append(dd)
            blocks.append(np.concatenate(parts, 1))
        return np.ascontiguousarray(np.stack(blocks, 0))
    out["wF0"] = ffn_pack(np.asarray(inp["w_ff_gate"][0]), np.asarray(inp["w_ff_up"][0]), np.asarray(inp["w_ff_down"][0]))
    we = [ffn_pack(np.asarray(inp["w_e_gate"][0][e]), np.asarray(inp["w_e_up"][0][e]), np.asarray(inp["w_e_down"][0][e])) for e in range(NEXP)]
    out["wE"] = np.ascontiguousarray(np.concatenate(we, 0))
    out["wR"] = lhsT(np.asarray(inp["w_router"][0]), None)
    gf = np.asarray(inp["g_final"]).reshape(8, 128).T
    out["gfin"] = np.ascontiguousarray(gf)
    return out


class Ctx:
    pass


def build_program(shapes, stop_after=None, dbg=None):
    nc = bass.Bass("TRN2", target_bir_lowering=False)
    dr = {}
    for name, shp in shapes.items():
        dr[name] = nc.dram_tensor(name, list(shp), F32, kind="ExternalInput").ap()
    y_out = nc.dram_tensor("y", [T, D], F32, kind="ExternalOutput").ap()
    dbg_out = {}
    if dbg:
        for name, shp in dbg.items():
            dbg_out[name] = nc.dram_tensor("dbg_" + name, list(shp), F32, kind="ExternalOutput").ap()
    es = ExitStack()
    with es:
        S = Sched(nc, es)
        uid = [0]

        def sbt(stack, name, shape, dt):
            uid[0] += 1
            return stack.enter_context(nc.sbuf_tensor(f"{name}_{uid[0]}", list(shape), dt))

        P = lambda name, shape, dt=F32: sbt(es, name, shape, dt)
        ps = [es.enter_context(nc.psum_tensor(f"ps{i}", [128, 512], F32)) for i in range(8)]
        rr = {}

        def psb(pool, banks):
            i = rr.get(pool, 0)
            rr[pool] = i + 1
            b = banks[i % len(banks)]
            return ps[b], ("ps", b)

        ALLB = [0, 1, 2, 3, 4, 5, 6, 7]
        PJ = [2, 3, 4, 5, 6, 7]

        xT = P("xT", [128, 8, T])
        hT = P("hT", [128, 8, T], BF16)
        cst = P("cst", [128, 7, 128])
        cstb = P("cstb", [128, 7, 128], BF16)
        vec = [P(f"vec{l}", [128, NVEC]) for l in range(DEPTH)]
        gfin = P("gfin", [128, 8])
        epsb = P("epsb", [128, 1])
        lnsc = P("lnsc", [128, 1])
        onesm = P("onesm", [128, 128])
        ones256 = P("ones256", [128, 128], BF16)
        ones128 = P("ones128", [128, 128])
        ones1 = P("ones1", [128, 128])
        CI = {n: i for i, n in enumerate(CONST_NAMES)}
        ident = cst[:, CI["ident"], :]

        def cload(eng, out, in_, key, sem="const"):
            S.op(eng, lambda e: e.dma_start(out=out, in_=in_), writes=[key], dma=sem, dma_batch=True)

        cload("sp", cst[:], dr["cst"][:, :, :], "cst")
        for l in range(DEPTH):
            cload("sp", vec[l][:], dr[f"vec{l}"][:, :], ("vec", l))
        cload("sp", gfin[:], dr["gfin"][:, :], "gfin")
        S.op("dve", lambda e: e.tensor_copy(out=cstb[:], in_=cst[:]), reads=["cst"], writes=["cstb"])
        S.op("dve", lambda e: e.memset(epsb[:], EPS), writes=["eps"])
        S.op("dve", lambda e: e.memset(lnsc[:], float(np.log(128.0 ** -0.5))), writes=["lnsc"])
        S.op("dve", lambda e: e.memset(onesm[:], 1.0 / 1024), writes=["onesm"])
        S.op("dve", lambda e: e.memset(ones256[:], 1.0 / 256), writes=["ones256"])
        S.op("dve", lambda e: e.memset(ones128[:], 1.0 / 128), writes=["ones128"])
        S.op("dve", lambda e: e.memset(ones1[:], 1.0), writes=["ones1"])

        def mm(out, pairs, reads, writes):
            def fn(e):
                n = len(pairs)
                ins = None
                for i, (l_, r_) in enumerate(pairs):
                    ins = e.matmul(out, l_, r_, start=(i == 0), stop=(i == n - 1))
                return ins
            return S.op("pe", fn, reads=reads, writes=writes)

        def xk(c, tb):
            return ("xT", c, tb)

        def hk(tb):
            return ("hT", tb)

        def TB(tb):
            return slice(tb * 512, (tb + 1) * 512)

        def dump(name, src_ap, key_reads=None):
            if name in dbg_out:
                S.barrier()
                if src_ap.dtype != F32:
                    with ExitStack() as ph2:
                        tmp = sbt(ph2, "dbgtmp", list(src_ap.shape), F32)
                        S.op("dve", lambda e: e.tensor_copy(out=tmp[:], in_=src_ap), writes=["dbgtmp"])
                        S.op("sp", lambda e: e.dma_start(out=dbg_out[name], in_=tmp[:]), reads=["dbgtmp"], dma="dbg")
                        S.barrier()
                else:
                    S.op("sp", lambda e: e.dma_start(out=dbg_out[name], in_=src_ap), dma="dbg")
                    S.barrier()

        with ExitStack() as ph:
            stage = [sbt(ph, "stg", [128, D], F32) for _ in range(2)]
            for tt in range(NT):
                st = stage[tt % 2]
                S.op("sp", lambda e, st=st, tt=tt: e.dma_start(out=st[:], in_=dr["x"][tt * 128:(tt + 1) * 128, :]),
                     writes=[("stg", tt % 2)], dma=f"xin{tt % 2}")
                for half in range(2):
                    p, pk = psb("all", ALLB)

                    def tr(e, st=st, half=half, p=p):
                        ins = None
                        for j in range(4):
                            c = half * 4 + j
                            ins = e.transpose(p[:, j * 128:(j + 1) * 128], st[:, c * 128:(c + 1) * 128], ident)
                        return ins
                    S.op("pe", tr, reads=[("stg", tt % 2), "cst"], writes=[pk])
                    S.op("dve" if half == 0 else "act",
                         (lambda e, half=half, p=p, tt=tt: e.tensor_copy(out=xT[:, half * 4:(half + 1) * 4, tt * 128:(tt + 1) * 128],
                                                                        in_=p[:].rearrange("p (j t) -> p j t", j=4))) if half == 0 else
                         (lambda e, half=half, p=p, tt=tt: e.copy(out=xT[:, half * 4:(half + 1) * 4, tt * 128:(tt + 1) * 128],
                                                                  in_=p[:].rearrange("p (j t) -> p j t", j=4))),
                         reads=[pk], writes=[xk(c, tt // 4) for c in range(half * 4, half * 4 + 4)])
            S.barrier()

        def rmsnorm_to_h(gcol_ap_fn, gkey, router=None):
            with ExitStack() as ph:
                sq = [sbt(ph, "sq", [128, 512], F32) for _ in range(2)]
                rs = [sbt(ph, "rs", [128, 512], F32) for _ in range(2)]
                if router is not None:
                    h32 = sbt(ph, "h32", [128, 8, 512], F32)
                for tb in range(NB):
                    p, pk = psb("n7", [0, 1, 2, 3, 4, 5, 6])
                    for c in range(8):
                        s_ = sq[c % 2]
                        S.op("act", lambda e, s_=s_, c=c, tb=tb: e.activation(out=s_[:], in_=xT[:, c, TB(tb)], func=AF.Square),
                             reads=[xk(c, tb)], writes=[("sq", c % 2)])
                        S.op("pe", lambda e, s_=s_, c=c, p=p: e.matmul(p[:], onesm[:], s_[:], start=(c == 0), stop=(c == 7)),
                             reads=[("sq", c % 2), "onesm"], writes=[pk])
                    r_ = rs[tb % 2]
                    S.op("act", lambda e, p=p, r_=r_: e.activation(out=r_[:], in_=p[:], func=AF.Ln, bias=epsb[:, 0:1], scale=1.0),
                         reads=[pk, "eps"], writes=[("rs", tb % 2)])
                    S.op("act", lambda e, r_=r_: e.activation(out=r_[:], in_=r_[:], func=AF.Exp, scale=-0.5), reads=[("rs", tb % 2)], writes=[("rs", tb % 2)])
                    for c in range(8):
                        S.op("dve",
                             lambda e, c=c, tb=tb, r_=r_: e.scalar_tensor_tensor(out=hT[:, c, TB(tb)], in0=xT[:, c, TB(tb)], scalar=gcol_ap_fn(c),
                                                                               in1=r_[:], op0=ALU.mult, op1=ALU.mult),
                             reads=[xk(c, tb), ("rs", tb % 2), gkey], writes=[("hT", c, tb)])
                    if router is not None:
                        for c in range(8):
                            S.op("dve", lambda e, c=c, tb=tb, r_=r_: e.scalar_tensor_tensor(out=h32[:, c, :], in0=xT[:, c, TB(tb)], scalar=gcol_ap_fn(c),
                                                                                        in1=r_[:], op0=ALU.mult, op1=ALU.mult),
                                 reads=[xk(c, tb), ("rs", tb % 2), gkey], writes=[("h32", c)])
                        router(tb, h32)
                S.barrier()

        def h_reads(tb):
            return [("hT", c, tb) for c in range(8)]

        def h_reads_all():
            return [("hT", c, tb) for c in range(8) for tb in range(NB)]

        C = Ctx()
        C.__dict__.update(locals())
        for l in range(DEPTH):
            g0 = V_GMIX
            rmsnorm_to_h(lambda c, l=l: vec[l][:, V_GMIX + c:V_GMIX + c + 1], ("vec", l))
            dump(f"h{l}", hT[:], None)
            if stop_after == f"norm{l}":
                break
            stop = False
            with ExitStack() as mixph:
                C.yT = [sbt(mixph, f"y{b}T", [128, 4, T], BF16) for b in range(3)]
                for nm, fn in (("B", mixer_B), ("A", mixer_A), ("C", mixer_C), ("M", merge_out)):
                    if nm in SKIP:
                        continue
                    fn(C, l)
                    if stop_after == f"{nm}{l}":
                        stop = True
                        break
                if stop:
                    S.barrier()
            if stop:
                break
            if l == 0:
                rmsnorm_to_h(lambda c, l=l: vec[l][:, V_GFFN + c:V_GFFN + c + 1], ("vec", l))
                ffn_dense(C)
            else:
                moe(C, l)
            if stop_after == f"F{l}":
                break
        final_out(C)
        waits = [("sp", n) for n in S.dma_sems if n.startswith("yout") or n == "dbg"]
        S.emit(final_waits=waits)
    return nc


def attn_core(C, k_ap, q_ap, v_ap, par, out_ap, scale, kkeys, qkeys, vkeys, okey_fn, PT, rec):
    S, ps, psb = C.S, C.ps, C.psb
    vr = slice(par * 64, par * 64 + 64)
    sr = slice((1 - par) * 64, (1 - par) * 64 + 64)
    for qb in range(NB):
        acc, acck = psb("acc", [0, 1])
        pend = []

        def issue_pv(item, last):
            kt, pt_i = item
            S.op("pe", lambda e, kt=kt, pt_i=pt_i, acc=acc: e.matmul(acc[:], v_ap(kt), PT[pt_i][:], start=(kt == 0), stop=(kt == NT - 1)),
                 reads=[("PT", pt_i)] + vkeys, writes=[acck])
        for kt in range(NT):
            sp_, spk = psb("s", [2, 3, 4])
            S.op("pe", lambda e, kt=kt, qb=qb, sp_=sp_: e.matmul(sp_[:], k_ap(kt), q_ap(qb), start=True, stop=True),
                 reads=kkeys + qkeys, writes=[spk])
            pt_i = C.pt_rr[0] % len(PT)
            C.pt_rr[0] += 1
            S.op("act", lambda e, sp_=sp_, pt_i=pt_i: e.activation(out=PT[pt_i][:], in_=sp_[:], func=AF.Exp, scale=scale),
                 reads=[spk], writes=[("PT", pt_i)])
            pend.append((kt, pt_i))
            if len(pend) > 1:
                issue_pv(pend.pop(0), False)
        while pend:
            issue_pv(pend.pop(0), True)
        r_i = C.pt_rr[1] % len(rec)
        C.pt_rr[1] += 1
        S.op("dve", lambda e, acc=acc, r_i=r_i: e.reciprocal(out=rec[r_i][vr, :], in_=acc[sr, :]),
             reads=[acck], writes=[("rec", r_i)])
        S.op("dve", lambda e, acc=acc, r_i=r_i, qb=qb: e.tensor_tensor(out=out_ap(qb), in0=acc[vr, :], in1=rec[r_i][vr, :], op=ALU.mult),
             reads=[acck, ("rec", r_i)], writes=[okey_fn(qb)])


def mixer_A(C, l):
    S, nc, dr, hT, vec, cstb, psb, sbt = C.S, C.nc, C.dr, C.hT, C.vec, C.cstb, C.psb, C.sbt
    CI, TB, epsb = C.CI, C.TB, C.epsb
    yaT = C.yT[0]
    PJ = C.PJ
    with ExitStack() as ph:
        wA = sbt(ph, "wA", [128, 8, 576], BF16)
        qT = sbt(ph, "qTa", [128, 2, T], BF16)
        kT = sbt(ph, "kTa", [128, 2, T], BF16)
        Va = [sbt(ph, "Va", [128, NT, 128], BF16) for _ in range(2)]
        PT = [sbt(ph, "PT", [128, 512], BF16) for _ in range(3)]
        rec = [sbt(ph, "rec", [128, 512], F32) for _ in range(1)]
        rope = [sbt(ph, "ropeA", [128, 2, 512], F32) for _ in range(1)]
        sqb = [sbt(ph, "sqb", [128, 512], BF16) for _ in range(1)]
        ub = [sbt(ph, "ub", [128, 512], BF16) for _ in range(1)]
        rsb = [sbt(ph, "rsb", [128, 512], F32) for _ in range(1)]
        t1 = [sbt(ph, "t1", [128, 512], F32) for _ in range(1)]
        t2 = [sbt(ph, "t2", [128, 512], F32) for _ in range(1)]
        onesA = sbt(ph, "onesA", [128, 512], F32)
        S.op("dve", lambda e: e.memset(onesA[:], 1.0), writes=["onesA"])
        C.pt_rr = [0, 0]
        blk = cstb[:, CI["blk64"], :]
        perm = cstb[:, CI["permA"], :]
        for g in range(2):
            S.op("pool", lambda e, g=g: e.dma_start(out=wA[:], in_=dr[f"wA{l}"][g]), writes=["wA"], dma="wA")
            S.op("pool", lambda e: e.memset(Va[0][:], 1.0), writes=["Va"])
            S.op("pool", lambda e: e.memset(Va[1][:], 1.0), writes=["Va1"])
            bi = 0
            for tb in range(NB):
                rp = rope[0]
                S.op("sp", lambda e, rp=rp, tb=tb: e.dma_start(out=rp[:], in_=dr["ropeA"][:, :, TB(tb)]),
                     writes=[("ropeA", 0)], dma="ropeA0")
                for blkid in range(4):
                    col0 = blkid * 128
                    gcol = vec[l][:, V_GAQ:V_GAQ + 1] if blkid < 2 else vec[l][:, V_GAK:V_GAK + 1]
                    i = 0
                    p1, p1k = psb("pj", PJ)
                    C.mm(p1[:], [(wA[:, k, col0:col0 + 128], hT[:, k, TB(tb)]) for k in range(8)],
                         reads=["wA"] + C.h_reads(tb), writes=[p1k])
                    S.op("act", lambda e, p1=p1, i=i: e.activation(out=sqb[i][:], in_=p1[:], func=AF.Square),
                         reads=[p1k], writes=[("sqb", i)])
                    S.op("dve", lambda e, p1=p1, i=i, gcol=gcol: e.scalar_tensor_tensor(out=ub[i][:], in0=p1[:], scalar=gcol, in1=onesA[:], op0=ALU.mult, op1=ALU.mult),
                         reads=[p1k, ("vec", l), "onesA"], writes=[("ub", i)])
                    p2, p2k = psb("pj", PJ)
                    C.mm(p2[:], [(blk, sqb[i][:])], reads=["cstb", ("sqb", i)], writes=[p2k])
                    p3, p3k = psb("pj", PJ)
                    C.mm(p3[:], [(perm, ub[i][:])], reads=["cstb", ("ub", i)], writes=[p3k])
                    S.op("act", lambda e, p2=p2, i=i: e.activation(out=rsb[i][:], in_=p2[:], func=AF.Ln, bias=epsb[:, 0:1], scale=1.0),
                         reads=[p2k, "eps"], writes=[("rsb", i)])
                    S.op("act", lambda e, i=i: e.activation(out=rsb[i][:], in_=rsb[i][:], func=AF.Exp, scale=-0.5), reads=[("rsb", i)], writes=[("rsb", i)])
                    S.op("dve", lambda e, i=i, rp=rp: e.tensor_tensor(out=t1[i][:], in0=ub[i][:], in1=rp[:, 0, :], op=ALU.mult),
                         reads=[("ub", i), ("ropeA", 0)], writes=[("t1", i)])
                    S.op("dve", lambda e, i=i, rp=rp, p3=p3: e.tensor_tensor(out=t2[i][:], in0=p3[:], in1=rp[:, 1, :], op=ALU.mult),
                         reads=[p3k, ("ropeA", 0)], writes=[("t2", i)])
                    S.op("pool", lambda e, i=i: e.tensor_tensor(out=t1[i][:], in0=t1[i][:], in1=t2[i][:], op=ALU.add),
                         reads=[("t1", i), ("t2", i)], writes=[("t1", i)])
                    dst = qT[:, blkid, TB(tb)] if blkid < 2 else kT[:, blkid - 2, TB(tb)]
                    dkey = ("qTa", blkid, tb) if blkid < 2 else ("kTa", blkid - 2, tb)
                    S.op("dve", lambda e, i=i, dst=dst: e.tensor_tensor(out=dst, in0=t1[i][:], in1=rsb[i][:], op=ALU.mult),
                         reads=[("t1", i), ("rsb", i)], writes=[dkey])
            for tg in range(4):
                pv, pvk = psb("pj", PJ)

                def vfn(e, tg=tg, pv=pv):
                    ins = None
                    for j in range(4):
                        tt = tg * 4 + j
                        for k in range(8):
                            ins = e.matmul(pv[:, j * 64:(j + 1) * 64], hT[:, k, tt * 128:(tt + 1) * 128], wA[:, k, 512:576],
                                           start=(k == 0), stop=(k == 7))
                    return ins
                S.op("pe", vfn, reads=["wA"] + C.h_reads(tg), writes=[pvk])
                S.op("dve", lambda e, tg=tg, pv=pv: e.tensor_copy(out=Va[0][:, tg * 4:(tg + 1) * 4, 0:64],
                                                                  in_=pv[:, 0:256].rearrange("p (j d) -> p j d", j=4)),
                     reads=[pvk, "Va"], writes=[("Vav", tg)])
                S.op("act", lambda e, tg=tg, pv=pv: e.copy(out=Va[1][:, tg * 4:(tg + 1) * 4, 64:128],
                                                          in_=pv[:, 0:256].rearrange("p (j d) -> p j d", j=4)),
                     reads=[pvk, "Va1"], writes=[("Vav1", tg)])
            vkeys = [("Vav", tg) for tg in range(4)] + ["Va", "Va1"] + [("Vav1", tg) for tg in range(4)]
            for hl in HLS:
                cl, par = hl // 2, hl % 2
                pr = slice(par * 64, par * 64 + 64)
                chunk = 2 * g + cl
                attn_core(C,
                          k_ap=lambda kt, par=par: kT[:, par, kt * 128:(kt + 1) * 128],
                          q_ap=lambda qb, cl=cl: qT[:, cl, TB(qb)],
                          v_ap=(lambda kt: Va[0][:, kt, :]) if par == 0 else (lambda kt: Va[1][:, kt, :]),
                          par=par,
                          out_ap=lambda qb, pr=pr, chunk=chunk: yaT[pr, chunk, TB(qb)],
                          scale=64.0 ** -0.5,
                          kkeys=[("kTa", par, tb) for tb in range(NB)], qkeys=[("qTa", cl, tb) for tb in range(NB)], vkeys=vkeys,
                          okey_fn=lambda qb, chunk=chunk, par=par: ("y0T", chunk, par, qb), PT=PT, rec=rec)
        S.barrier()
    C.dump(f"ya{l}", yaT[:])


def mixer_B(C, l):
    S, nc, dr, hT, vec, cstb, psb, sbt = C.S, C.nc, C.dr, C.hT, C.vec, C.cstb, C.psb, C.sbt
    CI, TB, epsb, ones256 = C.CI, C.TB, C.epsb, C.ones256
    ybT = C.yT[1]
    PJ = C.PJ
    with ExitStack() as ph:
        cqT = sbt(ph, "cqT", [128, 2, T], BF16)
        ckvT = sbt(ph, "ckvT", [128, 2, T], BF16)
        kh = sbt(ph, "kh", [128, T], BF16)
        rope = [sbt(ph, "ropeB", [128, 2, 512], F32) for _ in range(2)]
        t1 = [sbt(ph, "t1b", [128, 512], F32) for _ in range(1)]
        t2 = [sbt(ph, "t2b", [128, 512], F32) for _ in range(1)]
        with ExitStack() as ph1:
            w1 = sbt(ph1, "wB1", [128, 8, 704], BF16)
            sqb = [sbt(ph1, "sqbB", [128, 512], BF16) for _ in range(4)]
            rsb = [sbt(ph1, "rsbB", [128, 512], F32) for _ in range(2)]
            S.op("pool", lambda e: e.dma_start(out=w1[:], in_=dr[f"wB1{l}"][:, :, :]), writes=["wB1"], dma="wB1")
            ni = 0
            for tb in range(NB):
                rp = rope[0]
                S.op("sp", lambda e, rp=rp, tb=tb: e.dma_start(out=rp[:], in_=dr["ropeB"][:, :, TB(tb)]),
                     writes=[("ropeB", 0)], dma="ropeB0")
                for which in range(2):
                    dstT = cqT if which == 0 else ckvT
                    gbase = V_GBQ if which == 0 else V_GBKV
                    pp = []
                    for c in range(2):
                        p1, p1k = psb("pj", PJ)
                        col0 = which * 256 + c * 128
                        C.mm(p1[:], [(w1[:, k, col0:col0 + 128], hT[:, k, TB(tb)]) for k in range(8)],
                             reads=["wB1"] + C.h_reads(tb), writes=[p1k])
                        si = (ni * 2 + c) % 4
                        S.op("act", lambda e, p1=p1, si=si: e.activation(out=sqb[si][:], in_=p1[:], func=AF.Square),
                             reads=[p1k], writes=[("sqbB", si)])
                        pp.append((p1, p1k, si))
                    p2, p2k = psb("pj", PJ)
                    C.mm(p2[:], [(ones256[:], sqb[pp[0][2]][:]), (ones256[:], sqb[pp[1][2]][:])],
                         reads=["ones256", ("sqbB", pp[0][2]), ("sqbB", pp[1][2])], writes=[p2k])
                    ri = ni % 2
                    ni += 1
                    S.op("act", lambda e, p2=p2, ri=ri: e.activation(out=rsb[ri][:], in_=p2[:], func=AF.Ln, bias=epsb[:, 0:1], scale=1.0),
                         reads=[p2k, "eps"], writes=[("rsbB", ri)])
                    S.op("act", lambda e, ri=ri: e.activation(out=rsb[ri][:], in_=rsb[ri][:], func=AF.Exp, scale=-0.5), reads=[("rsbB", ri)], writes=[("rsbB", ri)])
                    for c in range(2):
                        p1, p1k, si = pp[c]
                        S.op("dve", lambda e, p1=p1, c=c, ri=ri, dstT=dstT, gbase=gbase, tb=tb: e.scalar_tensor_tensor(
                            out=dstT[:, c, TB(tb)], in0=p1[:], scalar=vec[l][:, gbase + c:gbase + c + 1], in1=rsb[ri][:], op0=ALU.mult, op1=ALU.mult),
                            reads=[p1k, ("rsbB", ri), ("vec", l)], writes=[("lat", which, c, tb)])
                pa, pak = psb("pj", PJ)
                C.mm(pa[0:96, :], [(w1[:, k, 512:608], hT[:, k, TB(tb)]) for k in range(8)], reads=["wB1"] + C.h_reads(tb), writes=[pak])
                pb, pbk = psb("pj", PJ)
                C.mm(pb[0:96, :], [(w1[:, k, 608:704], hT[:, k, TB(tb)]) for k in range(8)], reads=["wB1"] + C.h_reads(tb), writes=[pbk])
                i = 0
                rr_ = slice(64, 96)
                S.op("dve", lambda e, pa=pa, rp=rp, i=i: e.tensor_tensor(out=t1[i][rr_, :], in0=pa[rr_, :], in1=rp[rr_, 0, :], op=ALU.mult),
                     reads=[pak, ("ropeB", 0)], writes=[("t1b", i)])
                S.op("dve", lambda e, pb=pb, rp=rp, i=i: e.tensor_tensor(out=t2[i][rr_, :], in0=pb[rr_, :], in1=rp[rr_, 1, :], op=ALU.mult),
                     reads=[pbk, ("ropeB", 0)], writes=[("t2b", i)])
                S.op("pool", lambda e, i=i, tb=tb: e.tensor_tensor(out=kh[rr_, TB(tb)], in0=t1[i][rr_, :], in1=t2[i][rr_, :], op=ALU.add),
                     reads=[("t1b", i), ("t2b", i)], writes=[("krT", tb)])
            S.barrier()
        C.dump(f"cq{l}", cqT[:])
        w2 = sbt(ph, "wB2", [128, 5120], BF16)
        qh = sbt(ph, "qh", [128, T], BF16)
        Vb = sbt(ph, "Vb", [128, NT, 128], BF16)
        PT = [sbt(ph, "PTb", [128, 512], BF16) for _ in range(2)]
        rec = [sbt(ph, "recb", [128, 512], F32) for _ in range(1)]
        C.pt_rr = [0, 0]
        S.op("pool", lambda e: e.dma_start(out=w2[:], in_=dr[f"wB2{l}"][:, :]), writes=["wB2"], dma="wB2")
        wq = w2[:, 0:3072].rearrange("p (k h v m) -> p k h v m", k=2, h=8, v=2)
        wk = w2[:, 3072:4096].rearrange("p (k h m) -> p k h m", k=2, h=8)
        wv = w2[:, 4096:5120].rearrange("p (k h m) -> p k h m", k=2, h=8)
        latq = [("lat", 0, c, tb) for c in range(2) for tb in range(NB)]
        latkv = [("lat", 1, c, tb) for c in range(2) for tb in range(NB)]
        for h in range(8):
            par = h % 2
            for tb in range(NB):
                ri_ = (h * NB + tb) % 2
                rp = rope[ri_]
                S.op("sp", lambda e, rp=rp, tb=tb: e.dma_start(out=rp[:], in_=dr["ropeB"][:, :, TB(tb)]),
                     writes=[("ropeB", ri_)], dma=f"ropeB{ri_}")
                pa, pak = psb("pj", PJ)
                C.mm(pa[0:96, :], [(wq[:, k, h, 0, :], cqT[:, k, TB(tb)]) for k in range(2)], reads=["wB2"] + latq, writes=[pak])
                pb, pbk = psb("pj", PJ)
                C.mm(pb[0:96, :], [(wq[:, k, h, 1, :], cqT[:, k, TB(tb)]) for k in range(2)], reads=["wB2"] + latq, writes=[pbk])
                i = 0
                r96 = slice(0, 96)
                S.op("dve", lambda e, pa=pa, rp=rp, i=i: e.tensor_tensor(out=t1[i][r96, :], in0=pa[r96, :], in1=rp[r96, 0, :], op=ALU.mult),
                     reads=[pak, ("ropeB", ri_)], writes=[("t1b", i)])
                S.op("dve", lambda e, pb=pb, rp=rp, i=i: e.tensor_tensor(out=t2[i][r96, :], in0=pb[r96, :], in1=rp[r96, 1, :], op=ALU.mult),
                     reads=[pbk, ("ropeB", ri_)], writes=[("t2b", i)])
                S.op("pool", lambda e, i=i, tb=tb: e.tensor_tensor(out=qh[r96, TB(tb)], in0=t1[i][r96, :], in1=t2[i][r96, :], op=ALU.add),
                     reads=[("t1b", i), ("t2b", i)], writes=[("qh", tb)])
                pc, pck = psb("pj", PJ)
                C.mm(pc[0:64, :], [(wk[:, k, h, :], ckvT[:, k, TB(tb)]) for k in range(2)], reads=["wB2"] + latkv, writes=[pck])
                S.op("act", lambda e, pc=pc, tb=tb: e.copy(out=kh[0:64, TB(tb)], in_=pc[0:64, :]), reads=[pck], writes=[("kh", tb)])
            voff = 0 if par == 0 else 64
            S.op("pool", lambda e, ooff=64 - voff: e.memset(Vb[:, :, ooff:ooff + 64], 1.0), writes=["Vb1"])
            for tg in range(4):
                pv, pvk = psb("pj", PJ)

                def vfn(e, tg=tg, pv=pv, h=h):
                    ins = None
                    for j in range(4):
                        tt = tg * 4 + j
                        for k in range(2):
                            ins = e.matmul(pv[:, j * 64:(j + 1) * 64], ckvT[:, k, tt * 128:(tt + 1) * 128], wv[:, k, h, :],
                                           start=(k == 0), stop=(k == 1))
                    return ins
                S.op("pe", vfn, reads=["wB2"] + latkv, writes=[pvk])
                S.op("dve", lambda e, tg=tg, pv=pv, voff=voff: e.tensor_copy(out=Vb[:, tg * 4:(tg + 1) * 4, voff:voff + 64],
                                                                            in_=pv[:, 0:256].rearrange("p (j d) -> p j d", j=4)),
                     reads=[pvk], writes=[("Vbv", tg)])
            pr = slice(par * 64, par * 64 + 64)
            chunk = h // 2
            attn_core(C,
                      k_ap=lambda kt: kh[0:96, kt * 128:(kt + 1) * 128],
                      q_ap=lambda qb: qh[0:96, TB(qb)],
                      v_ap=lambda kt: Vb[:, kt, :],
                      par=par,
                      out_ap=lambda qb, pr=pr, chunk=chunk: ybT[pr, chunk, TB(qb)],
                      scale=96.0 ** -0.5,
                      kkeys=[("kh", tb) for tb in range(NB)], qkeys=[("qh", tb) for tb in range(NB)],
                      vkeys=[("Vbv", tg) for tg in range(4)] + ["Vb1"],
                      okey_fn=lambda qb, chunk=chunk, par=par: ("y1T", chunk, par, qb), PT=PT, rec=rec)
        S.barrier()
    C.dump(f"yb{l}", ybT[:])


def mixer_C(C, l):
    S, nc, dr, hT, vec, cst, cstb, sbt, ps = C.S, C.nc, C.dr, C.hT, C.vec, C.cst, C.cstb, C.sbt, C.ps
    CI, TB, epsb, ones128, lnsc = C.CI, C.TB, C.epsb, C.ones128, C.lnsc
    ycT = C.yT[2]
    with ExitStack() as ph:
        cT = sbt(ph, "cT", [128, T], BF16)
        with ExitStack() as ph0:
            wg_ = sbt(ph0, "wCg", [128, 8, 64], BF16)
            S.op("pool", lambda e: e.dma_start(out=wg_[:], in_=dr[f"wCg{l}"][:, :, :]), writes=["wCg"], dma="wCg")
            S.op("dve", lambda e: e.memset(cT[:], 1.0), writes=["cT"])
            for tb in range(NB):
                p, pk = C.psb("all", C.ALLB)
                C.mm(p[0:64, :], [(wg_[:, k, :], hT[:, k, TB(tb)]) for k in range(8)], reads=["wCg"] + C.h_reads(tb), writes=[pk])
                S.op("act", lambda e, p=p, tb=tb: e.copy(out=cT[0:48, TB(tb)], in_=p[0:48, :]), reads=[pk, "cT"], writes=[("cTv", tb)])
            S.barrier()
        wup = sbt(ph, "wup", [128, 2, 128], BF16)
        wh = sbt(ph, "wCh", [128, 5120], BF16)
        qT = sbt(ph, "qTc", [128, T], BF16)
        kT = sbt(ph, "kTc", [128, T], BF16)
        ktok = sbt(ph, "ktok", [128, NT, 128], BF16)
        vtok = sbt(ph, "vtok", [128, NT, 128], BF16)
        oT = sbt(ph, "oT", [128, T], F32)
        sp_ = sbt(ph, "sp", [128, 4, 128], F32)
        e1 = sp_[:].rearrange("p a b -> p (a b)")
        eb = sbt(ph, "eb", [128, 512], F32)
        enb = eb
        eg = sbt(ph, "eg", [128, 4, 128], F32)
        kt_ = sbt(ph, "kt", [128, 512], BF16)
        qt2 = [sbt(ph, "qt", [128, 512], BF16) for _ in range(2)]
        kd2 = [[sbt(ph, "kd", [128, 4, 128], BF16) for _ in range(2)] for _ in range(2)]
        ms2 = [sbt(ph, "ms", [128, 4, 128], BF16) for _ in range(2)]
        dec2 = [sbt(ph, "dec", [128, 8], F32) for _ in range(2)]
        Sst = [sbt(ph, "Sst", [128, 128], F32) for _ in range(2)]
        Sbf = [sbt(ph, "Sbf", [128, 128], BF16) for _ in range(4)]
        sqo = sp_[:].rearrange("p a b -> p (a b)")
        rso = eb
        sgo = eg[:].rearrange("p a b -> p (a b)")
        ckeys = []
        tri = {0: cst[:, CI["trif"], :], 1: cst[:, CI["trib"], :]}
        trib16 = {0: cstb[:, CI["trif"], :], 1: cstb[:, CI["trib"], :]}
        uu = {0: cst[:, CI["uf"], :], 1: cst[:, CI["ub"], :]}
        wl = wh[:, 0:3072].rearrange("p (k v m) -> p k v m", k=8, v=3)
        wr = wh[:, 3072:5120].rearrange("p (k m) -> p k m", k=8)
        si = [0]
        for h in range(4):
            S.op("pool", lambda e, h=h: e.dma_start(out=wh[:], in_=dr[f"wC{l}"][h]), writes=["wCh"], dma="wCh")
            S.op("pool", lambda e, h=h: e.dma_start(out=wup[:].rearrange("p b c -> p (b c)"), in_=dr[f"wCup{l}"][:, h * 256:(h + 1) * 256]),
                 writes=["wup"], dma="wCup")
            for gi in range(NB):
                for which, dst, dk in ((0, qT, "qTc"), (1, kT, "kTc")):
                    p, pk = C.psb("all", C.ALLB)
                    C.mm(p[:], [(wl[:, k, which, :], hT[:, k, TB(gi)]) for k in range(8)], reads=["wCh"] + C.h_reads(gi), writes=[pk])
                    S.op("act", lambda e, p=p, dst=dst, gi=gi: e.copy(out=dst[:, TB(gi)], in_=p[:]), reads=[pk], writes=[(dk, gi)])
                for half in range(2):
                    p, pk = C.psb("all", C.ALLB)

                    def kvfn(e, p=p, gi=gi, half=half):
                        ins = None
                        for j in range(2):
                            tt = gi * 4 + half * 2 + j
                            for k in range(8):
                                ins = e.matmul(p[:, j * 256:(j + 1) * 256], hT[:, k, tt * 128:(tt + 1) * 128], wr[:, k, :], start=(k == 0), stop=(k == 7))
                        return ins
                    S.op("pe", kvfn, reads=["wCh"] + C.h_reads(gi), writes=[pk])
                    t0 = gi * 4 + half * 2
                    pv = p[:].rearrange("p (j w) -> p j w", j=2)
                    S.op("dve", lambda e, pv=pv, t0=t0: e.tensor_copy(out=ktok[:, t0:t0 + 2, :], in_=pv[:, :, 0:128]), reads=[pk], writes=[("ktok", t0 // 2)])
                    S.op("act", lambda e, pv=pv, t0=t0: e.copy(out=vtok[:, t0:t0 + 2, :], in_=pv[:, :, 128:256]), reads=[pk], writes=[("vtok", t0 // 2)])
            def prologue(d, gi, pb_, part):
                qt, kd, ms, dec = qt2[pb_], kd2[pb_], ms2[pb_], dec2[pb_]
                pspk = ("ps", 0)
                if part == 0:
                    def spfn(e, gi=gi, d=d):
                        ins = None
                        for j in range(4):
                            tt = gi * 4 + j
                            ins = e.matmul(ps[0][:, j * 128:(j + 1) * 128], cT[:, tt * 128:(tt + 1) * 128], wup[:, d, :], start=True, stop=True)
                        return ins
                    S.op("pe", spfn, reads=ckeys + ["wup"], writes=[pspk])
                    S.op("act", lambda e: e.activation(out=e1, in_=ps[0][:], func=AF.Exp, scale=-1.0), reads=[pspk], writes=["sp"])
                    S.op("act", lambda e: e.activation(out=sp_[:].rearrange("p a b -> p (a b)"), in_=e1, func=AF.Ln, bias=C.ones1[:, 0:1], scale=1.0),
                         reads=["sp", "ones1"], writes=["sp"])

                if part == 1:
                    def cfn(e, d=d):
                        ins = None
                        for j in range(4):
                            ins = e.matmul(ps[1][:, j * 128:(j + 1) * 128], sp_[:, j, :], tri[d], start=True, stop=True)
                        return ins
                    S.op("pe", cfn, reads=["sp", "cst"], writes=[("ps", 1)])
                    S.op("act", lambda e: e.activation(out=eb[:], in_=ps[1][:], func=AF.Exp, scale=-1.0 / 16, bias=lnsc[:, 0:1]),
                         reads=[("ps", 1), "lnsc"], writes=["eb"])
                    S.op("dve", lambda e, gi=gi: e.tensor_tensor(out=qt[:], in0=qT[:, TB(gi)], in1=eb[:], op=ALU.mult), reads=[("qTc", gi), "eb"], writes=[("qt", pb_)])
                    S.op("act", lambda e: e.activation(out=enb[:], in_=ps[1][:], func=AF.Exp, scale=1.0 / 16), reads=[("ps", 1)], writes=["eb"])
                    S.op("pool", lambda e, gi=gi: e.tensor_tensor(out=kt_[:], in0=kT[:, TB(gi)], in1=enb[:], op=ALU.mult), reads=[("kTc", gi), "eb"], writes=["kt"])
                    pb3 = ps[1][:].rearrange("p (c s) -> p c s", c=8)
                    col = 63 if d == 0 else 0
                    S.op("act", lambda e, pb3=pb3, col=col: e.activation(out=dec[:], in_=pb3[:, :, col], func=AF.Exp, scale=-1.0 / 16),
                         reads=[("ps", 1)], writes=[("dec", pb_)])

                    def gfn(e, d=d):
                        ins = None
                        for j in range(4):
                            ins = e.matmul(ps[2][:, j * 128:(j + 1) * 128], uu[d], sp_[:, j, :], start=True, stop=True)
                        return ins
                    S.op("pe", gfn, reads=["sp", "cst"], writes=[("ps", 2)])
                    S.op("act", lambda e: e.activation(out=eg[:].rearrange("p a b -> p (a b)"), in_=ps[2][:], func=AF.Exp, scale=-1.0 / 16),
                         reads=[("ps", 2)], writes=["eg"])
                if part == 2:
                    for ch_ in range(2):
                        rmask = cst[:, CI["trif"], 63:64] if ch_ == 0 else cst[:, CI["trib"], 64:65]
                        S.op("dve", lambda e, gi=gi, ch_=ch_, rmask=rmask: e.scalar_tensor_tensor(out=kd[ch_][:], in0=eg[:], scalar=rmask, in1=ktok[:, gi * 4:gi * 4 + 4, :],
                                                                                              op0=ALU.mult, op1=ALU.mult),
                             reads=[("ktok", gi * 2), ("ktok", gi * 2 + 1), "eg", "cst"], writes=[("kd", pb_, ch_)])

                    def sfn(e):
                        ins = None
                        for j in range(4):
                            ins = e.matmul(ps[3][:, j * 128:(j + 1) * 128], kt_[:, j * 128:(j + 1) * 128], qt[:, j * 128:(j + 1) * 128], start=True, stop=True)
                        return ins
                    S.op("pe", sfn, reads=["kt", ("qt", pb_)], writes=[("ps", 3)])
                if part == 3:
                    S.op("dve", lambda e, d=d: e.tensor_tensor(out=ms[:], in0=ps[3][:].rearrange("p (j c) -> p j c", j=4),
                                                              in1=trib16[d].unsqueeze(1).to_broadcast([128, 4, 128]), op=ALU.mult),
                         reads=[("ps", 3), "cstb"], writes=[("ms", pb_)])

            def chain(d, gi, pb_, first, after_tile=None):
                qt, kd, ms, dec = qt2[pb_], kd2[pb_], ms2[pb_], dec2[pb_]
                if first:
                    cur = si[0] % 2
                    S.op("dve", lambda e, cur=cur: e.memset(Sst[cur][:], 0.0), writes=[("Sst", cur)])
                    sb_i = si[0] % 4
                    S.op("pool", lambda e, sb_i=sb_i: e.memset(Sbf[sb_i][:], 0.0), writes=[("Sbf", sb_i)])
                tiles = list(range(gi * 4, gi * 4 + 4))
                ob = 4 + (si[0] % 2)
                po, pok = ps[ob], ("ps", ob)
                torder = tiles if d == 0 else tiles[::-1]
                for tt in torder:
                    j = tt - gi * 4
                    corder = (0, 1) if d == 0 else (1, 0)
                    S.op("pe", lambda e, tt=tt, j=j, po=po: e.matmul(po[:, j * 128:(j + 1) * 128], vtok[:, tt, :], ms[:, j, :], start=True, stop=False),
                         reads=[("vtok", tt // 2), ("ms", pb_)], writes=[pok])
                    for ci, ch in enumerate(corder):
                        cur = si[0] % 2
                        sb_i = si[0] % 4
                        csl = slice(j * 128 + ch * 64, j * 128 + ch * 64 + 64)
                        S.op("pe", lambda e, po=po, csl=csl, sb_i=sb_i, last=(ci == 1): e.matmul(po[:, csl], Sbf[sb_i][:], qt[:, csl], start=False, stop=last),
                             reads=[("Sbf", sb_i), ("qt", pb_)], writes=[pok])
                        kb = 6 + (si[0] % 2)
                        S.op("pe", lambda e, kb=kb, ch=ch, j=j, tt=tt: e.matmul(ps[kb][:, 0:128], kd[ch][:, j, :], vtok[:, tt, :], start=True, stop=True),
                             reads=[("kd", pb_, ch), ("vtok", tt // 2)], writes=[("ps", kb)])
                        nxt = 1 - cur
                        dcol = j * 2 + ch
                        nb_ = (si[0] + 1) % 4
                        S.op("dve", lambda e, kb=kb, cur=cur, nb_=nb_, dcol=dcol: e.scalar_tensor_tensor(
                            out=Sbf[nb_][:], in0=Sst[cur][:], scalar=dec[:, dcol:dcol + 1], in1=ps[kb][:, 0:128], op0=ALU.mult, op1=ALU.add),
                            reads=[("Sst", cur), ("dec", pb_), ("ps", kb)], writes=[("Sbf", nb_)])
                        S.op("dve", lambda e, kb=kb, cur=cur, nxt=nxt, dcol=dcol: e.scalar_tensor_tensor(
                            out=Sst[nxt][:], in0=Sst[cur][:], scalar=dec[:, dcol:dcol + 1], in1=ps[kb][:, 0:128], op0=ALU.mult, op1=ALU.add),
                            reads=[("Sst", cur), ("dec", pb_), ("ps", kb)], writes=[("Sst", nxt)])
                        si[0] += 1
                    if after_tile is not None:
                        after_tile(torder.index(tt))
                if d == 0:
                    S.op("act", lambda e, po=po, gi=gi: e.copy(out=oT[:, TB(gi)], in_=po[:]), reads=[pok], writes=[("oT", gi)])
                else:
                    S.op("dve", lambda e, po=po, gi=gi: e.tensor_tensor(out=oT[:, TB(gi)], in0=oT[:, TB(gi)], in1=po[:], op=ALU.add),
                         reads=[pok, ("oT", gi)], writes=[("oT", gi)])

            steps = [(0, gi, gi == 0) for gi in range(NB)] + [(1, gi, gi == NB - 1) for gi in range(NB - 1, -1, -1)]
            for part in range(4):
                prologue(steps[0][0], steps[0][1], 0, part)
            for k_, (d, gi, first) in enumerate(steps):
                if k_ + 1 < len(steps):
                    nd, ngi, npb = steps[k_ + 1][0], steps[k_ + 1][1], (k_ + 1) % 2
                    prologue(nd, ngi, npb, 0)
                    chain(d, gi, k_ % 2, first, after_tile=lambda ti, nd=nd, ngi=ngi, npb=npb: prologue(nd, ngi, npb, ti + 1) if ti < 3 else None)
                else:
                    chain(d, gi, k_ % 2, first)
            for gi in range(NB):
                S.op("act", lambda e, gi=gi: e.activation(out=sqo, in_=oT[:, TB(gi)], func=AF.Square), reads=[("oT", gi)], writes=["sp"])
                C.mm(ps[0][:], [(ones128[:], sqo)], reads=["ones128", "sp"], writes=[("ps", 0)])
                S.op("act", lambda e: e.activation(out=rso[:], in_=ps[0][:], func=AF.Ln, bias=epsb[:, 0:1], scale=1.0), reads=[("ps", 0), "eps"], writes=["eb"])
                S.op("act", lambda e: e.activation(out=rso[:], in_=rso[:], func=AF.Exp, scale=-0.5), reads=["eb"], writes=["eb"])
                C.mm(ps[1][:], [(wl[:, k, 2, :], hT[:, k, TB(gi)]) for k in range(8)], reads=["wCh"] + C.h_reads(gi), writes=[("ps", 1)])
                S.op("act", lambda e: e.activation(out=sgo, in_=ps[1][:], func=AF.Silu), reads=[("ps", 1)], writes=["eg"])
                S.op("dve", lambda e, gi=gi: e.scalar_tensor_tensor(out=oT[:, TB(gi)], in0=oT[:, TB(gi)], scalar=vec[l][:, V_GCO:V_GCO + 1], in1=rso[:],
                                                                 op0=ALU.mult, op1=ALU.mult), reads=[("oT", gi), "eb", ("vec", l)], writes=[("oT", gi)])
                S.op("dve", lambda e, gi=gi, h=h: e.tensor_tensor(out=ycT[:, h, TB(gi)], in0=oT[:, TB(gi)], in1=sgo, op=ALU.mult),
                     reads=[("oT", gi), "eg"], writes=[("y2T", h, gi)])
            if h == 0:
                C.dump(f"oc{l}", oT[:])
        S.barrier()
    C.dump(f"yc{l}", ycT[:])


def merge_out(C, l):
    S, dr, hT, vec, sbt, psb, TB, xT = C.S, C.dr, C.hT, C.vec, C.sbt, C.psb, C.TB, C.xT
    yT = C.yT
    with ExitStack() as ph:
        mT = sbt(ph, "mT", [128, 8, T], BF16)
        with ExitStack() as ph1:
            wm = [sbt(ph1, "wm", [128, 1536], BF16) for _ in range(3)]
            sg = [sbt(ph1, "sg", [128, 512], BF16) for _ in range(2)]
            acc = sbt(ph1, "macc", [128, NB, 512], F32)
            tm = [sbt(ph1, "tm", [128, 512], F32) for _ in range(2)]
            ykeys = [[k_ for k_ in S.last_writer if isinstance(k_, tuple) and k_[0] == f"y{br}T"] for br in range(3)]
            pi = 0
            si = 0
            for j in range(8):
                for br in range(3):
                    w_i = pi % 3
                    pi += 1
                    S.op("pool", lambda e, w_i=w_i, j=j, br=br: e.dma_start(out=wm[w_i][:], in_=dr[f"wM{l}"][j * 3 + br]),
                         writes=[("wm", w_i)], dma=f"wm{w_i}")
                    wp = wm[w_i][:, 0:512].rearrange("p (k m) -> p k m", k=4)
                    wgt = wm[w_i][:, 512:1536].rearrange("p (k m) -> p k m", k=8)
                    bcol = V_BG + br * 8 + j
                    for tb in range(NB):
                        pg, pgk = psb("mg", [0, 1, 2, 3])
                        C.mm(pg[:], [(wgt[:, k, :], hT[:, k, TB(tb)]) for k in range(8)], reads=[("wm", w_i)] + C.h_reads(tb), writes=[pgk])
                        pp, ppk = psb("mp", [4, 5, 6, 7])
                        C.mm(pp[:], [(wp[:, k, :], yT[br][:, k, TB(tb)]) for k in range(4)], reads=[("wm", w_i)] + ykeys[br], writes=[ppk])
                        s_i = si % 2
                        si += 1
                        S.op("act", lambda e, pg=pg, s_i=s_i, bcol=bcol: e.activation(out=sg[s_i][:], in_=pg[:], func=AF.Sigmoid,
                                                                                     bias=vec[l][:, bcol:bcol + 1], scale=1.0),
                             reads=[pgk, ("vec", l)], writes=[("sg", s_i)])
                        if br == 0:
                            S.op("dve", lambda e, pp=pp, s_i=s_i, tb=tb: e.tensor_tensor(out=acc[:, tb, :], in0=pp[:], in1=sg[s_i][:], op=ALU.mult),
                                 reads=[ppk, ("sg", s_i)], writes=[("macc", tb)])
                        else:
                            S.op("dve", lambda e, pp=pp, s_i=s_i: e.tensor_tensor(out=tm[s_i][:], in0=pp[:], in1=sg[s_i][:], op=ALU.mult),
                                 reads=[ppk, ("sg", s_i)], writes=[("tm", s_i)])
                            if br == 1:
                                S.op("pool", lambda e, s_i=s_i, tb=tb: e.tensor_tensor(out=acc[:, tb, :], in0=acc[:, tb, :], in1=tm[s_i][:], op=ALU.add),
                                     reads=[("macc", tb), ("tm", s_i)], writes=[("macc", tb)])
                            else:
                                S.op("pool", lambda e, s_i=s_i, tb=tb, j=j: e.tensor_tensor(out=mT[:, j, TB(tb)], in0=acc[:, tb, :], in1=tm[s_i][:], op=ALU.add),
                                     reads=[("macc", tb), ("tm", s_i)], writes=[("mT", j, tb)])
            S.barrier()
        wo = [sbt(ph, "wo", [128, 1024], BF16) for _ in range(2)]
        for j in range(8):
            w_i = j % 2
            S.op("pool", lambda e, w_i=w_i, j=j: e.dma_start(out=wo[w_i][:], in_=dr[f"wO{l}"][j]), writes=[("wo", w_i)], dma=f"wo{w_i}")
            wov = wo[w_i][:].rearrange("p (k m) -> p k m", k=8)
            for tb in range(NB):
                po, pok = psb("all", C.ALLB)
                C.mm(po[:], [(wov[:, k, :], mT[:, k, TB(tb)]) for k in range(8)], reads=[("wo", w_i)], writes=[pok])
                S.op("dve", lambda e, po=po, j=j, tb=tb: e.tensor_tensor(out=xT[:, j, TB(tb)], in0=xT[:, j, TB(tb)], in1=po[:], op=ALU.add),
                     reads=[pok, C.xk(j, tb)], writes=[C.xk(j, tb)])
        S.barrier()
    C.dump(f"xmix{l}", xT[:])


def ffn_groups(C, wname, g0, ng, gbc=None, gkey=None):
    S, dr, hT, psb, TB, xT = C.S, C.dr, C.hT, C.psb, C.TB, C.xT
    wf, abuf, sgf = C.wf, C.abuf, C.sgf
    GU = [0, 1, 2, 3]
    OB = [4, 5, 6, 7]
    g = 0
    while g < ng:
        n_in = min(2, ng - g)
        a_i = C.ffi[1] % 2
        C.ffi[1] += 1
        ab = abuf[a_i]
        wviews = []
        for q in range(n_in):
            w_i = C.ffi[0] % 4
            C.ffi[0] += 1
            S.op("pool", lambda e, w_i=w_i, gg=g0 + g + q: e.dma_start(out=wf[w_i][:], in_=dr[wname][gg]), writes=[("wf", w_i)], dma=f"wf{w_i}")
            wgv = wf[w_i][:, 0:2048].rearrange("p (c k m) -> p c k m", c=2, k=8)
            wuv = wf[w_i][:, 2048:4096].rearrange("p (c k m) -> p c k m", c=2, k=8)
            wdv = wf[w_i][:, 4096:6144].rearrange("p (c j m) -> p c j m", c=2, j=8)
            wviews.append((w_i, wdv))
            for c in range(2):
                cc = q * 2 + c
                for tb in range(NB):
                    pg, pgk = psb("gu", GU)
                    C.mm(pg[:], [(wgv[:, c, k, :], hT[:, k, TB(tb)]) for k in range(8)], reads=[("wf", w_i)] + C.h_reads(tb), writes=[pgk])
                    pu, puk = psb("gu", GU)
                    C.mm(pu[:], [(wuv[:, c, k, :], hT[:, k, TB(tb)]) for k in range(8)], reads=[("wf", w_i)] + C.h_reads(tb), writes=[puk])
                    s_i = C.ffi[2] % 2
                    C.ffi[2] += 1
                    S.op("act", lambda e, pg=pg, s_i=s_i: e.activation(out=sgf[s_i][:], in_=pg[:], func=AF.Silu), reads=[pgk], writes=[("sgf", s_i)])
                    if gbc is not None:
                        S.op("pool", lambda e, s_i=s_i, tb=tb: e.tensor_tensor(out=sgf[s_i][:], in0=sgf[s_i][:], in1=gbc[:, TB(tb)], op=ALU.mult),
                             reads=[("sgf", s_i), gkey], writes=[("sgf", s_i)])
                    S.op("dve", lambda e, pu=pu, s_i=s_i, ab=ab, cc=cc, tb=tb: e.tensor_tensor(out=ab[:, cc, TB(tb)], in0=pu[:], in1=sgf[s_i][:], op=ALU.mult),
                         reads=[puk, ("sgf", s_i)], writes=[("ab", a_i, cc, tb)])
        for tb in range(NB):
            for j in range(8):
                po, pok = psb("ob", OB)
                pairs = [(wdv[:, c, j, :], ab[:, q * 2 + c, TB(tb)]) for q, (w_i, wdv) in enumerate(wviews) for c in range(2)]
                C.mm(po[:], pairs,
                     reads=[("wf", w_i) for (w_i, _) in wviews] + [("ab", a_i, cc, tb) for cc in range(2 * n_in)], writes=[pok])
                S.op("dve", lambda e, po=po, j=j, tb=tb: e.tensor_tensor(out=xT[:, j, TB(tb)], in0=xT[:, j, TB(tb)], in1=po[:], op=ALU.add),
                     reads=[pok, C.xk(j, tb)], writes=[C.xk(j, tb)])
        g += n_in


def ffn_alloc(C, ph):
    sbt = C.sbt
    C.wf = [sbt(ph, "wf", [128, 6144], BF16) for _ in range(4)]
    C.abuf = [sbt(ph, "abuf", [128, 4, T], BF16) for _ in range(2)]
    C.sgf = [sbt(ph, "sgf", [128, 512], F32) for _ in range(2)]
    C.ffi = [0, 0, 0]


def ffn_dense(C):
    with ExitStack() as ph:
        ffn_alloc(C, ph)
        ffn_groups(C, "wF0", 0, DFF // 256)
        C.S.barrier()
    C.dump("xffn0", C.xT[:])


def moe(C, l):
    S, dr, sbt, vec, cst, CI, ps, xT = C.S, C.dr, C.sbt, C.vec, C.cst, C.CI, C.ps, C.xT
    with ExitStack() as ph0:
        gate = sbt(ph0, "gate", [128, NT, NEXP], F32)
        with ExitStack() as ph:
            wr = sbt(ph, "wR", [128, 8, 8], F32)
            lg = sbt(ph, "lg", [128, NT, NEXP], F32)
            lg2 = sbt(ph, "lg2", [128, NT, NEXP], F32)
            mk1 = sbt(ph, "mk1", [128, NT, NEXP], F32)
            mk2 = sbt(ph, "mk2", [128, NT, NEXP], F32)
            m1 = sbt(ph, "m1", [128, NT], F32)
            m2 = sbt(ph, "m2", [128, NT], F32)
            w1 = sbt(ph, "w1", [128, NT], F32)
            w2 = sbt(ph, "w2", [128, NT], F32)
            S.op("sp", lambda e: e.dma_start(out=wr[:], in_=dr["wR"][:, :, :]), writes=["wR"], dma="wR")
            plg = ps[7]

            def router(tb, h32):
                def rfn(e):
                    ins = None
                    for jt in range(4):
                        tt = tb * 4 + jt
                        for c in range(8):
                            ins = e.matmul(plg[:, tt * 8:(tt + 1) * 8], h32[:, c, jt * 128:(jt + 1) * 128], wr[:, c, :], start=(c == 0), stop=(c == 7))
                    return ins
                S.op("pe", rfn, reads=[("h32", c) for c in range(8)] + ["wR"], writes=[("plg", tb)])
            C.rmsnorm_to_h(lambda c: vec[l][:, V_GFFN + c:V_GFFN + c + 1], ("vec", l), router=router)
            bc = lambda a: a[:].unsqueeze(2).to_broadcast([128, NT, NEXP])
            S.op("dve", lambda e: e.tensor_copy(out=lg[:].rearrange("p a b -> p (a b)"), in_=plg[:, 0:NT * NEXP]), writes=["lg"])
            S.op("dve", lambda e: e.tensor_reduce(out=m1[:], in_=lg[:], axis=AX.X, op=ALU.max), reads=["lg"], writes=["m1"])
            S.op("dve", lambda e: e.tensor_tensor(out=mk1[:], in0=lg[:], in1=bc(m1), op=ALU.is_equal), reads=["lg", "m1"], writes=["mk1"])
            S.op("dve", lambda e: e.scalar_tensor_tensor(out=lg2[:], in0=mk1[:], scalar=-1e30, in1=lg[:], op0=ALU.mult, op1=ALU.add),
                 reads=["mk1", "lg"], writes=["lg2"])
            S.op("dve", lambda e: e.tensor_reduce(out=m2[:], in_=lg2[:], axis=AX.X, op=ALU.max), reads=["lg2"], writes=["m2"])
            S.op("dve", lambda e: e.tensor_tensor(out=mk2[:], in0=lg2[:], in1=bc(m2), op=ALU.is_equal), reads=["lg2", "m2"], writes=["mk2"])
            S.op("dve", lambda e: e.tensor_tensor(out=w2[:], in0=m2[:], in1=m1[:], op=ALU.subtract), reads=["m1", "m2"], writes=["w2"])
            S.op("act", lambda e: e.activation(out=w2[:], in_=w2[:], func=AF.Exp), reads=["w2"], writes=["w2"])
            S.op("dve", lambda e: e.tensor_tensor(out=w1[:], in0=w2[:], in1=C.ones1[:, 0:NT], op=ALU.add), reads=["w2", "ones1"], writes=["w1"])
            S.op("dve", lambda e: e.reciprocal(out=w1[:], in_=w1[:]), reads=["w1"], writes=["w1"])
            S.op("dve", lambda e: e.tensor_tensor(out=w2[:], in0=w2[:], in1=w1[:], op=ALU.mult), reads=["w1", "w2"], writes=["w2"])
            S.op("dve", lambda e: e.tensor_tensor(out=mk1[:], in0=mk1[:], in1=bc(w1), op=ALU.mult), reads=["mk1", "w1"], writes=["mk1"])
            S.op("dve", lambda e: e.tensor_tensor(out=mk2[:], in0=mk2[:], in1=bc(w2), op=ALU.mult), reads=["mk2", "w2"], writes=["mk2"])
            S.op("dve", lambda e: e.tensor_tensor(out=gate[:], in0=mk1[:], in1=mk2[:], op=ALU.add), reads=["mk1", "mk2"], writes=["gate"])
            S.barrier()
        C.dump("gate", gate[:].rearrange("p a b -> p (a b)"))
        with ExitStack() as ph:
            ffn_alloc(C, ph)
            gbc = [sbt(ph, "gbc", [128, T], F32) for _ in range(2)]
            dg = [sbt(ph, "dg", [128, 128], F32) for _ in range(2)]
            ident = cst[:, CI["ident"], :]
            di = 0
            for ex in range(NEXP):
                gb = gbc[ex % 2]
                for tg in range(4):
                    pb_, pbk = C.psb("ob", [4, 5, 6, 7])
                    for jt in range(4):
                        tt = tg * 4 + jt
                        d_i = di % 2
                        di += 1
                        S.op("dve", lambda e, d_i=d_i, tt=tt, ex=ex: e.scalar_tensor_tensor(out=dg[d_i][:], in0=ident, scalar=gate[:, tt, ex:ex + 1], in1=C.ones1[:], op0=ALU.mult, op1=ALU.mult),
                             reads=["gate", "cst", "ones1"], writes=[("dg", d_i)])
                        S.op("pe", lambda e, d_i=d_i, jt=jt, pb_=pb_: e.matmul(pb_[:, jt * 128:(jt + 1) * 128], C.ones1[:], dg[d_i][:], start=True, stop=True),
                             reads=[("dg", d_i), "ones1"], writes=[pbk])
                    S.op("act", lambda e, pb_=pb_, gb=gb, tg=tg: e.copy(out=gb[:, tg * 512:(tg + 1) * 512], in_=pb_[:]), reads=[pbk], writes=[("gbc", ex % 2)])
                ffn_groups(C, "wE", ex * (DFFE // 256), DFFE // 256, gbc=gb, gkey=("gbc", ex % 2))
            S.barrier()
    C.dump("xffn1", xT[:])


def final_out(C):
    S, sbt, xT, TB, gfin, epsb, onesm, psb, ident = C.S, C.sbt, C.xT, C.TB, C.gfin, C.epsb, C.onesm, C.psb, C.ident
    with ExitStack() as ph:
        sq = [sbt(ph, "sqf", [128, 512], F32) for _ in range(2)]
        rs = sbt(ph, "rsf", [128, T], F32)
        yt = [sbt(ph, "ytf", [128, 128], F32) for _ in range(4)]
        stage = [sbt(ph, "stgo", [128, D], F32) for _ in range(2)]
        for tb in range(NB):
            p, pk = psb("all", C.ALLB)
            for c in range(8):
                s_ = sq[c % 2]
                S.op("act", lambda e, s_=s_, c=c, tb=tb: e.activation(out=s_[:], in_=xT[:, c, TB(tb)], func=AF.Square),
                     reads=[C.xk(c, tb)], writes=[("sqf", c % 2)])
                S.op("pe", lambda e, s_=s_, c=c, p=p: e.matmul(p[:], onesm[:], s_[:], start=(c == 0), stop=(c == 7)),
                     reads=[("sqf", c % 2), "onesm"], writes=[pk])
            S.op("act", lambda e, p=p, tb=tb: e.activation(out=rs[:, TB(tb)], in_=p[:], func=AF.Ln, bias=epsb[:, 0:1], scale=1.0),
                 reads=[pk, "eps"], writes=[("rsf", tb)])
            S.op("act", lambda e, tb=tb: e.activation(out=rs[:, TB(tb)], in_=rs[:, TB(tb)], func=AF.Exp, scale=-0.5), reads=[("rsf", tb)], writes=[("rsf", tb)])
        yi = 0
        for tt in range(NT):
            tb = tt // 4
            st = stage[tt % 2]
            for half in range(2):
                p, pk = psb("all", C.ALLB)
                for j in range(4):
                    c = half * 4 + j
                    y_i = yi % 4
                    yi += 1
                    S.op("dve", lambda e, y_i=y_i, c=c, tt=tt: e.scalar_tensor_tensor(out=yt[y_i][:], in0=xT[:, c, tt * 128:(tt + 1) * 128], scalar=gfin[:, c:c + 1],
                                                                                 in1=rs[:, tt * 128:(tt + 1) * 128], op0=ALU.mult, op1=ALU.mult),
                         reads=[C.xk(c, tb), ("rsf", tb), "gfin"], writes=[("ytf", y_i)])
                    S.op("pe", lambda e, y_i=y_i, j=j, p=p: e.transpose(p[:, j * 128:(j + 1) * 128], yt[y_i][:], ident),
                         reads=[("ytf", y_i), "cst"], writes=[pk])
                S.op("act", lambda e, p=p, st=st, half=half: e.copy(out=st[:, half * 512:(half + 1) * 512], in_=p[:]),
                     reads=[pk], writes=[("stgo", tt % 2, half)])
            S.op("sp", lambda e, st=st, tt=tt: e.dma_start(out=C.y_out[tt * 128:(tt + 1) * 128, :], in_=st[:]),
                 reads=[("stgo", tt % 2, 0), ("stgo", tt % 2, 1)], dma=f"yout{tt % 2}")


_CACHE = {}


def kernel(**inputs):
    x = np.asarray(inputs["x"], dtype=np.float32)
    packed = pack_inputs(inputs)
    shapes = {k: v.shape for k, v in packed.items()}
    shapes["x"] = (T, D)
    nc = build_program(shapes)
    in_maps = []
    for b in range(8):
        m = dict(packed)
        m["x"] = np.ascontiguousarray(x[b])
        in_maps.append(m)
    res = run_bass_kernel_spmd(nc, in_maps, core_ids=list(range(8)))
    return np.stack([np.asarray(res.results[b]["y"], dtype=np.float32) for b in range(8)], 0)
```

```python
import numpy as np
import concourse.bass as bass
import concourse.mybir as mybir
from concourse.bass_utils import run_bass_kernel_spmd
from contextlib import ExitStack

F32 = mybir.dt.float32
BF16 = mybir.dt.bfloat16
AF = mybir.ActivationFunctionType
ALU = mybir.AluOpType
AX = mybir.AxisListType

T = 2048
D = 1024
NT = 16
NB = 4
DEPTH = 2
DFF = 2816
NEXP = 8
DFFE = 3584
EPS = 1e-6
ENGS = ("sp", "act", "dve", "pool", "pe")
SEM_EPOCH = 30000
SKIP = ''
STRICT = True
HLS = (0, 1, 2, 3)
DEBUG_OUT = {}


class Sched:
    def __init__(self, nc, es):
        self.nc = nc
        self.es = es
        self.ops = []
        self.last_writer = {}
        self.readers = {}
        self.dma_sems = {}
        self.eng_ops = {e: [] for e in ENGS}
        self.bar_start = 0

    def op(self, eng, fn, reads=(), writes=(), dma=None, dma_batch=False, extra=()):
        deps = set(extra)
        raw = set(extra)
        for k in reads:
            w = self.last_writer.get(k)
            if w is not None:
                deps.add(w)
                raw.add(w)
            if isinstance(k, tuple) and k and k[0] in ("ps", "plg"):
                for r in self.readers.get(k, ()):
                    if self.ops[r]["eng"] != eng:
                        deps.add(r)
                        raw.add(r)
        for k in writes:
            w = self.last_writer.get(k)
            if w is not None:
                deps.add(w)
            for r in self.readers.get(k, ()):
                deps.add(r)
        idx = len(self.ops)
        o = dict(eng=eng, fn=fn, deps=deps, dma=dma, needed=False, idx=idx, raw=raw)
        if dma is not None:
            d = self.dma_sems.setdefault(dma, dict(total=0, batch=dma_batch))
            d["total"] += 16
            o["dma_val"] = d["total"]
        self.ops.append(o)
        self.eng_ops[eng].append(idx)
        for k in reads:
            self.readers.setdefault(k, []).append(idx)
        for k in writes:
            self.last_writer[k] = idx
            self.readers[k] = []
        return idx

    def barrier(self):
        last = [self.eng_ops[e][-1] for e in ENGS if self.eng_ops[e] and not self.ops[self.eng_ops[e][-1]].get("bar")]
        dmas = [i for i in range(self.bar_start, len(self.ops)) if self.ops[i]["dma"] is not None]
        ex = set(last + dmas)
        for e in ENGS:
            i_ = self.op(e, lambda eng: eng.nop(), extra=ex)
            self.ops[i_]["bar"] = True
        self.bar_start = len(self.ops)
        self.last_writer = {}
        self.readers = {}

    def emit(self, final_waits=()):
        nc = self.nc
        ops = self.ops
        for o in ops:
            nd = set()
            for d in o["deps"]:
                p = ops[d]
                if p["dma"] is None and p["eng"] == o["eng"] and o["dma"] is None:
                    if o["eng"] == "pe":
                        continue
                    if d not in o["raw"] and not STRICT:
                        continue
                nd.add(d)
            o["deps"] = nd
            for d in nd:
                ops[d]["needed"] = True
        cnt = {e: 0 for e in ENGS}
        for o in ops:
            if o["dma"] is None and o["needed"]:
                cnt[o["eng"]] += 1
                o["sig"] = cnt[o["eng"]]
        self.cnt = cnt
        eng_sems = {}
        for e in ENGS:
            n_ep = max(cnt[e] - 1, 0) // SEM_EPOCH + 1
            eng_sems[e] = [self.es.enter_context(nc.semaphore(f"s_{e}{i}")) for i in range(n_ep)]
        dsem = {}
        for name in self.dma_sems:
            dsem[name] = self.es.enter_context(nc.semaphore(f"d_{name}"))

        def event(o):
            if o["dma"] is not None:
                d = self.dma_sems[o["dma"]]
                v = d["total"] if d["batch"] else o["dma_val"]
                return (("d", o["dma"]), dsem[o["dma"]], v)
            s = o["sig"]
            ep = (s - 1) // SEM_EPOCH
            return (("e", o["eng"], ep), eng_sems[o["eng"]][ep], s - ep * SEM_EPOCH)

        block = self.es.enter_context(nc.Block())

        def run_engine(ename):
            def body(eng):
                wm = {}
                for idx in self.eng_ops[ename]:
                    o = ops[idx]
                    need = {}
                    for d in o["deps"]:
                        key, sem, val = event(ops[d])
                        if val > need.get(key, (None, 0))[1]:
                            need[key] = (sem, val)
                    for key, (sem, val) in need.items():
                        if key[0] == "e":
                            if any(k[0] == "e" and k[1] == key[1] and k[2] > key[2] for k in wm):
                                continue
                        if wm.get(key, 0) >= val:
                            continue
                        eng.wait_ge(sem, val)
                        wm[key] = val
                    ins = o["fn"](eng)
                    if o["dma"] is not None:
                        ins.then_inc(dsem[o["dma"]], 16)
                    elif o["needed"]:
                        key, sem, val = event(o)
                        ins.then_inc(sem, 1)
                for (e2, name) in final_waits:
                    if e2 == ename and name in dsem:
                        eng.wait_ge(dsem[name], self.dma_sems[name]["total"])
            return body

        block.sync(run_engine("sp"))
        block.scalar(run_engine("act"))
        block.vector(run_engine("dve"))
        block.gpsimd(run_engine("pool"))
        block.tensor(run_engine("pe"))


IN_SIZES = (512, 128, 128, 256, 256, 32, 512, 512, 512, 512, 16, 16, 3072)
OFF = np.concatenate([[0], np.cumsum(IN_SIZES)]).astype(int)
(O_AQ, O_AK, O_AV, O_BCQ, O_BCKV, O_BKR, O_CQ, O_CK, O_CV, O_CG, O_CAF, O_CAB, O_GATE) = OFF[:13]


def lhsT(W, cols):
    W = np.asarray(W)
    kc = W.shape[0] // 128
    sub = W[:, cols] if cols is not None else W
    return np.ascontiguousarray(sub.reshape(kc, 128, sub.shape[1]).transpose(1, 0, 2))


def partner(n):
    h = n // 2
    return np.concatenate([np.arange(h, n), np.arange(0, h)])


def rope_tables():
    GRID_W = 64
    rows = T // GRID_W
    row = np.repeat(np.arange(rows, dtype=np.float32), GRID_W)
    col = np.tile(np.arange(GRID_W, dtype=np.float32), rows)

    def tabs(rot_dim):
        nf = rot_dim // 4
        inv = (10000.0 ** (-np.arange(nf, dtype=np.float32) / nf)).astype(np.float32)
        ar = row[:, None] * inv
        ac = col[:, None] * inv
        half = rot_dim // 2
        cos = np.concatenate([np.cos(ar), np.cos(ar), np.cos(ac), np.cos(ac)], axis=1)
        sin = np.concatenate([-np.sin(ar), np.sin(ar), -np.sin(ac), np.sin(ac)], axis=1)
        return cos.T.astype(np.float32), sin.T.astype(np.float32)
    ca, sa = tabs(64)
    cb, sb = tabs(32)
    cosA = np.concatenate([ca, ca], 0)
    sinA = np.concatenate([sa, sa], 0)
    cosB = np.concatenate([np.ones((64, T), np.float32), cb, np.zeros((32, T), np.float32)], 0)
    sinB = np.concatenate([np.zeros((64, T), np.float32), sb, np.zeros((32, T), np.float32)], 0)
    return (np.ascontiguousarray(cosA), np.ascontiguousarray(sinA),
            np.ascontiguousarray(cosB), np.ascontiguousarray(sinB))


def rope_partner_cols(n_rot):
    h = n_rot // 2
    p = partner(h)
    return np.concatenate([p, h + p])


def consts_pack():
    c = {}
    c["ident"] = np.eye(128, dtype=np.float32)
    blk = np.zeros((128, 128), np.float32)
    blk[:64, :64] = 1.0 / 64
    blk[64:, 64:] = 1.0 / 64
    c["blk64"] = blk
    pa = rope_partner_cols(64)
    full = np.concatenate([pa, 64 + pa])
    P = np.zeros((128, 128), np.float32)
    P[full, np.arange(128)] = 1.0
    c["permA"] = P
    s = np.arange(128)
    same = (s[:, None] // 64) == (s[None, :] // 64)
    c["trif"] = (same & (s[:, None] <= s[None, :])).astype(np.float32)
    c["trib"] = (same & (s[:, None] >= s[None, :])).astype(np.float32)
    c["uf"] = (same & (s[:, None] > s[None, :])).astype(np.float32)
    c["ub"] = (same & (s[:, None] < s[None, :])).astype(np.float32)
    names = ["ident", "blk64", "permA", "trif", "trib", "uf", "ub"]
    return np.ascontiguousarray(np.stack([c[n] for n in names], 1)), names


CONST_NAMES = ["ident", "blk64", "permA", "trif", "trib", "uf", "ub"]
V_GMIX, V_GFFN, V_GAQ, V_GAK, V_GBQ, V_GBKV, V_GCO, V_BG = 0, 8, 16, 17, 18, 20, 22, 23
NVEC = 23 + 24


def pack_inputs(inp):
    out = {}
    cst, _ = consts_pack()
    out["cst"] = cst
    cosA, sinA, cosB, sinB = rope_tables()
    out["ropeA"] = np.ascontiguousarray(np.stack([cosA, sinA], 1))
    out["ropeB"] = np.ascontiguousarray(np.stack([cosB, sinB], 1))
    pk96 = rope_partner_cols(32)
    for l in range(DEPTH):
        w_in = np.asarray(inp["w_in"][l])
        vec = np.zeros((128, NVEC), np.float32)
        vec[:, V_GMIX:V_GMIX + 8] = np.asarray(inp["g_mix"][l]).reshape(8, 128).T
        vec[:, V_GFFN:V_GFFN + 8] = np.asarray(inp["g_ffn"][l]).reshape(8, 128).T
        vec[:, V_GAQ] = np.tile(np.asarray(inp["g_a_q"][l]), 2)
        vec[:, V_GAK] = np.tile(np.asarray(inp["g_a_k"][l]), 2)
        vec[:, V_GBQ:V_GBQ + 2] = np.asarray(inp["g_b_q"][l]).reshape(2, 128).T
        vec[:, V_GBKV:V_GBKV + 2] = np.asarray(inp["g_b_kv"][l]).reshape(2, 128).T
        vec[:, V_GCO] = np.asarray(inp["g_c_out"][l])
        vec[:, V_BG:V_BG + 24] = np.asarray(inp["b_gate"][l]).reshape(24, 128).T
        out[f"vec{l}"] = vec
        wa = []
        for g in range(2):
            kg = lhsT(w_in, O_AK + np.arange(g * 64, g * 64 + 64))
            zz = np.zeros_like(kg)
            wa.append(np.concatenate([lhsT(w_in, O_AQ + np.arange(2 * g * 128, (2 * g + 2) * 128)), kg, zz, zz, kg,
                                      lhsT(w_in, O_AV + np.arange(g * 64, g * 64 + 64))], axis=2))
        out[f"wA{l}"] = np.ascontiguousarray(np.stack(wa, 0))
        z64 = np.zeros((128, 8, 64), np.float32)
        kr = lhsT(w_in, O_BKR + np.arange(32))
        krp = lhsT(w_in, O_BKR + pk96)
        out[f"wB1{l}"] = np.ascontiguousarray(np.concatenate(
            [lhsT(w_in, O_BCQ + np.arange(256)), lhsT(w_in, O_BCKV + np.arange(256)), z64, kr, z64, krp], axis=2))
        wq = np.asarray(inp["w_b_q_up"][l])
        qcols, qpcols = [], []
        for h in range(8):
            base = h * 96
            qcols.append(base + np.arange(96))
            qpcols.append(np.concatenate([base + np.arange(64), base + 64 + pk96]))
        wq_l = np.stack([np.stack([lhsT(wq, qcols[h]), lhsT(wq, qpcols[h])], 2) for h in range(8)], 2)
        wkv = np.asarray(inp["w_b_kv_up"][l])
        wk_l = np.stack([lhsT(wkv, h * 128 + np.arange(64)) for h in range(8)], 2)
        wv_l = np.stack([lhsT(wkv, h * 128 + 64 + np.arange(64)) for h in range(8)], 2)
        out[f"wB2{l}"] = np.ascontiguousarray(np.concatenate(
            [wq_l.reshape(128, -1), wk_l.reshape(128, -1), wv_l.reshape(128, -1)], axis=1))
        wc = []
        for h in range(4):
            sl = np.arange(h * 128, (h + 1) * 128)
            a = np.stack([lhsT(w_in, O_CQ + sl), lhsT(w_in, O_CK + sl), lhsT(w_in, O_CG + sl)], 2)
            b = np.concatenate([lhsT(w_in, O_CK + sl), lhsT(w_in, O_CV + sl)], 2)
            wc.append(np.concatenate([a.reshape(128, -1), b.reshape(128, -1)], 1))
        out[f"wC{l}"] = np.ascontiguousarray(np.stack(wc, 0))
        z16 = np.zeros((128, 8, 16), np.float32)
        out[f"wCg{l}"] = np.ascontiguousarray(np.concatenate(
            [lhsT(w_in, O_CAF + np.arange(16)), z16, lhsT(w_in, O_CAB + np.arange(16)), z16], 2))
        up = np.zeros((128, 4, 2, 128), np.float32)
        waf = np.asarray(inp["w_c_af_up"][l]); wab = np.asarray(inp["w_c_ab_up"][l])
        baf = np.asarray(inp["b_c_af"][l]); bab = np.asarray(inp["b_c_ab"][l])
        for h in range(4):
            sl = slice(h * 128, (h + 1) * 128)
            up[0:16, h, 0] = waf[:, sl]
            up[48, h, 0] = baf[sl]
            up[32:48, h, 1] = wab[:, sl]
            up[48, h, 1] = bab[sl]
        out[f"wCup{l}"] = np.ascontiguousarray(up.reshape(128, -1))
        wps = [np.asarray(inp[n][l]) for n in ("w_pa", "w_pb", "w_pc")]
        wm = []
        for j in range(8):
            cj = np.arange(j * 128, (j + 1) * 128)
            for br in range(3):
                a = lhsT(wps[br], cj).reshape(128, -1)
                gts = lhsT(w_in, O_GATE + br * 1024 + cj).reshape(128, -1)
                wm.append(np.concatenate([a, gts], 1))
        out[f"wM{l}"] = np.ascontiguousarray(np.stack(wm, 0))
        wo = np.asarray(inp["w_out"][l])
        out[f"wO{l}"] = np.ascontiguousarray(np.stack([lhsT(wo, np.arange(j * 128, (j + 1) * 128)).reshape(128, -1) for j in range(8)], 0))

    def ffn_pack(wg, wu, wd):
        dff = wg.shape[1]
        ng = dff // 256
        blocks = []
        for g in range(ng):
            parts = []
            for w in (wg, wu):
                parts.append(np.stack([lhsT(w, np.arange((2 * g + c) * 128, (2 * g + c + 1) * 128)) for c in range(2)], 1).reshape(128, -1))
            dd = np.stack([np.asarray(wd)[(2 * g + c) * 128:(2 * g + c + 1) * 128, :] for c in range(2)], 1).reshape(128, -1)
            parts.append(dd)
            blocks.append(np.concatenate(parts, 1))
        return np.ascontiguousarray(np.stack(blocks, 0))
    out["wF0"] = ffn_pack(np.asarray(inp["w_ff_gate"][0]), np.asarray(inp["w_ff_up"][0]), np.asarray(inp["w_ff_down"][0]))
    we = [ffn_pack(np.asarray(inp["w_e_gate"][0][e]), np.asarray(inp["w_e_up"][0][e]), np.asarray(inp["w_e_down"][0][e])) for e in range(NEXP)]
    out["wE"] = np.ascontiguousarray(np.concatenate(we, 0))
    out["wR"] = lhsT(np.asarray(inp["w_router"][0]), None)
    gf = np.asarray(inp["g_final"]).reshape(8, 128).T
    out["gfin"] = np.ascontiguousarray(gf)
    return out


class Ctx:
    pass


def build_program(shapes, stop_after=None, dbg=None):
    nc = bass.Bass("TRN2", target_bir_lowering=False)
    dr = {}
    for name, shp in shapes.items():
        dr[name] = nc.dram_tensor(name, list(shp), F32, kind="ExternalInput").ap()
    y_out = nc.dram_tensor("y", [T, D], F32, kind="ExternalOutput").ap()
    dbg_out = {}
    if dbg:
        for name, shp in dbg.items():
            dbg_out[name] = nc.dram_tensor("dbg_" + name, list(shp), F32, kind="ExternalOutput").ap()
    es = ExitStack()
    with es:
        S = Sched(nc, es)
        uid = [0]

        def sbt(stack, name, shape, dt):
            uid[0] += 1
            return stack.enter_context(nc.sbuf_tensor(f"{name}_{uid[0]}", list(shape), dt))

        P = lambda name, shape, dt=F32: sbt(es, name, shape, dt)
        ps = [es.enter_context(nc.psum_tensor(f"ps{i}", [128, 512], F32)) for i in range(8)]
        rr = {}

        def psb(pool, banks):
            i = rr.get(pool, 0)
            rr[pool] = i + 1
            b = banks[i % len(banks)]
            return ps[b], ("ps", b)

        ALLB = [0, 1, 2, 3, 4, 5, 6, 7]
        PJ = [2, 3, 4, 5, 6, 7]

        xT = P("xT", [128, 8, T])
        hT = P("hT", [128, 8, T], BF16)
        cst = P("cst", [128, 7, 128])
        cstb = P("cstb", [128, 7, 128], BF16)
        vec = [P(f"vec{l}", [128, NVEC]) for l in range(DEPTH)]
        gfin = P("gfin", [128, 8])
        epsb = P("epsb", [128, 1])
        lnsc = P("lnsc", [128, 1])
        onesm = P("onesm", [128, 128])
        ones256 = P("ones256", [128, 128], BF16)
        ones128 = P("ones128", [128, 128])
        ones1 = P("ones1", [128, 128])
        CI = {n: i for i, n in enumerate(CONST_NAMES)}
        ident = cst[:, CI["ident"], :]

        def cload(eng, out, in_, key, sem="const"):
            S.op(eng, lambda e: e.dma_start(out=out, in_=in_), writes=[key], dma=sem, dma_batch=True)

        cload("sp", cst[:], dr["cst"][:, :, :], "cst")
        for l in range(DEPTH):
            cload("sp", vec[l][:], dr[f"vec{l}"][:, :], ("vec", l))
        cload("sp", gfin[:], dr["gfin"][:, :], "gfin")
        S.op("dve", lambda e: e.tensor_copy(out=cstb[:], in_=cst[:]), reads=["cst"], writes=["cstb"])
        S.op("dve", lambda e: e.memset(epsb[:], EPS), writes=["eps"])
        S.op("dve", lambda e: e.memset(lnsc[:], float(np.log(128.0 ** -0.5))), writes=["lnsc"])
        S.op("dve", lambda e: e.memset(onesm[:], 1.0 / 1024), writes=["onesm"])
        S.op("dve", lambda e: e.memset(ones256[:], 1.0 / 256), writes=["ones256"])
        S.op("dve", lambda e: e.memset(ones128[:], 1.0 / 128), writes=["ones128"])
        S.op("dve", lambda e: e.memset(ones1[:], 1.0), writes=["ones1"])

        def mm(out, pairs, reads, writes):
            def fn(e):
                n = len(pairs)
                ins = None
                for i, (l_, r_) in enumerate(pairs):
                    ins = e.matmul(out, l_, r_, start=(i == 0), stop=(i == n - 1))
                return ins
            return S.op("pe", fn, reads=reads, writes=writes)

        def xk(c, tb):
            return ("xT", c, tb)

        def hk(tb):
            return ("hT", tb)

        def TB(tb):
            return slice(tb * 512, (tb + 1) * 512)

        def dump(name, src_ap, key_reads=None):
            if name in dbg_out:
                S.barrier()
                if src_ap.dtype != F32:
                    with ExitStack() as ph2:
                        tmp = sbt(ph2, "dbgtmp", list(src_ap.shape), F32)
                        S.op("dve", lambda e: e.tensor_copy(out=tmp[:], in_=src_ap), writes=["dbgtmp"])
                        S.op("sp", lambda e: e.dma_start(out=dbg_out[name], in_=tmp[:]), reads=["dbgtmp"], dma="dbg")
                        S.barrier()
                else:
                    S.op("sp", lambda e: e.dma_start(out=dbg_out[name], in_=src_ap), dma="dbg")
                    S.barrier()

        with ExitStack() as ph:
            stage = [sbt(ph, "stg", [128, D], F32) for _ in range(2)]
            for tt in range(NT):
                st = stage[tt % 2]
                S.op("sp", lambda e, st=st, tt=tt: e.dma_start(out=st[:], in_=dr["x"][tt * 128:(tt + 1) * 128, :]),
                     writes=[("stg", tt % 2)], dma=f"xin{tt % 2}")
                for half in range(2):
                    p, pk = psb("all", ALLB)

                    def tr(e, st=st, half=half, p=p):
                        ins = None
                        for j in range(4):
                            c = half * 4 + j
                            ins = e.transpose(p[:, j * 128:(j + 1) * 128], st[:, c * 128:(c + 1) * 128], ident)
                        return ins
                    S.op("pe", tr, reads=[("stg", tt % 2), "cst"], writes=[pk])
                    S.op("dve" if half == 0 else "act",
                         (lambda e, half=half, p=p, tt=tt: e.tensor_copy(out=xT[:, half * 4:(half + 1) * 4, tt * 128:(tt + 1) * 128],
                                                                        in_=p[:].rearrange("p (j t) -> p j t", j=4))) if half == 0 else
                         (lambda e, half=half, p=p, tt=tt: e.copy(out=xT[:, half * 4:(half + 1) * 4, tt * 128:(tt + 1) * 128],
                                                                  in_=p[:].rearrange("p (j t) -> p j t", j=4))),
                         reads=[pk], writes=[xk(c, tt // 4) for c in range(half * 4, half * 4 + 4)])
            S.barrier()

        def rmsnorm_to_h(gcol_ap_fn, gkey, router=None):
            with ExitStack() as ph:
                sq = [sbt(ph, "sq", [128, 512], F32) for _ in range(2)]
                rs = [sbt(ph, "rs", [128, 512], F32) for _ in range(2)]
                if router is not None:
                    h32 = sbt(ph, "h32", [128, 8, 512], F32)
                for tb in range(NB):
                    p, pk = psb("n7", [0, 1, 2, 3, 4, 5, 6])
                    for c in range(8):
                        s_ = sq[c % 2]
                        S.op("act", lambda e, s_=s_, c=c, tb=tb: e.activation(out=s_[:], in_=xT[:, c, TB(tb)], func=AF.Square),
                             reads=[xk(c, tb)], writes=[("sq", c % 2)])
                        S.op("pe", lambda e, s_=s_, c=c, p=p: e.matmul(p[:], onesm[:], s_[:], start=(c == 0), stop=(c == 7)),
                             reads=[("sq", c % 2), "onesm"], writes=[pk])
                    r_ = rs[tb % 2]
                    S.op("act", lambda e, p=p, r_=r_: e.activation(out=r_[:], in_=p[:], func=AF.Ln, bias=epsb[:, 0:1], scale=1.0),
                         reads=[pk, "eps"], writes=[("rs", tb % 2)])
                    S.op("act", lambda e, r_=r_: e.activation(out=r_[:], in_=r_[:], func=AF.Exp, scale=-0.5), reads=[("rs", tb % 2)], writes=[("rs", tb % 2)])
                    for c in range(8):
                        S.op("dve",
                             lambda e, c=c, tb=tb, r_=r_: e.scalar_tensor_tensor(out=hT[:, c, TB(tb)], in0=xT[:, c, TB(tb)], scalar=gcol_ap_fn(c),
                                                                               in1=r_[:], op0=ALU.mult, op1=ALU.mult),
                             reads=[xk(c, tb), ("rs", tb % 2), gkey], writes=[("hT", c, tb)])
                    if router is not None:
                        for c in range(8):
                            S.op("dve", lambda e, c=c, tb=tb, r_=r_: e.scalar_tensor_tensor(out=h32[:, c, :], in0=xT[:, c, TB(tb)], scalar=gcol_ap_fn(c),
                                                                                        in1=r_[:], op0=ALU.mult, op1=ALU.mult),
                                 reads=[xk(c, tb), ("rs", tb % 2), gkey], writes=[("h32", c)])
                        router(tb, h32)
                S.barrier()

        def h_reads(tb):
            return [("hT", c, tb) for c in range(8)]

        def h_reads_all():
            return [("hT", c, tb) for c in range(8) for tb in range(NB)]

        C = Ctx()
        C.__dict__.update(locals())
        for l in range(DEPTH):
            g0 = V_GMIX
            rmsnorm_to_h(lambda c, l=l: vec[l][:, V_GMIX + c:V_GMIX + c + 1], ("vec", l))
            dump(f"h{l}", hT[:], None)
            if stop_after == f"norm{l}":
                break
            stop = False
            with ExitStack() as mixph:
                C.yT = [sbt(mixph, f"y{b}T", [128, 4, T], BF16) for b in range(3)]
                for nm, fn in (("B", mixer_B), ("A", mixer_A), ("C", mixer_C), ("M", merge_out)):
                    if nm in SKIP:
                        continue
                    fn(C, l)
                    if stop_after == f"{nm}{l}":
                        stop = True
                        break
                if stop:
                    S.barrier()
            if stop:
                break
            if l == 0:
                rmsnorm_to_h(lambda c, l=l: vec[l][:, V_GFFN + c:V_GFFN + c + 1], ("vec", l))
                ffn_dense(C)
            else:
                moe(C, l)
            if stop_after == f"F{l}":
                break
        final_out(C)
        waits = [("sp", n) for n in S.dma_sems if n.startswith("yout") or n == "dbg"]
        S.emit(final_waits=waits)
    return nc


def attn_core(C, k_ap, q_ap, v_ap, par, out_ap, scale, kkeys, qkeys, vkeys, okey_fn, PT, rec):
    S, ps, psb = C.S, C.ps, C.psb
    vr = slice(par * 64, par * 64 + 64)
    sr = slice((1 - par) * 64, (1 - par) * 64 + 64)
    for qb in range(NB):
        acc, acck = psb("acc", [0, 1])
        pend = []

        def issue_pv(item, last):
            kt, pt_i = item
            S.op("pe", lambda e, kt=kt, pt_i=pt_i, acc=acc: e.matmul(acc[:], v_ap(kt), PT[pt_i][:], start=(kt == 0), stop=(kt == NT - 1)),
                 reads=[("PT", pt_i)] + vkeys, writes=[acck])
        for kt in range(NT):
            sp_, spk = psb("s", [2, 3, 4])
            S.op("pe", lambda e, kt=kt, qb=qb, sp_=sp_: e.matmul(sp_[:], k_ap(kt), q_ap(qb), start=True, stop=True),
                 reads=kkeys + qkeys, writes=[spk])
            pt_i = C.pt_rr[0] % len(PT)
            C.pt_rr[0] += 1
            S.op("act", lambda e, sp_=sp_, pt_i=pt_i: e.activation(out=PT[pt_i][:], in_=sp_[:], func=AF.Exp, scale=scale),
                 reads=[spk], writes=[("PT", pt_i)])
            pend.append((kt, pt_i))
            if len(pend) > 1:
                issue_pv(pend.pop(0), False)
        while pend:
            issue_pv(pend.pop(0), True)
        r_i = C.pt_rr[1] % len(rec)
        C.pt_rr[1] += 1
        S.op("dve", lambda e, acc=acc, r_i=r_i: e.reciprocal(out=rec[r_i][vr, :], in_=acc[sr, :]),
             reads=[acck], writes=[("rec", r_i)])
        S.op("dve", lambda e, acc=acc, r_i=r_i, qb=qb: e.tensor_tensor(out=out_ap(qb), in0=acc[vr, :], in1=rec[r_i][vr, :], op=ALU.mult),
             reads=[acck, ("rec", r_i)], writes=[okey_fn(qb)])


def mixer_A(C, l):
    S, nc, dr, hT, vec, cstb, psb, sbt = C.S, C.nc, C.dr, C.hT, C.vec, C.cstb, C.psb, C.sbt
    CI, TB, epsb = C.CI, C.TB, C.epsb
    yaT = C.yT[0]
    PJ = C.PJ
    with ExitStack() as ph:
        wA = sbt(ph, "wA", [128, 8, 576], BF16)
        qT = sbt(ph, "qTa", [128, 2, T], BF16)
        kT = sbt(ph, "kTa", [128, 2, T], BF16)
        Va = [sbt(ph, "Va", [128, NT, 128], BF16) for _ in range(2)]
        PT = [sbt(ph, "PT", [128, 512], BF16) for _ in range(3)]
        rec = [sbt(ph, "rec", [128, 512], F32) for _ in range(1)]
        rope = [sbt(ph, "ropeA", [128, 2, 512], F32) for _ in range(1)]
        sqb = [sbt(ph, "sqb", [128, 512], BF16) for _ in range(1)]
        ub = [sbt(ph, "ub", [128, 512], BF16) for _ in range(1)]
        rsb = [sbt(ph, "rsb", [128, 512], F32) for _ in range(1)]
        t1 = [sbt(ph, "t1", [128, 512], F32) for _ in range(1)]
        t2 = [sbt(ph, "t2", [128, 512], F32) for _ in range(1)]
        onesA = sbt(ph, "onesA", [128, 512], F32)
        S.op("dve", lambda e: e.memset(onesA[:], 1.0), writes=["onesA"])
        C.pt_rr = [0, 0]
        blk = cstb[:, CI["blk64"], :]
        perm = cstb[:, CI["permA"], :]
        for g in range(2):
            S.op("pool", lambda e, g=g: e.dma_start(out=wA[:], in_=dr[f"wA{l}"][g]), writes=["wA"], dma="wA")
            S.op("pool", lambda e: e.memset(Va[0][:], 1.0), writes=["Va"])
            S.op("pool", lambda e: e.memset(Va[1][:], 1.0), writes=["Va1"])
            bi = 0
            for tb in range(NB):
                rp = rope[0]
                S.op("sp", lambda e, rp=rp, tb=tb: e.dma_start(out=rp[:], in_=dr["ropeA"][:, :, TB(tb)]),
                     writes=[("ropeA", 0)], dma="ropeA0")
                for blkid in range(4):
                    col0 = blkid * 128
                    gcol = vec[l][:, V_GAQ:V_GAQ + 1] if blkid < 2 else vec[l][:, V_GAK:V_GAK + 1]
                    i = 0
                    p1, p1k = psb("pj", PJ)
                    C.mm(p1[:], [(wA[:, k, col0:col0 + 128], hT[:, k, TB(tb)]) for k in range(8)],
                         reads=["wA"] + C.h_reads(tb), writes=[p1k])
                    S.op("act", lambda e, p1=p1, i=i: e.activation(out=sqb[i][:], in_=p1[:], func=AF.Square),
                         reads=[p1k], writes=[("sqb", i)])
                    S.op("dve", lambda e, p1=p1, i=i, gcol=gcol: e.scalar_tensor_tensor(out=ub[i][:], in0=p1[:], scalar=gcol, in1=onesA[:], op0=ALU.mult, op1=ALU.mult),
                         reads=[p1k, ("vec", l), "onesA"], writes=[("ub", i)])
                    p2, p2k = psb("pj", PJ)
                    C.mm(p2[:], [(blk, sqb[i][:])], reads=["cstb", ("sqb", i)], writes=[p2k])
                    p3, p3k = psb("pj", PJ)
                    C.mm(p3[:], [(perm, ub[i][:])], reads=["cstb", ("ub", i)], writes=[p3k])
                    S.op("act", lambda e, p2=p2, i=i: e.activation(out=rsb[i][:], in_=p2[:], func=AF.Ln, bias=epsb[:, 0:1], scale=1.0),
                         reads=[p2k, "eps"], writes=[("rsb", i)])
                    S.op("act", lambda e, i=i: e.activation(out=rsb[i][:], in_=rsb[i][:], func=AF.Exp, scale=-0.5), reads=[("rsb", i)], writes=[("rsb", i)])
                    S.op("dve", lambda e, i=i, rp=rp: e.tensor_tensor(out=t1[i][:], in0=ub[i][:], in1=rp[:, 0, :], op=ALU.mult),
                         reads=[("ub", i), ("ropeA", 0)], writes=[("t1", i)])
                    S.op("dve", lambda e, i=i, rp=rp, p3=p3: e.tensor_tensor(out=t2[i][:], in0=p3[:], in1=rp[:, 1, :], op=ALU.mult),
                         reads=[p3k, ("ropeA", 0)], writes=[("t2", i)])
                    S.op("pool", lambda e, i=i: e.tensor_tensor(out=t1[i][:], in0=t1[i][:], in1=t2[i][:], op=ALU.add),
                         reads=[("t1", i), ("t2", i)], writes=[("t1", i)])
                    dst = qT[:, blkid, TB(tb)] if blkid < 2 else kT[:, blkid - 2, TB(tb)]
                    dkey = ("qTa", blkid, tb) if blkid < 2 else ("kTa", blkid - 2, tb)
                    S.op("dve", lambda e, i=i, dst=dst: e.tensor_tensor(out=dst, in0=t1[i][:], in1=rsb[i][:], op=ALU.mult),
                         reads=[("t1", i), ("rsb", i)], writes=[dkey])
            for tg in range(4):
                pv, pvk = psb("pj", PJ)

                def vfn(e, tg=tg, pv=pv):
                    ins = None
                    for j in range(4):
                        tt = tg * 4 + j
                        for k in range(8):
                            ins = e.matmul(pv[:, j * 64:(j + 1) * 64], hT[:, k, tt * 128:(tt + 1) * 128], wA[:, k, 512:576],
                                           start=(k == 0), stop=(k == 7))
                    return ins
                S.op("pe", vfn, reads=["wA"] + C.h_reads(tg), writes=[pvk])
                S.op("dve", lambda e, tg=tg, pv=pv: e.tensor_copy(out=Va[0][:, tg * 4:(tg + 1) * 4, 0:64],
                                                                  in_=pv[:, 0:256].rearrange("p (j d) -> p j d", j=4)),
                     reads=[pvk, "Va"], writes=[("Vav", tg)])
                S.op("act", lambda e, tg=tg, pv=pv: e.copy(out=Va[1][:, tg * 4:(tg + 1) * 4, 64:128],
                                                          in_=pv[:, 0:256].rearrange("p (j d) -> p j d", j=4)),
                     reads=[pvk, "Va1"], writes=[("Vav1", tg)])
            vkeys = [("Vav", tg) for tg in range(4)] + ["Va", "Va1"] + [("Vav1", tg) for tg in range(4)]
            for hl in HLS:
                cl, par = hl // 2, hl % 2
                pr = slice(par * 64, par * 64 + 64)
                chunk = 2 * g + cl
                attn_core(C,
                          k_ap=lambda kt, par=par: kT[:, par, kt * 128:(kt + 1) * 128],
                          q_ap=lambda qb, cl=cl: qT[:, cl, TB(qb)],
                          v_ap=(lambda kt: Va[0][:, kt, :]) if par == 0 else (lambda kt: Va[1][:, kt, :]),
                          par=par,
                          out_ap=lambda qb, pr=pr, chunk=chunk: yaT[pr, chunk, TB(qb)],
                          scale=64.0 ** -0.5,
                          kkeys=[("kTa", par, tb) for tb in range(NB)], qkeys=[("qTa", cl, tb) for tb in range(NB)], vkeys=vkeys,
                          okey_fn=lambda qb, chunk=chunk, par=par: ("y0T", chunk, par, qb), PT=PT, rec=rec)
        S.barrier()
    C.dump(f"ya{l}", yaT[:])


def mixer_B(C, l):
    S, nc, dr, hT, vec, cstb, psb, sbt = C.S, C.nc, C.dr, C.hT, C.vec, C.cstb, C.psb, C.sbt
    CI, TB, epsb, ones256 = C.CI, C.TB, C.epsb, C.ones256
    ybT = C.yT[1]
    PJ = C.PJ
    with ExitStack() as ph:
        cqT = sbt(ph, "cqT", [128, 2, T], BF16)
        ckvT = sbt(ph, "ckvT", [128, 2, T], BF16)
        kh = sbt(ph, "kh", [128, T], BF16)
        rope = [sbt(ph, "ropeB", [128, 2, 512], F32) for _ in range(2)]
        t1 = [sbt(ph, "t1b", [128, 512], F32) for _ in range(1)]
        t2 = [sbt(ph, "t2b", [128, 512], F32) for _ in range(1)]
        with ExitStack() as ph1:
            w1 = sbt(ph1, "wB1", [128, 8, 704], BF16)
            sqb = [sbt(ph1, "sqbB", [128, 512], BF16) for _ in range(4)]
            rsb = [sbt(ph1, "rsbB", [128, 512], F32) for _ in range(2)]
            S.op("pool", lambda e: e.dma_start(out=w1[:], in_=dr[f"wB1{l}"][:, :, :]), writes=["wB1"], dma="wB1")
            ni = 0
            for tb in range(NB):
                rp = rope[0]
                S.op("sp", lambda e, rp=rp, tb=tb: e.dma_start(out=rp[:], in_=dr["ropeB"][:, :, TB(tb)]),
                     writes=[("ropeB", 0)], dma="ropeB0")
                for which in range(2):
                    dstT = cqT if which == 0 else ckvT
                    gbase = V_GBQ if which == 0 else V_GBKV
                    pp = []
                    for c in range(2):
                        p1, p1k = psb("pj", PJ)
                        col0 = which * 256 + c * 128
                        C.mm(p1[:], [(w1[:, k, col0:col0 + 128], hT[:, k, TB(tb)]) for k in range(8)],
                             reads=["wB1"] + C.h_reads(tb), writes=[p1k])
                        si = (ni * 2 + c) % 4
                        S.op("act", lambda e, p1=p1, si=si: e.activation(out=sqb[si][:], in_=p1[:], func=AF.Square),
                             reads=[p1k], writes=[("sqbB", si)])
                        pp.append((p1, p1k, si))
                    p2, p2k = psb("pj", PJ)
                    C.mm(p2[:], [(ones256[:], sqb[pp[0][2]][:]), (ones256[:], sqb[pp[1][2]][:])],
                         reads=["ones256", ("sqbB", pp[0][2]), ("sqbB", pp[1][2])], writes=[p2k])
                    ri = ni % 2
                    ni += 1
                    S.op("act", lambda e, p2=p2, ri=ri: e.activation(out=rsb[ri][:], in_=p2[:], func=AF.Ln, bias=epsb[:, 0:1], scale=1.0),
                         reads=[p2k, "eps"], writes=[("rsbB", ri)])
                    S.op("act", lambda e, ri=ri: e.activation(out=rsb[ri][:], in_=rsb[ri][:], func=AF.Exp, scale=-0.5), reads=[("rsbB", ri)], writes=[("rsbB", ri)])
                    for c in range(2):
                        p1, p1k, si = pp[c]
                        S.op("dve", lambda e, p1=p1, c=c, ri=ri, dstT=dstT, gbase=gbase, tb=tb: e.scalar_tensor_tensor(
                            out=dstT[:, c, TB(tb)], in0=p1[:], scalar=vec[l][:, gbase + c:gbase + c + 1], in1=rsb[ri][:], op0=ALU.mult, op1=ALU.mult),
                            reads=[p1k, ("rsbB", ri), ("vec", l)], writes=[("lat", which, c, tb)])
                pa, pak = psb("pj", PJ)
                C.mm(pa[0:96, :], [(w1[:, k, 512:608], hT[:, k, TB(tb)]) for k in range(8)], reads=["wB1"] + C.h_reads(tb), writes=[pak])
                pb, pbk = psb("pj", PJ)
                C.mm(pb[0:96, :], [(w1[:, k, 608:704], hT[:, k, TB(tb)]) for k in range(8)], reads=["wB1"] + C.h_reads(tb), writes=[pbk])
                i = 0
                rr_ = slice(64, 96)
                S.op("dve", lambda e, pa=pa, rp=rp, i=i: e.tensor_tensor(out=t1[i][rr_, :], in0=pa[rr_, :], in1=rp[rr_, 0, :], op=ALU.mult),
                     reads=[pak, ("ropeB", 0)], writes=[("t1b", i)])
                S.op("dve", lambda e, pb=pb, rp=rp, i=i: e.tensor_tensor(out=t2[i][rr_, :], in0=pb[rr_, :], in1=rp[rr_, 1, :], op=ALU.mult),
                     reads=[pbk, ("ropeB", 0)], writes=[("t2b", i)])
                S.op("pool", lambda e, i=i, tb=tb: e.tensor_tensor(out=kh[rr_, TB(tb)], in0=t1[i][rr_, :], in1=t2[i][rr_, :], op=ALU.add),
                     reads=[("t1b", i), ("t2b", i)], writes=[("krT", tb)])
            S.barrier()
        C.dump(f"cq{l}", cqT[:])
        w2 = sbt(ph, "wB2", [128, 5120], BF16)
        qh = sbt(ph, "qh", [128, T], BF16)
        Vb = sbt(ph, "Vb", [128, NT, 128], BF16)
        PT = [sbt(ph, "PTb", [128, 512], BF16) for _ in range(2)]
        rec = [sbt(ph, "recb", [128, 512], F32) for _ in range(1)]
        C.pt_rr = [0, 0]
        S.op("pool", lambda e: e.dma_start(out=w2[:], in_=dr[f"wB2{l}"][:, :]), writes=["wB2"], dma="wB2")
        wq = w2[:, 0:3072].rearrange("p (k h v m) -> p k h v m", k=2, h=8, v=2)
        wk = w2[:, 3072:4096].rearrange("p (k h m) -> p k h m", k=2, h=8)
        wv = w2[:, 4096:5120].rearrange("p (k h m) -> p k h m", k=2, h=8)
        latq = [("lat", 0, c, tb) for c in range(2) for tb in range(NB)]
        latkv = [("lat", 1, c, tb) for c in range(2) for tb in range(NB)]
        for h in range(8):
            par = h % 2
            for tb in range(NB):
                ri_ = (h * NB + tb) % 2
                rp = rope[ri_]
                S.op("sp", lambda e, rp=rp, tb=tb: e.dma_start(out=rp[:], in_=dr["ropeB"][:, :, TB(tb)]),
                     writes=[("ropeB", ri_)], dma=f"ropeB{ri_}")
                pa, pak = psb("pj", PJ)
                C.mm(pa[0:96, :], [(wq[:, k, h, 0, :], cqT[:, k, TB(tb)]) for k in range(2)], reads=["wB2"] + latq, writes=[pak])
                pb, pbk = psb("pj", PJ)
                C.mm(pb[0:96, :], [(wq[:, k, h, 1, :], cqT[:, k, TB(tb)]) for k in range(2)], reads=["wB2"] + latq, writes=[pbk])
                i = 0
                r96 = slice(0, 96)
                S.op("dve", lambda e, pa=pa, rp=rp, i=i: e.tensor_tensor(out=t1[i][r96, :], in0=pa[r96, :], in1=rp[r96, 0, :], op=ALU.mult),
                     reads=[pak, ("ropeB", ri_)], writes=[("t1b", i)])
                S.op("dve", lambda e, pb=pb, rp=rp, i=i: e.tensor_tensor(out=t2[i][r96, :], in0=pb[r96, :], in1=rp[r96, 1, :], op=ALU.mult),
                     reads=[pbk, ("ropeB", ri_)], writes=[("t2b", i)])
                S.op("pool", lambda e, i=i, tb=tb: e.tensor_tensor(out=qh[r96, TB(tb)], in0=t1[i][r96, :], in1=t2[i][r96, :], op=ALU.add),
                     reads=[("t1b", i), ("t2b", i)], writes=[("qh", tb)])
                pc, pck = psb("pj", PJ)
                C.mm(pc[0:64, :], [(wk[:, k, h, :], ckvT[:, k, TB(tb)]) for k in range(2)], reads=["wB2"] + latkv, writes=[pck])
                S.op("act", lambda e, pc=pc, tb=tb: e.copy(out=kh[0:64, TB(tb)], in_=pc[0:64, :]), reads=[pck], writes=[("kh", tb)])
            voff = 0 if par == 0 else 64
            S.op("pool", lambda e, ooff=64 - voff: e.memset(Vb[:, :, ooff:ooff + 64], 1.0), writes=["Vb1"])
            for tg in range(4):
                pv, pvk = psb("pj", PJ)

                def vfn(e, tg=tg, pv=pv, h=h):
                    ins = None
                    for j in range(4):
                        tt = tg * 4 + j
                        for k in range(2):
                            ins = e.matmul(pv[:, j * 64:(j + 1) * 64], ckvT[:, k, tt * 128:(tt + 1) * 128], wv[:, k, h, :],
                                           start=(k == 0), stop=(k == 1))
                    return ins
                S.op("pe", vfn, reads=["wB2"] + latkv, writes=[pvk])
                S.op("dve", lambda e, tg=tg, pv=pv, voff=voff: e.tensor_copy(out=Vb[:, tg * 4:(tg + 1) * 4, voff:voff + 64],
                                                                            in_=pv[:, 0:256].rearrange("p (j d) -> p j d", j=4)),
                     reads=[pvk], writes=[("Vbv", tg)])
            pr = slice(par * 64, par * 64 + 64)
            chunk = h // 2
            attn_core(C,
                      k_ap=lambda kt: kh[0:96, kt * 128:(kt + 1) * 128],
                      q_ap=lambda qb: qh[0:96, TB(qb)],
                      v_ap=lambda kt: Vb[:, kt, :],
                      par=par,
                      out_ap=lambda qb, pr=pr, chunk=chunk: ybT[pr, chunk, TB(qb)],
                      scale=96.0 ** -0.5,
                      kkeys=[("kh", tb) for tb in range(NB)], qkeys=[("qh", tb) for tb in range(NB)],
                      vkeys=[("Vbv", tg) for tg in range(4)] + ["Vb1"],
                      okey_fn=lambda qb, chunk=chunk, par=par: ("y1T", chunk, par, qb), PT=PT, rec=rec)
        S.barrier()
    C.dump(f"yb{l}", ybT[:])


def mixer_C(C, l):
    S, nc, dr, hT, vec, cst, cstb, sbt, ps = C.S, C.nc, C.dr, C.hT, C.vec, C.cst, C.cstb, C.sbt, C.ps
    CI, TB, epsb, ones128, lnsc = C.CI, C.TB, C.epsb, C.ones128, C.lnsc
    ycT = C.yT[2]
    with ExitStack() as ph:
        cT = sbt(ph, "cT", [128, T], BF16)
        with ExitStack() as ph0:
            wg_ = sbt(ph0, "wCg", [128, 8, 64], BF16)
            S.op("pool", lambda e: e.dma_start(out=wg_[:], in_=dr[f"wCg{l}"][:, :, :]), writes=["wCg"], dma="wCg")
            S.op("dve", lambda e: e.memset(cT[:], 1.0), writes=["cT"])
            for tb in range(NB):
                p, pk = C.psb("all", C.ALLB)
                C.mm(p[0:64, :], [(wg_[:, k, :], hT[:, k, TB(tb)]) for k in range(8)], reads=["wCg"] + C.h_reads(tb), writes=[pk])
                S.op("act", lambda e, p=p, tb=tb: e.copy(out=cT[0:48, TB(tb)], in_=p[0:48, :]), reads=[pk, "cT"], writes=[("cTv", tb)])
            S.barrier()
        wup = sbt(ph, "wup", [128, 2, 128], BF16)
        wh = sbt(ph, "wCh", [128, 5120], BF16)
        qT = sbt(ph, "qTc", [128, T], BF16)
        kT = sbt(ph, "kTc", [128, T], BF16)
        ktok = sbt(ph, "ktok", [128, NT, 128], BF16)
        vtok = sbt(ph, "vtok", [128, NT, 128], BF16)
        oT = sbt(ph, "oT", [128, T], F32)
        sp_ = sbt(ph, "sp", [128, 4, 128], F32)
        e1 = sp_[:].rearrange("p a b -> p (a b)")
        eb = sbt(ph, "eb", [128, 512], F32)
        enb = eb
        eg = sbt(ph, "eg", [128, 4, 128], F32)
        kt_ = sbt(ph, "kt", [128, 512], BF16)
        qt2 = [sbt(ph, "qt", [128, 512], BF16) for _ in range(2)]
        kd2 = [[sbt(ph, "kd", [128, 4, 128], BF16) for _ in range(2)] for _ in range(2)]
        ms2 = [sbt(ph, "ms", [128, 4, 128], BF16) for _ in range(2)]
        dec2 = [sbt(ph, "dec", [128, 8], F32) for _ in range(2)]
        Sst = [sbt(ph, "Sst", [128, 128], F32) for _ in range(2)]
        Sbf = [sbt(ph, "Sbf", [128, 128], BF16) for _ in range(4)]
        sqo = sp_[:].rearrange("p a b -> p (a b)")
        rso = eb
        sgo = eg[:].rearrange("p a b -> p (a b)")
        ckeys = []
        tri = {0: cst[:, CI["trif"], :], 1: cst[:, CI["trib"], :]}
        trib16 = {0: cstb[:, CI["trif"], :], 1: cstb[:, CI["trib"], :]}
        uu = {0: cst[:, CI["uf"], :], 1: cst[:, CI["ub"], :]}
        wl = wh[:, 0:3072].rearrange("p (k v m) -> p k v m", k=8, v=3)
        wr = wh[:, 3072:5120].rearrange("p (k m) -> p k m", k=8)
        si = [0]
        for h in range(4):
            S.op("pool", lambda e, h=h: e.dma_start(out=wh[:], in_=dr[f"wC{l}"][h]), writes=["wCh"], dma="wCh")
            S.op("pool", lambda e, h=h: e.dma_start(out=wup[:].rearrange("p b c -> p (b c)"), in_=dr[f"wCup{l}"][:, h * 256:(h + 1) * 256]),
                 writes=["wup"], dma="wCup")
            for gi in range(NB):
                for which, dst, dk in ((0, qT, "qTc"), (1, kT, "kTc")):
                    p, pk = C.psb("all", C.ALLB)
                    C.mm(p[:], [(wl[:, k, which, :], hT[:, k, TB(gi)]) for k in range(8)], reads=["wCh"] + C.h_reads(gi), writes=[pk])
                    S.op("act", lambda e, p=p, dst=dst, gi=gi: e.copy(out=dst[:, TB(gi)], in_=p[:]), reads=[pk], writes=[(dk, gi)])
                for half in range(2):
                    p, pk = C.psb("all", C.ALLB)

                    def kvfn(e, p=p, gi=gi, half=half):
                        ins = None
                        for j in range(2):
                            tt = gi * 4 + half * 2 + j
                            for k in range(8):
                                ins = e.matmul(p[:, j * 256:(j + 1) * 256], hT[:, k, tt * 128:(tt + 1) * 128], wr[:, k, :], start=(k == 0), stop=(k == 7))
                        return ins
                    S.op("pe", kvfn, reads=["wCh"] + C.h_reads(gi), writes=[pk])
                    t0 = gi * 4 + half * 2
                    pv = p[:].rearrange("p (j w) -> p j w", j=2)
                    S.op("dve", lambda e, pv=pv, t0=t0: e.tensor_copy(out=ktok[:, t0:t0 + 2, :], in_=pv[:, :, 0:128]), reads=[pk], writes=[("ktok", t0 // 2)])
                    S.op("act", lambda e, pv=pv, t0=t0: e.copy(out=vtok[:, t0:t0 + 2, :], in_=pv[:, :, 128:256]), reads=[pk], writes=[("vtok", t0 // 2)])
            def prologue(d, gi, pb_, part):
                qt, kd, ms, dec = qt2[pb_], kd2[pb_], ms2[pb_], dec2[pb_]
                pspk = ("ps", 0)
                if part == 0:
                    def spfn(e, gi=gi, d=d):
                        ins = None
                        for j in range(4):
                            tt = gi * 4 + j
                            ins = e.matmul(ps[0][:, j * 128:(j + 1) * 128], cT[:, tt * 128:(tt + 1) * 128], wup[:, d, :], start=True, stop=True)
                        return ins
                    S.op("pe", spfn, reads=ckeys + ["wup"], writes=[pspk])
                    S.op("act", lambda e: e.activation(out=e1, in_=ps[0][:], func=AF.Exp, scale=-1.0), reads=[pspk], writes=["sp"])
                    S.op("act", lambda e: e.activation(out=sp_[:].rearrange("p a b -> p (a b)"), in_=e1, func=AF.Ln, bias=C.ones1[:, 0:1], scale=1.0),
                         reads=["sp", "ones1"], writes=["sp"])

                if part == 1:
                    def cfn(e, d=d):
                        ins = None
                        for j in range(4):
                            ins = e.matmul(ps[1][:, j * 128:(j + 1) * 128], sp_[:, j, :], tri[d], start=True, stop=True)
                        return ins
                    S.op("pe", cfn, reads=["sp", "cst"], writes=[("ps", 1)])
                    S.op("act", lambda e: e.activation(out=eb[:], in_=ps[1][:], func=AF.Exp, scale=-1.0 / 16, bias=lnsc[:, 0:1]),
                         reads=[("ps", 1), "lnsc"], writes=["eb"])
                    S.op("dve", lambda e, gi=gi: e.tensor_tensor(out=qt[:], in0=qT[:, TB(gi)], in1=eb[:], op=ALU.mult), reads=[("qTc", gi), "eb"], writes=[("qt", pb_)])
                    S.op("act", lambda e: e.activation(out=enb[:], in_=ps[1][:], func=AF.Exp, scale=1.0 / 16), reads=[("ps", 1)], writes=["eb"])
                    S.op("pool", lambda e, gi=gi: e.tensor_tensor(out=kt_[:], in0=kT[:, TB(gi)], in1=enb[:], op=ALU.mult), reads=[("kTc", gi), "eb"], writes=["kt"])
                    pb3 = ps[1][:].rearrange("p (c s) -> p c s", c=8)
                    col = 63 if d == 0 else 0
                    S.op("act", lambda e, pb3=pb3, col=col: e.activation(out=dec[:], in_=pb3[:, :, col], func=AF.Exp, scale=-1.0 / 16),
                         reads=[("ps", 1)], writes=[("dec", pb_)])

                    def gfn(e, d=d):
                        ins = None
                        for j in range(4):
                            ins = e.matmul(ps[2][:, j * 128:(j + 1) * 128], uu[d], sp_[:, j, :], start=True, stop=True)
                        return ins
                    S.op("pe", gfn, reads=["sp", "cst"], writes=[("ps", 2)])
                    S.op("act", lambda e: e.activation(out=eg[:].rearrange("p a b -> p (a b)"), in_=ps[2][:], func=AF.Exp, scale=-1.0 / 16),
                         reads=[("ps", 2)], writes=["eg"])
                if part == 2:
                    for ch_ in range(2):
                        rmask = cst[:, CI["trif"], 63:64] if ch_ == 0 else cst[:, CI["trib"], 64:65]
                        S.op("dve", lambda e, gi=gi, ch_=ch_, rmask=rmask: e.scalar_tensor_tensor(out=kd[ch_][:], in0=eg[:], scalar=rmask, in1=ktok[:, gi * 4:gi * 4 + 4, :],
                                                                                              op0=ALU.mult, op1=ALU.mult),
                             reads=[("ktok", gi * 2), ("ktok", gi * 2 + 1), "eg", "cst"], writes=[("kd", pb_, ch_)])

                    def sfn(e):
                        ins = None
                        for j in range(4):
                            ins = e.matmul(ps[3][:, j * 128:(j + 1) * 128], kt_[:, j * 128:(j + 1) * 128], qt[:, j * 128:(j + 1) * 128], start=True, stop=True)
                        return ins
                    S.op("pe", sfn, reads=["kt", ("qt", pb_)], writes=[("ps", 3)])
                if part == 3:
                    S.op("dve", lambda e, d=d: e.tensor_tensor(out=ms[:], in0=ps[3][:].rearrange("p (j c) -> p j c", j=4),
                                                              in1=trib16[d].unsqueeze(1).to_broadcast([128, 4, 128]), op=ALU.mult),
                         reads=[("ps", 3), "cstb"], writes=[("ms", pb_)])

            def chain(d, gi, pb_, first, after_tile=None):
                qt, kd, ms, dec = qt2[pb_], kd2[pb_], ms2[pb_], dec2[pb_]
                if first:
                    cur = si[0] % 2
                    S.op("dve", lambda e, cur=cur: e.memset(Sst[cur][:], 0.0), writes=[("Sst", cur)])
                    sb_i = si[0] % 4
                    S.op("pool", lambda e, sb_i=sb_i: e.memset(Sbf[sb_i][:], 0.0), writes=[("Sbf", sb_i)])
                tiles = list(range(gi * 4, gi * 4 + 4))
                ob = 4 + (si[0] % 2)
                po, pok = ps[ob], ("ps", ob)
                torder = tiles if d == 0 else tiles[::-1]
                for tt in torder:
                    j = tt - gi * 4
                    corder = (0, 1) if d == 0 else (1, 0)
                    S.op("pe", lambda e, tt=tt, j=j, po=po: e.matmul(po[:, j * 128:(j + 1) * 128], vtok[:, tt, :], ms[:, j, :], start=True, stop=False),
                         reads=[("vtok", tt // 2), ("ms", pb_)], writes=[pok])
                    for ci, ch in enumerate(corder):
                        cur = si[0] % 2
                        sb_i = si[0] % 4
                        csl = slice(j * 128 + ch * 64, j * 128 + ch * 64 + 64)
                        S.op("pe", lambda e, po=po, csl=csl, sb_i=sb_i, last=(ci == 1): e.matmul(po[:, csl], Sbf[sb_i][:], qt[:, csl], start=False, stop=last),
                             reads=[("Sbf", sb_i), ("qt", pb_)], writes=[pok])
                        kb = 6 + (si[0] % 2)
                        S.op("pe", lambda e, kb=kb, ch=ch, j=j, tt=tt: e.matmul(ps[kb][:, 0:128], kd[ch][:, j, :], vtok[:, tt, :], start=True, stop=True),
                             reads=[("kd", pb_, ch), ("vtok", tt // 2)], writes=[("ps", kb)])
                        nxt = 1 - cur
                        dcol = j * 2 + ch
                        nb_ = (si[0] + 1) % 4
                        S.op("dve", lambda e, kb=kb, cur=cur, nb_=nb_, dcol=dcol: e.scalar_tensor_tensor(
                            out=Sbf[nb_][:], in0=Sst[cur][:], scalar=dec[:, dcol:dcol + 1], in1=ps[kb][:, 0:128], op0=ALU.mult, op1=ALU.add),
                            reads=[("Sst", cur), ("dec", pb_), ("ps", kb)], writes=[("Sbf", nb_)])
                        S.op("dve", lambda e, kb=kb, cur=cur, nxt=nxt, dcol=dcol: e.scalar_tensor_tensor(
                            out=Sst[nxt][:], in0=Sst[cur][:], scalar=dec[:, dcol:dcol + 1], in1=ps[kb][:, 0:128], op0=ALU.mult, op1=ALU.add),
                            reads=[("Sst", cur), ("dec", pb_), ("ps", kb)], writes=[("Sst", nxt)])
                        si[0] += 1
                    if after_tile is not None:
                        after_tile(torder.index(tt))
                if d == 0:
                    S.op("act", lambda e, po=po, gi=gi: e.copy(out=oT[:, TB(gi)], in_=po[:]), reads=[pok], writes=[("oT", gi)])
                else:
                    S.op("dve", lambda e, po=po, gi=gi: e.tensor_tensor(out=oT[:, TB(gi)], in0=oT[:, TB(gi)], in1=po[:], op=ALU.add),
                         reads=[pok, ("oT", gi)], writes=[("oT", gi)])

            steps = [(0, gi, gi == 0) for gi in range(NB)] + [(1, gi, gi == NB - 1) for gi in range(NB - 1, -1, -1)]
            for part in range(4):
                prologue(steps[0][0], steps[0][1], 0, part)
            for k_, (d, gi, first) in enumerate(steps):
                if k_ + 1 < len(steps):
                    nd, ngi, npb = steps[k_ + 1][0], steps[k_ + 1][1], (k_ + 1) % 2
                    prologue(nd, ngi, npb, 0)
                    chain(d, gi, k_ % 2, first, after_tile=lambda ti, nd=nd, ngi=ngi, npb=npb: prologue(nd, ngi, npb, ti + 1) if ti < 3 else None)
                else:
                    chain(d, gi, k_ % 2, first)
            for gi in range(NB):
                S.op("act", lambda e, gi=gi: e.activation(out=sqo, in_=oT[:, TB(gi)], func=AF.Square), reads=[("oT", gi)], writes=["sp"])
                C.mm(ps[0][:], [(ones128[:], sqo)], reads=["ones128", "sp"], writes=[("ps", 0)])
                S.op("act", lambda e: e.activation(out=rso[:], in_=ps[0][:], func=AF.Ln, bias=epsb[:, 0:1], scale=1.0), reads=[("ps", 0), "eps"], writes=["eb"])
                S.op("act", lambda e: e.activation(out=rso[:], in_=rso[:], func=AF.Exp, scale=-0.5), reads=["eb"], writes=["eb"])
                C.mm(ps[1][:], [(wl[:, k, 2, :], hT[:, k, TB(gi)]) for k in range(8)], reads=["wCh"] + C.h_reads(gi), writes=[("ps", 1)])
                S.op("act", lambda e: e.activation(out=sgo, in_=ps[1][:], func=AF.Silu), reads=[("ps", 1)], writes=["eg"])
                S.op("dve", lambda e, gi=gi: e.scalar_tensor_tensor(out=oT[:, TB(gi)], in0=oT[:, TB(gi)], scalar=vec[l][:, V_GCO:V_GCO + 1], in1=rso[:],
                                                                 op0=ALU.mult, op1=ALU.mult), reads=[("oT", gi), "eb", ("vec", l)], writes=[("oT", gi)])
                S.op("dve", lambda e, gi=gi, h=h: e.tensor_tensor(out=ycT[:, h, TB(gi)], in0=oT[:, TB(gi)], in1=sgo, op=ALU.mult),
                     reads=[("oT", gi), "eg"], writes=[("y2T", h, gi)])
            if h == 0:
                C.dump(f"oc{l}", oT[:])
        S.barrier()
    C.dump(f"yc{l}", ycT[:])


def merge_out(C, l):
    S, dr, hT, vec, sbt, psb, TB, xT = C.S, C.dr, C.hT, C.vec, C.sbt, C.psb, C.TB, C.xT
    yT = C.yT
    with ExitStack() as ph:
        mT = sbt(ph, "mT", [128, 8, T], BF16)
        with ExitStack() as ph1:
            wm = [sbt(ph1, "wm", [128, 1536], BF16) for _ in range(3)]
            sg = [sbt(ph1, "sg", [128, 512], BF16) for _ in range(2)]
            acc = sbt(ph1, "macc", [128, NB, 512], F32)
            tm = [sbt(ph1, "tm", [128, 512], F32) for _ in range(2)]
            ykeys = [[k_ for k_ in S.last_writer if isinstance(k_, tuple) and k_[0] == f"y{br}T"] for br in range(3)]
            pi = 0
            si = 0
            for j in range(8):
                for br in range(3):
                    w_i = pi % 3
                    pi += 1
                    S.op("pool", lambda e, w_i=w_i, j=j, br=br: e.dma_start(out=wm[w_i][:], in_=dr[f"wM{l}"][j * 3 + br]),
                         writes=[("wm", w_i)], dma=f"wm{w_i}")
                    wp = wm[w_i][:, 0:512].rearrange("p (k m) -> p k m", k=4)
                    wgt = wm[w_i][:, 512:1536].rearrange("p (k m) -> p k m", k=8)
                    bcol = V_BG + br * 8 + j
                    for tb in range(NB):
                        pg, pgk = psb("mg", [0, 1, 2, 3])
                        C.mm(pg[:], [(wgt[:, k, :], hT[:, k, TB(tb)]) for k in range(8)], reads=[("wm", w_i)] + C.h_reads(tb), writes=[pgk])
                        pp, ppk = psb("mp", [4, 5, 6, 7])
                        C.mm(pp[:], [(wp[:, k, :], yT[br][:, k, TB(tb)]) for k in range(4)], reads=[("wm", w_i)] + ykeys[br], writes=[ppk])
                        s_i = si % 2
                        si += 1
                        S.op("act", lambda e, pg=pg, s_i=s_i, bcol=bcol: e.activation(out=sg[s_i][:], in_=pg[:], func=AF.Sigmoid,
                                                                                     bias=vec[l][:, bcol:bcol + 1], scale=1.0),
                             reads=[pgk, ("vec", l)], writes=[("sg", s_i)])
                        if br == 0:
                            S.op("dve", lambda e, pp=pp, s_i=s_i, tb=tb: e.tensor_tensor(out=acc[:, tb, :], in0=pp[:], in1=sg[s_i][:], op=ALU.mult),
                                 reads=[ppk, ("sg", s_i)], writes=[("macc", tb)])
                        else:
                            S.op("dve", lambda e, pp=pp, s_i=s_i: e.tensor_tensor(out=tm[s_i][:], in0=pp[:], in1=sg[s_i][:], op=ALU.mult),
                                 reads=[ppk, ("sg", s_i)], writes=[("tm", s_i)])
                            if br == 1:
                                S.op("pool", lambda e, s_i=s_i, tb=tb: e.tensor_tensor(out=acc[:, tb, :], in0=acc[:, tb, :], in1=tm[s_i][:], op=ALU.add),
                                     reads=[("macc", tb), ("tm", s_i)], writes=[("macc", tb)])
                            else:
                                S.op("pool", lambda e, s_i=s_i, tb=tb, j=j: e.tensor_tensor(out=mT[:, j, TB(tb)], in0=acc[:, tb, :], in1=tm[s_i][:], op=ALU.add),
                                     reads=[("macc", tb), ("tm", s_i)], writes=[("mT", j, tb)])
            S.barrier()
        wo = [sbt(ph, "wo", [128, 1024], BF16) for _ in range(2)]
        for j in range(8):
            w_i = j % 2
            S.op("pool", lambda e, w_i=w_i, j=j: e.dma_start(out=wo[w_i][:], in_=dr[f"wO{l}"][j]), writes=[("wo", w_i)], dma=f"wo{w_i}")
            wov = wo[w_i][:].rearrange("p (k m) -> p k m", k=8)
            for tb in range(NB):
                po, pok = psb("all", C.ALLB)
                C.mm(po[:], [(wov[:, k, :], mT[:, k, TB(tb)]) for k in range(8)], reads=[("wo", w_i)], writes=[pok])
                S.op("dve", lambda e, po=po, j=j, tb=tb: e.tensor_tensor(out=xT[:, j, TB(tb)], in0=xT[:, j, TB(tb)], in1=po[:], op=ALU.add),
                     reads=[pok, C.xk(j, tb)], writes=[C.xk(j, tb)])
        S.barrier()
    C.dump(f"xmix{l}", xT[:])


def ffn_groups(C, wname, g0, ng, gbc=None, gkey=None):
    S, dr, hT, psb, TB, xT = C.S, C.dr, C.hT, C.psb, C.TB, C.xT
    wf, abuf, sgf = C.wf, C.abuf, C.sgf
    GU = [0, 1, 2, 3]
    OB = [4, 5, 6, 7]
    g = 0
    while g < ng:
        n_in = min(2, ng - g)
        a_i = C.ffi[1] % 2
        C.ffi[1] += 1
        ab = abuf[a_i]
        wviews = []
        w_is = []
        for q in range(n_in):
            w_i = C.ffi[0] % 4
            C.ffi[0] += 1
            w_is.append(w_i)
            S.op("pool", lambda e, w_i=w_i, gg=g0 + g + q: e.dma_start(out=wf[w_i][:], in_=dr[wname][gg]), writes=[("wf", w_i)], dma=f"wf{w_i}")
        for q in range(n_in):
            w_i = w_is[q]
            wgv = wf[w_i][:, 0:2048].rearrange("p (c k m) -> p c k m", c=2, k=8)
            wuv = wf[w_i][:, 2048:4096].rearrange("p (c k m) -> p c k m", c=2, k=8)
            wdv = wf[w_i][:, 4096:6144].rearrange("p (c j m) -> p c j m", c=2, j=8)
            wviews.append((w_i, wdv))
            for c in range(2):
                cc = q * 2 + c
                for tb in range(NB):
                    pg, pgk = psb("gu", GU)
                    C.mm(pg[:], [(wgv[:, c, k, :], hT[:, k, TB(tb)]) for k in range(8)], reads=[("wf", w_i)] + C.h_reads(tb), writes=[pgk])
                    pu, puk = psb("gu", GU)
                    C.mm(pu[:], [(wuv[:, c, k, :], hT[:, k, TB(tb)]) for k in range(8)], reads=[("wf", w_i)] + C.h_reads(tb), writes=[puk])
                    s_i = C.ffi[2] % 2
                    C.ffi[2] += 1
                    S.op("act", lambda e, pg=pg, s_i=s_i: e.activation(out=sgf[s_i][:], in_=pg[:], func=AF.Silu), reads=[pgk], writes=[("sgf", s_i)])
                    if gbc is not None:
                        S.op("pool", lambda e, s_i=s_i, tb=tb: e.tensor_tensor(out=sgf[s_i][:], in0=sgf[s_i][:], in1=gbc[:, TB(tb)], op=ALU.mult),
                             reads=[("sgf", s_i), gkey], writes=[("sgf", s_i)])
                    S.op("dve", lambda e, pu=pu, s_i=s_i, ab=ab, cc=cc, tb=tb: e.tensor_tensor(out=ab[:, cc, TB(tb)], in0=pu[:], in1=sgf[s_i][:], op=ALU.mult),
                         reads=[puk, ("sgf", s_i)], writes=[("ab", a_i, cc, tb)])
        for tb in range(NB):
            for j in range(8):
                po, pok = psb("ob", OB)
                pairs = [(wdv[:, c, j, :], ab[:, q * 2 + c, TB(tb)]) for q, (w_i, wdv) in enumerate(wviews) for c in range(2)]
                C.mm(po[:], pairs,
                     reads=[("wf", w_i) for (w_i, _) in wviews] + [("ab", a_i, cc, tb) for cc in range(2 * n_in)], writes=[pok])
                S.op("dve", lambda e, po=po, j=j, tb=tb: e.tensor_tensor(out=xT[:, j, TB(tb)], in0=xT[:, j, TB(tb)], in1=po[:], op=ALU.add),
                     reads=[pok, C.xk(j, tb)], writes=[C.xk(j, tb)])
        g += n_in


def ffn_alloc(C, ph):
    sbt = C.sbt
    C.wf = [sbt(ph, "wf", [128, 6144], BF16) for _ in range(4)]
    C.abuf = [sbt(ph, "abuf", [128, 4, T], BF16) for _ in range(2)]
    C.sgf = [sbt(ph, "sgf", [128, 512], F32) for _ in range(2)]
    C.ffi = [0, 0, 0]


def ffn_dense(C):
    with ExitStack() as ph:
        ffn_alloc(C, ph)
        ffn_groups(C, "wF0", 0, DFF // 256)
        C.S.barrier()
    C.dump("xffn0", C.xT[:])


def moe(C, l):
    S, dr, sbt, vec, cst, CI, ps, xT = C.S, C.dr, C.sbt, C.vec, C.cst, C.CI, C.ps, C.xT
    with ExitStack() as ph0:
        gate = sbt(ph0, "gate", [128, NT, NEXP], F32)
        with ExitStack() as ph:
            wr = sbt(ph, "wR", [128, 8, 8], F32)
            lg = sbt(ph, "lg", [128, NT, NEXP], F32)
            lg2 = sbt(ph, "lg2", [128, NT, NEXP], F32)
            mk1 = sbt(ph, "mk1", [128, NT, NEXP], F32)
            mk2 = sbt(ph, "mk2", [128, NT, NEXP], F32)
            m1 = sbt(ph, "m1", [128, NT], F32)
            m2 = sbt(ph, "m2", [128, NT], F32)
            w1 = sbt(ph, "w1", [128, NT], F32)
            w2 = sbt(ph, "w2", [128, NT], F32)
            S.op("sp", lambda e: e.dma_start(out=wr[:], in_=dr["wR"][:, :, :]), writes=["wR"], dma="wR")
            plg = ps[7]

            def router(tb, h32):
                def rfn(e):
                    ins = None
                    for jt in range(4):
                        tt = tb * 4 + jt
                        for c in range(8):
                            ins = e.matmul(plg[:, tt * 8:(tt + 1) * 8], h32[:, c, jt * 128:(jt + 1) * 128], wr[:, c, :], start=(c == 0), stop=(c == 7))
                    return ins
                S.op("pe", rfn, reads=[("h32", c) for c in range(8)] + ["wR"], writes=[("plg", tb)])
            C.rmsnorm_to_h(lambda c: vec[l][:, V_GFFN + c:V_GFFN + c + 1], ("vec", l), router=router)
            bc = lambda a: a[:].unsqueeze(2).to_broadcast([128, NT, NEXP])
            S.op("dve", lambda e: e.tensor_copy(out=lg[:].rearrange("p a b -> p (a b)"), in_=plg[:, 0:NT * NEXP]), writes=["lg"])
            S.op("dve", lambda e: e.tensor_reduce(out=m1[:], in_=lg[:], axis=AX.X, op=ALU.max), reads=["lg"], writes=["m1"])
            S.op("dve", lambda e: e.tensor_tensor(out=mk1[:], in0=lg[:], in1=bc(m1), op=ALU.is_equal), reads=["lg", "m1"], writes=["mk1"])
            S.op("dve", lambda e: e.scalar_tensor_tensor(out=lg2[:], in0=mk1[:], scalar=-1e30, in1=lg[:], op0=ALU.mult, op1=ALU.add),
                 reads=["mk1", "lg"], writes=["lg2"])
            S.op("dve", lambda e: e.tensor_reduce(out=m2[:], in_=lg2[:], axis=AX.X, op=ALU.max), reads=["lg2"], writes=["m2"])
            S.op("dve", lambda e: e.tensor_tensor(out=mk2[:], in0=lg2[:], in1=bc(m2), op=ALU.is_equal), reads=["lg2", "m2"], writes=["mk2"])
            S.op("dve", lambda e: e.tensor_tensor(out=w2[:], in0=m2[:], in1=m1[:], op=ALU.subtract), reads=["m1", "m2"], writes=["w2"])
            S.op("act", lambda e: e.activation(out=w2[:], in_=w2[:], func=AF.Exp), reads=["w2"], writes=["w2"])
            S.op("dve", lambda e: e.tensor_tensor(out=w1[:], in0=w2[:], in1=C.ones1[:, 0:NT], op=ALU.add), reads=["w2", "ones1"], writes=["w1"])
            S.op("dve", lambda e: e.reciprocal(out=w1[:], in_=w1[:]), reads=["w1"], writes=["w1"])
            S.op("dve", lambda e: e.tensor_tensor(out=w2[:], in0=w2[:], in1=w1[:], op=ALU.mult), reads=["w1", "w2"], writes=["w2"])
            S.op("dve", lambda e: e.tensor_tensor(out=mk1[:], in0=mk1[:], in1=bc(w1), op=ALU.mult), reads=["mk1", "w1"], writes=["mk1"])
            S.op("dve", lambda e: e.tensor_tensor(out=mk2[:], in0=mk2[:], in1=bc(w2), op=ALU.mult), reads=["mk2", "w2"], writes=["mk2"])
            S.op("dve", lambda e: e.tensor_tensor(out=gate[:], in0=mk1[:], in1=mk2[:], op=ALU.add), reads=["mk1", "mk2"], writes=["gate"])
            S.barrier()
        C.dump("gate", gate[:].rearrange("p a b -> p (a b)"))
        with ExitStack() as ph:
            ffn_alloc(C, ph)
            gbc = [sbt(ph, "gbc", [128, T], F32) for _ in range(2)]
            dg = [sbt(ph, "dg", [128, 128], F32) for _ in range(2)]
            ident = cst[:, CI["ident"], :]
            di = 0
            for ex in range(NEXP):
                gb = gbc[ex % 2]
                for tg in range(4):
                    pb_, pbk = C.psb("ob", [4, 5, 6, 7])
                    for jt in range(4):
                        tt = tg * 4 + jt
                        d_i = di % 2
                        di += 1
                        S.op("dve", lambda e, d_i=d_i, tt=tt, ex=ex: e.scalar_tensor_tensor(out=dg[d_i][:], in0=ident, scalar=gate[:, tt, ex:ex + 1], in1=C.ones1[:], op0=ALU.mult, op1=ALU.mult),
                             reads=["gate", "cst", "ones1"], writes=[("dg", d_i)])
                        S.op("pe", lambda e, d_i=d_i, jt=jt, pb_=pb_: e.matmul(pb_[:, jt * 128:(jt + 1) * 128], C.ones1[:], dg[d_i][:], start=True, stop=True),
                             reads=[("dg", d_i), "ones1"], writes=[pbk])
                    S.op("act", lambda e, pb_=pb_, gb=gb, tg=tg: e.copy(out=gb[:, tg * 512:(tg + 1) * 512], in_=pb_[:]), reads=[pbk], writes=[("gbc", ex % 2)])
                ffn_groups(C, "wE", ex * (DFFE // 256), DFFE // 256, gbc=gb, gkey=("gbc", ex % 2))
            S.barrier()
    C.dump("xffn1", xT[:])


def final_out(C):
    S, sbt, xT, TB, gfin, epsb, onesm, psb, ident = C.S, C.sbt, C.xT, C.TB, C.gfin, C.epsb, C.onesm, C.psb, C.ident
    with ExitStack() as ph:
        sq = [sbt(ph, "sqf", [128, 512], F32) for _ in range(2)]
        rs = sbt(ph, "rsf", [128, T], F32)
        yt = [sbt(ph, "ytf", [128, 128], F32) for _ in range(4)]
        stage = [sbt(ph, "stgo", [128, D], F32) for _ in range(2)]
        for tb in range(NB):
            p, pk = psb("all", C.ALLB)
            for c in range(8):
                s_ = sq[c % 2]
                S.op("act", lambda e, s_=s_, c=c, tb=tb: e.activation(out=s_[:], in_=xT[:, c, TB(tb)], func=AF.Square),
                     reads=[C.xk(c, tb)], writes=[("sqf", c % 2)])
                S.op("pe", lambda e, s_=s_, c=c, p=p: e.matmul(p[:], onesm[:], s_[:], start=(c == 0), stop=(c == 7)),
                     reads=[("sqf", c % 2), "onesm"], writes=[pk])
            S.op("act", lambda e, p=p, tb=tb: e.activation(out=rs[:, TB(tb)], in_=p[:], func=AF.Ln, bias=epsb[:, 0:1], scale=1.0),
                 reads=[pk, "eps"], writes=[("rsf", tb)])
            S.op("act", lambda e, tb=tb: e.activation(out=rs[:, TB(tb)], in_=rs[:, TB(tb)], func=AF.Exp, scale=-0.5), reads=[("rsf", tb)], writes=[("rsf", tb)])
        yi = 0
        for tt in range(NT):
            tb = tt // 4
            st = stage[tt % 2]
            for half in range(2):
                p, pk = psb("all", C.ALLB)
                for j in range(4):
                    c = half * 4 + j
                    y_i = yi % 4
                    yi += 1
                    S.op("dve", lambda e, y_i=y_i, c=c, tt=tt: e.scalar_tensor_tensor(out=yt[y_i][:], in0=xT[:, c, tt * 128:(tt + 1) * 128], scalar=gfin[:, c:c + 1],
                                                                                 in1=rs[:, tt * 128:(tt + 1) * 128], op0=ALU.mult, op1=ALU.mult),
                         reads=[C.xk(c, tb), ("rsf", tb), "gfin"], writes=[("ytf", y_i)])
                    S.op("pe", lambda e, y_i=y_i, j=j, p=p: e.transpose(p[:, j * 128:(j + 1) * 128], yt[y_i][:], ident),
                         reads=[("ytf", y_i), "cst"], writes=[pk])
                S.op("act", lambda e, p=p, st=st, half=half: e.copy(out=st[:, half * 512:(half + 1) * 512], in_=p[:]),
                     reads=[pk], writes=[("stgo", tt % 2, half)])
            S.op("sp", lambda e, st=st, tt=tt: e.dma_start(out=C.y_out[tt * 128:(tt + 1) * 128, :], in_=st[:]),
                 reads=[("stgo", tt % 2, 0), ("stgo", tt % 2, 1)], dma=f"yout{tt % 2}")


_CACHE = {}


def kernel(**inputs):
    x = np.asarray(inputs["x"], dtype=np.float32)
    packed = pack_inputs(inputs)
    shapes = {k: v.shape for k, v in packed.items()}
    shapes["x"] = (T, D)
    nc = build_program(shapes)
    in_maps = []
    for b in range(8):
        m = dict(packed)
        m["x"] = np.ascontiguousarray(x[b])
        in_maps.append(m)
    res = run_bass_kernel_spmd(nc, in_maps, core_ids=list(range(8)))
    return np.stack([np.asarray(res.results[b]["y"], dtype=np.float32) for b in range(8)], 0)
```

```python
import numpy as np
import concourse.bass as bass
import concourse.mybir as mybir
from concourse.bass_utils import run_bass_kernel_spmd
from contextlib import ExitStack

F32 = mybir.dt.float32
BF16 = mybir.dt.bfloat16
AF = mybir.ActivationFunctionType
ALU = mybir.AluOpType
AX = mybir.AxisListType

T = 2048
D = 1024
NT = 16
NB = 4
DEPTH = 2
DFF = 2816
NEXP = 8
DFFE = 3584
EPS = 1e-6
ENGS = ("sp", "act", "dve", "pool", "pe")
SEM_EPOCH = 30000
SKIP = ''
STRICT = True
HLS = (0, 1, 2, 3)
DEBUG_OUT = {}


class Sched:
    def __init__(self, nc, es):
        self.nc = nc
        self.es = es
        self.ops = []
        self.last_writer = {}
        self.readers = {}
        self.dma_sems = {}
        self.eng_ops = {e: [] for e in ENGS}
        self.bar_start = 0

    def op(self, eng, fn, reads=(), writes=(), dma=None, dma_batch=False, extra=()):
        deps = set(extra)
        raw = set(extra)
        for k in reads:
            w = self.last_writer.get(k)
            if w is not None:
                deps.add(w)
                raw.add(w)
            if isinstance(k, tuple) and k and k[0] in ("ps", "plg"):
                for r in self.readers.get(k, ()):
                    if self.ops[r]["eng"] != eng:
                        deps.add(r)
                        raw.add(r)
        for k in writes:
            w = self.last_writer.get(k)
            if w is not None:
                deps.add(w)
            for r in self.readers.get(k, ()):
                deps.add(r)
        idx = len(self.ops)
        o = dict(eng=eng, fn=fn, deps=deps, dma=dma, needed=False, idx=idx, raw=raw)
        if dma is not None:
            d = self.dma_sems.setdefault(dma, dict(total=0, batch=dma_batch))
            d["total"] += 16
            o["dma_val"] = d["total"]
        self.ops.append(o)
        self.eng_ops[eng].append(idx)
        for k in reads:
            self.readers.setdefault(k, []).append(idx)
        for k in writes:
            self.last_writer[k] = idx
            self.readers[k] = []
        return idx

    def barrier(self):
        last = [self.eng_ops[e][-1] for e in ENGS if self.eng_ops[e] and not self.ops[self.eng_ops[e][-1]].get("bar")]
        dmas = [i for i in range(self.bar_start, len(self.ops)) if self.ops[i]["dma"] is not None]
        ex = set(last + dmas)
        for e in ENGS:
            i_ = self.op(e, lambda eng: eng.nop(), extra=ex)
            self.ops[i_]["bar"] = True
        self.bar_start = len(self.ops)
        self.last_writer = {}
        self.readers = {}

    def emit(self, final_waits=()):
        nc = self.nc
        ops = self.ops
        for o in ops:
            nd = set()
            for d in o["deps"]:
                p = ops[d]
                if p["dma"] is None and p["eng"] == o["eng"] and o["dma"] is None:
                    if o["eng"] == "pe":
                        continue
                    if d not in o["raw"] and not STRICT:
                        continue
                nd.add(d)
            o["deps"] = nd
            for d in nd:
                ops[d]["needed"] = True
        cnt = {e: 0 for e in ENGS}
        for o in ops:
            if o["dma"] is None and o["needed"]:
                cnt[o["eng"]] += 1
                o["sig"] = cnt[o["eng"]]
        self.cnt = cnt
        eng_sems = {}
        for e in ENGS:
            n_ep = max(cnt[e] - 1, 0) // SEM_EPOCH + 1
            eng_sems[e] = [self.es.enter_context(nc.semaphore(f"s_{e}{i}")) for i in range(n_ep)]
        dsem = {}
        for name in self.dma_sems:
            dsem[name] = self.es.enter_context(nc.semaphore(f"d_{name}"))

        def event(o):
            if o["dma"] is not None:
                d = self.dma_sems[o["dma"]]
                v = d["total"] if d["batch"] else o["dma_val"]
                return (("d", o["dma"]), dsem[o["dma"]], v)
            s = o["sig"]
            ep = (s - 1) // SEM_EPOCH
            return (("e", o["eng"], ep), eng_sems[o["eng"]][ep], s - ep * SEM_EPOCH)

        block = self.es.enter_context(nc.Block())

        def run_engine(ename):
            def body(eng):
                wm = {}
                for idx in self.eng_ops[ename]:
                    o = ops[idx]
                    need = {}
                    for d in o["deps"]:
                        key, sem, val = event(ops[d])
                        if val > need.get(key, (None, 0))[1]:
                            need[key] = (sem, val)
                    for key, (sem, val) in need.items():
                        if key[0] == "e":
                            if any(k[0] == "e" and k[1] == key[1] and k[2] > key[2] for k in wm):
                                continue
                        if wm.get(key, 0) >= val:
                            continue
                        eng.wait_ge(sem, val)
                        wm[key] = val
                    ins = o["fn"](eng)
                    if o["dma"] is not None:
                        ins.then_inc(dsem[o["dma"]], 16)
                    elif o["needed"]:
                        key, sem, val = event(o)
                        ins.then_inc(sem, 1)
                for (e2, name) in final_waits:
                    if e2 == ename and name in dsem:
                        eng.wait_ge(dsem[name], self.dma_sems[name]["total"])
            return body

        block.sync(run_engine("sp"))
        block.scalar(run_engine("act"))
        block.vector(run_engine("dve"))
        block.gpsimd(run_engine("pool"))
        block.tensor(run_engine("pe"))


IN_SIZES = (512, 128, 128, 256, 256, 32, 512, 512, 512, 512, 16, 16, 3072)
OFF = np.concatenate([[0], np.cumsum(IN_SIZES)]).astype(int)
(O_AQ, O_AK, O_AV, O_BCQ, O_BCKV, O_BKR, O_CQ, O_CK, O_CV, O_CG, O_CAF, O_CAB, O_GATE) = OFF[:13]


def lhsT(W, cols):
    W = np.asarray(W)
    kc = W.shape[0] // 128
    sub = W[:, cols] if cols is not None else W
    return np.ascontiguousarray(sub.reshape(kc, 128, sub.shape[1]).transpose(1, 0, 2))


def partner(n):
    h = n // 2
    return np.concatenate([np.arange(h, n), np.arange(0, h)])


def rope_tables():
    GRID_W = 64
    rows = T // GRID_W
    row = np.repeat(np.arange(rows, dtype=np.float32), GRID_W)
    col = np.tile(np.arange(GRID_W, dtype=np.float32), rows)

    def tabs(rot_dim):
        nf = rot_dim // 4
        inv = (10000.0 ** (-np.arange(nf, dtype=np.float32) / nf)).astype(np.float32)
        ar = row[:, None] * inv
        ac = col[:, None] * inv
        half = rot_dim // 2
        cos = np.concatenate([np.cos(ar), np.cos(ar), np.cos(ac), np.cos(ac)], axis=1)
        sin = np.concatenate([-np.sin(ar), np.sin(ar), -np.sin(ac), np.sin(ac)], axis=1)
        return cos.T.astype(np.float32), sin.T.astype(np.float32)
    ca, sa = tabs(64)
    cb, sb = tabs(32)
    cosA = np.concatenate([ca, ca], 0)
    sinA = np.concatenate([sa, sa], 0)
    cosB = np.concatenate([np.ones((64, T), np.float32), cb, np.zeros((32, T), np.float32)], 0)
    sinB = np.concatenate([np.zeros((64, T), np.float32), sb, np.zeros((32, T), np.float32)], 0)
    return (np.ascontiguousarray(cosA), np.ascontiguousarray(sinA),
            np.ascontiguousarray(cosB), np.ascontiguousarray(sinB))


def rope_partner_cols(n_rot):
    h = n_rot // 2
    p = partner(h)
    return np.concatenate([p, h + p])


def consts_pack():
    c = {}
    c["ident"] = np.eye(128, dtype=np.float32)
    blk = np.zeros((128, 128), np.float32)
    blk[:64, :64] = 1.0 / 64
    blk[64:, 64:] = 1.0 / 64
    c["blk64"] = blk
    pa = rope_partner_cols(64)
    full = np.concatenate([pa, 64 + pa])
    P = np.zeros((128, 128), np.float32)
    P[full, np.arange(128)] = 1.0
    c["permA"] = P
    s = np.arange(128)
    same = (s[:, None] // 64) == (s[None, :] // 64)
    c["trif"] = (same & (s[:, None] <= s[None, :])).astype(np.float32)
    c["trib"] = (same & (s[:, None] >= s[None, :])).astype(np.float32)
    c["uf"] = (same & (s[:, None] > s[None, :])).astype(np.float32)
    c["ub"] = (same & (s[:, None] < s[None, :])).astype(np.float32)
    names = ["ident", "blk64", "permA", "trif", "trib", "uf", "ub"]
    return np.ascontiguousarray(np.stack([c[n] for n in names], 1)), names


CONST_NAMES = ["ident", "blk64", "permA", "trif", "trib", "uf", "ub"]
V_GMIX, V_GFFN, V_GAQ, V_GAK, V_GBQ, V_GBKV, V_GCO, V_BG = 0, 8, 16, 17, 18, 20, 22, 23
NVEC = 23 + 24


def pack_inputs(inp):
    out = {}
    cst, _ = consts_pack()
    out["cst"] = cst
    cosA, sinA, cosB, sinB = rope_tables()
    out["ropeA"] = np.ascontiguousarray(np.stack([cosA, sinA], 1))
    out["ropeB"] = np.ascontiguousarray(np.stack([cosB, sinB], 1))
    pk96 = rope_partner_cols(32)
    for l in range(DEPTH):
        w_in = np.asarray(inp["w_in"][l])
        vec = np.zeros((128, NVEC), np.float32)
        vec[:, V_GMIX:V_GMIX + 8] = np.asarray(inp["g_mix"][l]).reshape(8, 128).T
        vec[:, V_GFFN:V_GFFN + 8] = np.asarray(inp["g_ffn"][l]).reshape(8, 128).T
        vec[:, V_GAQ] = np.tile(np.asarray(inp["g_a_q"][l]), 2)
        vec[:, V_GAK] = np.tile(np.asarray(inp["g_a_k"][l]), 2)
        vec[:, V_GBQ:V_GBQ + 2] = np.asarray(inp["g_b_q"][l]).reshape(2, 128).T
        vec[:, V_GBKV:V_GBKV + 2] = np.asarray(inp["g_b_kv"][l]).reshape(2, 128).T
        vec[:, V_GCO] = np.asarray(inp["g_c_out"][l])
        vec[:, V_BG:V_BG + 24] = np.asarray(inp["b_gate"][l]).reshape(24, 128).T
        out[f"vec{l}"] = vec
        wa = []
        for g in range(2):
            kg = lhsT(w_in, O_AK + np.arange(g * 64, g * 64 + 64))
            zz = np.zeros_like(kg)
            wa.append(np.concatenate([lhsT(w_in, O_AQ + np.arange(2 * g * 128, (2 * g + 2) * 128)), kg, zz, zz, kg,
                                      lhsT(w_in, O_AV + np.arange(g * 64, g * 64 + 64))], axis=2))
        out[f"wA{l}"] = np.ascontiguousarray(np.stack(wa, 0))
        z64 = np.zeros((128, 8, 64), np.float32)
        kr = lhsT(w_in, O_BKR + np.arange(32))
        krp = lhsT(w_in, O_BKR + pk96)
        out[f"wB1{l}"] = np.ascontiguousarray(np.concatenate(
            [lhsT(w_in, O_BCQ + np.arange(256)), lhsT(w_in, O_BCKV + np.arange(256)), z64, kr, z64, krp], axis=2))
        wq = np.asarray(inp["w_b_q_up"][l])
        qcols, qpcols = [], []
        for h in range(8):
            base = h * 96
            qcols.append(base + np.arange(96))
            qpcols.append(np.concatenate([base + np.arange(64), base + 64 + pk96]))
        wq_l = np.stack([np.stack([lhsT(wq, qcols[h]), lhsT(wq, qpcols[h])], 2) for h in range(8)], 2)
        wkv = np.asarray(inp["w_b_kv_up"][l])
        wk_l = np.stack([lhsT(wkv, h * 128 + np.arange(64)) for h in range(8)], 2)
        wv_l = np.stack([lhsT(wkv, h * 128 + 64 + np.arange(64)) for h in range(8)], 2)
        out[f"wB2{l}"] = np.ascontiguousarray(np.concatenate(
            [wq_l.reshape(128, -1), wk_l.reshape(128, -1), wv_l.reshape(128, -1)], axis=1))
        wc = []
        for h in range(4):
            sl = np.arange(h * 128, (h + 1) * 128)
            a = np.stack([lhsT(w_in, O_CQ + sl), lhsT(w_in, O_CK + sl), lhsT(w_in, O_CG + sl)], 2)
            b = np.concatenate([lhsT(w_in, O_CK + sl), lhsT(w_in, O_CV + sl)], 2)
            wc.append(np.concatenate([a.reshape(128, -1), b.reshape(128, -1)], 1))
        out[f"wC{l}"] = np.ascontiguousarray(np.stack(wc, 0))
        z16 = np.zeros((128, 8, 16), np.float32)
        out[f"wCg{l}"] = np.ascontiguousarray(np.concatenate(
            [lhsT(w_in, O_CAF + np.arange(16)), z16, lhsT(w_in, O_CAB + np.arange(16)), z16], 2))
        up = np.zeros((128, 4, 2, 128), np.float32)
        waf = np.asarray(inp["w_c_af_up"][l]); wab = np.asarray(inp["w_c_ab_up"][l])
        baf = np.asarray(inp["b_c_af"][l]); bab = np.asarray(inp["b_c_ab"][l])
        for h in range(4):
            sl = slice(h * 128, (h + 1) * 128)
            up[0:16, h, 0] = waf[:, sl]
            up[48, h, 0] = baf[sl]
            up[32:48, h, 1] = wab[:, sl]
            up[48, h, 1] = bab[sl]
        out[f"wCup{l}"] = np.ascontiguousarray(up.reshape(128, -1))
        wps = [np.asarray(inp[n][l]) for n in ("w_pa", "w_pb", "w_pc")]
        wm = []
        for j in range(8):
            cj = np.arange(j * 128, (j + 1) * 128)
            for br in range(3):
                a = lhsT(wps[br], cj).reshape(128, -1)
                gts = lhsT(w_in, O_GATE + br * 1024 + cj).reshape(128, -1)
                wm.append(np.concatenate([a, gts], 1))
        out[f"wM{l}"] = np.ascontiguousarray(np.stack(wm, 0))
        wo = np.asarray(inp["w_out"][l])
        out[f"wO{l}"] = np.ascontiguousarray(np.stack([lhsT(wo, np.arange(j * 128, (j + 1) * 128)).reshape(128, -1) for j in range(8)], 0))

    def ffn_pack(wg, wu, wd):
        dff = wg.shape[1]
        ng = dff // 256
        blocks = []
        for g in range(ng):
            parts = []
            for w in (wg, wu):
                parts.append(np.stack([lhsT(w, np.arange((2 * g + c) * 128, (2 * g + c + 1) * 128)) for c in range(2)], 1).reshape(128, -1))
            dd = np.stack([np.asarray(wd)[(2 * g + c) * 128:(2 * g + c + 1) * 128, :] for c in range(2)], 1).reshape(128, -1)
            parts.append(dd)
            blocks.append(np.concatenate(parts, 1))
        return np.ascontiguousarray(np.stack(blocks, 0))
    out["wF0"] = ffn_pack(np.asarray(inp["w_ff_gate"][0]), np.asarray(inp["w_ff_up"][0]), np.asarray(inp["w_ff_down"][0]))
    we = [ffn_pack(np.asarray(inp["w_e_gate"][0][e]), np.asarray(inp["w_e_up"][0][e]), np.asarray(inp["w_e_down"][0][e])) for e in range(NEXP)]
    out["wE"] = np.ascontiguousarray(np.concatenate(we, 0))
    out["wR"] = lhsT(np.asarray(inp["w_router"][0]), None)
    gf = np.asarray(inp["g_final"]).reshape(8, 128).T
    out["gfin"] = np.ascontiguousarray(gf)
    return out


class Ctx:
    pass


def build_program(shapes, stop_after=None, dbg=None):
    nc = bass.Bass("TRN2", target_bir_lowering=False)
    dr = {}
    for name, shp in shapes.items():
        dr[name] = nc.dram_tensor(name, list(shp), F32, kind="ExternalInput").ap()
    y_out = nc.dram_tensor("y", [T, D], F32, kind="ExternalOutput").ap()
    dbg_out = {}
    if dbg:
        for name, shp in dbg.items():
            dbg_out[name] = nc.dram_tensor("dbg_" + name, list(shp), F32, kind="ExternalOutput").ap()
    es = ExitStack()
    with es:
        S = Sched(nc, es)
        uid = [0]

        def sbt(stack, name, shape, dt):
            uid[0] += 1
            return stack.enter_context(nc.sbuf_tensor(f"{name}_{uid[0]}", list(shape), dt))

        P = lambda name, shape, dt=F32: sbt(es, name, shape, dt)
        ps = [es.enter_context(nc.psum_tensor(f"ps{i}", [128, 512], F32)) for i in range(8)]
        rr = {}

        def psb(pool, banks):
            i = rr.get(pool, 0)
            rr[pool] = i + 1
            b = banks[i % len(banks)]
            return ps[b], ("ps", b)

        ALLB = [0, 1, 2, 3, 4, 5, 6, 7]
        PJ = [2, 3, 4, 5, 6, 7]

        xT = P("xT", [128, 8, T])
        hT = P("hT", [128, 8, T], BF16)
        cst = P("cst", [128, 7, 128])
        cstb = P("cstb", [128, 7, 128], BF16)
        vec = [P(f"vec{l}", [128, NVEC]) for l in range(DEPTH)]
        gfin = P("gfin", [128, 8])
        epsb = P("epsb", [128, 1])
        lnsc = P("lnsc", [128, 1])
        onesm = P("onesm", [128, 128])
        ones256 = P("ones256", [128, 128], BF16)
        ones128 = P("ones128", [128, 128])
        ones1 = P("ones1", [128, 128])
        CI = {n: i for i, n in enumerate(CONST_NAMES)}
        ident = cst[:, CI["ident"], :]

        def cload(eng, out, in_, key, sem="const"):
            S.op(eng, lambda e: e.dma_start(out=out, in_=in_), writes=[key], dma=sem, dma_batch=True)

        cload("sp", cst[:], dr["cst"][:, :, :], "cst")
        for l in range(DEPTH):
            cload("sp", vec[l][:], dr[f"vec{l}"][:, :], ("vec", l))
        cload("sp", gfin[:], dr["gfin"][:, :], "gfin")
        S.op("dve", lambda e: e.tensor_copy(out=cstb[:], in_=cst[:]), reads=["cst"], writes=["cstb"])
        S.op("dve", lambda e: e.memset(epsb[:], EPS), writes=["eps"])
        S.op("dve", lambda e: e.memset(lnsc[:], float(np.log(128.0 ** -0.5))), writes=["lnsc"])
        S.op("dve", lambda e: e.memset(onesm[:], 1.0 / 1024), writes=["onesm"])
        S.op("dve", lambda e: e.memset(ones256[:], 1.0 / 256), writes=["ones256"])
        S.op("dve", lambda e: e.memset(ones128[:], 1.0 / 128), writes=["ones128"])
        S.op("dve", lambda e: e.memset(ones1[:], 1.0), writes=["ones1"])

        def mm(out, pairs, reads, writes):
            def fn(e):
                n = len(pairs)
                ins = None
                for i, (l_, r_) in enumerate(pairs):
                    ins = e.matmul(out, l_, r_, start=(i == 0), stop=(i == n - 1))
                return ins
            return S.op("pe", fn, reads=reads, writes=writes)

        def xk(c, tb):
            return ("xT", c, tb)

        def hk(tb):
            return ("hT", tb)

        def TB(tb):
            return slice(tb * 512, (tb + 1) * 512)

        def dump(name, src_ap, key_reads=None):
            if name in dbg_out:
                S.barrier()
                if src_ap.dtype != F32:
                    with ExitStack() as ph2:
                        tmp = sbt(ph2, "dbgtmp", list(src_ap.shape), F32)
                        S.op("dve", lambda e: e.tensor_copy(out=tmp[:], in_=src_ap), writes=["dbgtmp"])
                        S.op("sp", lambda e: e.dma_start(out=dbg_out[name], in_=tmp[:]), reads=["dbgtmp"], dma="dbg")
                        S.barrier()
                else:
                    S.op("sp", lambda e: e.dma_start(out=dbg_out[name], in_=src_ap), dma="dbg")
                    S.barrier()

        with ExitStack() as ph:
            stage = [sbt(ph, "stg", [128, D], F32) for _ in range(2)]
            for tt in range(NT):
                st = stage[tt % 2]
                S.op("sp", lambda e, st=st, tt=tt: e.dma_start(out=st[:], in_=dr["x"][tt * 128:(tt + 1) * 128, :]),
                     writes=[("stg", tt % 2)], dma=f"xin{tt % 2}")
                for half in range(2):
                    p, pk = psb("all", ALLB)

                    def tr(e, st=st, half=half, p=p):
                        ins = None
                        for j in range(4):
                            c = half * 4 + j
                            ins = e.transpose(p[:, j * 128:(j + 1) * 128], st[:, c * 128:(c + 1) * 128], ident)
                        return ins
                    S.op("pe", tr, reads=[("stg", tt % 2), "cst"], writes=[pk])
                    S.op("dve" if half == 0 else "act",
                         (lambda e, half=half, p=p, tt=tt: e.tensor_copy(out=xT[:, half * 4:(half + 1) * 4, tt * 128:(tt + 1) * 128],
                                                                        in_=p[:].rearrange("p (j t) -> p j t", j=4))) if half == 0 else
                         (lambda e, half=half, p=p, tt=tt: e.copy(out=xT[:, half * 4:(half + 1) * 4, tt * 128:(tt + 1) * 128],
                                                                  in_=p[:].rearrange("p (j t) -> p j t", j=4))),
                         reads=[pk], writes=[xk(c, tt // 4) for c in range(half * 4, half * 4 + 4)])
            S.barrier()

        def rmsnorm_to_h(gcol_ap_fn, gkey, router=None):
            with ExitStack() as ph:
                sq = [sbt(ph, "sq", [128, 512], F32) for _ in range(2)]
                rs = [sbt(ph, "rs", [128, 512], F32) for _ in range(2)]
                if router is not None:
                    h32 = sbt(ph, "h32", [128, 8, 512], F32)
                for tb in range(NB):
                    p, pk = psb("n7", [0, 1, 2, 3, 4, 5, 6])
                    for c in range(8):
                        s_ = sq[c % 2]
                        S.op("act", lambda e, s_=s_, c=c, tb=tb: e.activation(out=s_[:], in_=xT[:, c, TB(tb)], func=AF.Square),
                             reads=[xk(c, tb)], writes=[("sq", c % 2)])
                        S.op("pe", lambda e, s_=s_, c=c, p=p: e.matmul(p[:], onesm[:], s_[:], start=(c == 0), stop=(c == 7)),
                             reads=[("sq", c % 2), "onesm"], writes=[pk])
                    r_ = rs[tb % 2]
                    S.op("act", lambda e, p=p, r_=r_: e.activation(out=r_[:], in_=p[:], func=AF.Ln, bias=epsb[:, 0:1], scale=1.0),
                         reads=[pk, "eps"], writes=[("rs", tb % 2)])
                    S.op("act", lambda e, r_=r_: e.activation(out=r_[:], in_=r_[:], func=AF.Exp, scale=-0.5), reads=[("rs", tb % 2)], writes=[("rs", tb % 2)])
                    for c in range(8):
                        S.op("dve",
                             lambda e, c=c, tb=tb, r_=r_: e.scalar_tensor_tensor(out=hT[:, c, TB(tb)], in0=xT[:, c, TB(tb)], scalar=gcol_ap_fn(c),
                                                                               in1=r_[:], op0=ALU.mult, op1=ALU.mult),
                             reads=[xk(c, tb), ("rs", tb % 2), gkey], writes=[("hT", c, tb)])
                    if router is not None:
                        for c in range(8):
                            S.op("dve", lambda e, c=c, tb=tb, r_=r_: e.scalar_tensor_tensor(out=h32[:, c, :], in0=xT[:, c, TB(tb)], scalar=gcol_ap_fn(c),
                                                                                        in1=r_[:], op0=ALU.mult, op1=ALU.mult),
                                 reads=[xk(c, tb), ("rs", tb % 2), gkey], writes=[("h32", c)])
                        router(tb, h32)
                S.barrier()

        def h_reads(tb):
            return [("hT", c, tb) for c in range(8)]

        def h_reads_all():
            return [("hT", c, tb) for c in range(8) for tb in range(NB)]

        C = Ctx()
        C.__dict__.update(locals())
        for l in range(DEPTH):
            g0 = V_GMIX
            rmsnorm_to_h(lambda c, l=l: vec[l][:, V_GMIX + c:V_GMIX + c + 1], ("vec", l))
            dump(f"h{l}", hT[:], None)
            if stop_after == f"norm{l}":
                break
            stop = False
            with ExitStack() as mixph:
                C.yT = [sbt(mixph, f"y{b}T", [128, 4, T], BF16) for b in range(3)]
                for nm, fn in (("B", mixer_B), ("A", mixer_A), ("C", mixer_C), ("M", merge_out)):
                    if nm in SKIP:
                        continue
                    fn(C, l)
                    if stop_after == f"{nm}{l}":
                        stop = True
                        break
                if stop:
                    S.barrier()
            if stop:
                break
            if l == 0:
                rmsnorm_to_h(lambda c, l=l: vec[l][:, V_GFFN + c:V_GFFN + c + 1], ("vec", l))
                ffn_dense(C)
            else:
                moe(C, l)
            if stop_after == f"F{l}":
                break
        final_out(C)
        waits = [("sp", n) for n in S.dma_sems if n.startswith("yout") or n == "dbg"]
        S.emit(final_waits=waits)
    return nc


def attn_core(C, k_ap, q_ap, v_ap, par, out_ap, scale, kkeys, qkeys, vkeys, okey_fn, PT, rec):
    S, ps, psb = C.S, C.ps, C.psb
    vr = slice(par * 64, par * 64 + 64)
    sr = slice((1 - par) * 64, (1 - par) * 64 + 64)
    SB = [2, 3, 4, 5]
    for qb in range(NB):
        acc, acck = psb("acc", [0, 1])
        pend = []

        def issue_pv(item):
            kt0, p0, p1 = item

            def fn(e, kt0=kt0, p0=p0, p1=p1, acc=acc):
                e.matmul(acc[:], v_ap(kt0), PT[p0][:], start=(kt0 == 0), stop=False)
                return e.matmul(acc[:], v_ap(kt0 + 1), PT[p1][:], start=False, stop=(kt0 + 1 == NT - 1))
            S.op("pe", fn, reads=[("PT", p0), ("PT", p1)] + vkeys, writes=[acck])
        for kt0 in range(0, NT, 2):
            s0, sk0 = psb("s", SB)
            s1, sk1 = psb("s", SB)

            def sfn(e, kt0=kt0, qb=qb, s0=s0, s1=s1):
                e.matmul(s0[:], k_ap(kt0), q_ap(qb), start=True, stop=True)
                return e.matmul(s1[:], k_ap(kt0 + 1), q_ap(qb), start=True, stop=True)
            S.op("pe", sfn, reads=kkeys + qkeys, writes=[sk0, sk1])
            pts = []
            for sp_, spk in ((s0, sk0), (s1, sk1)):
                pt_i = C.pt_rr[0] % len(PT)
                C.pt_rr[0] += 1
                S.op("act", lambda e, sp_=sp_, pt_i=pt_i: e.activation(out=PT[pt_i][:], in_=sp_[:], func=AF.Exp, scale=scale),
                     reads=[spk], writes=[("PT", pt_i)])
                pts.append(pt_i)
            pend.append((kt0, pts[0], pts[1]))
            if len(pend) > 1:
                issue_pv(pend.pop(0))
        while pend:
            issue_pv(pend.pop(0))
        r_i = C.pt_rr[1] % len(rec)
        C.pt_rr[1] += 1
        S.op("dve", lambda e, acc=acc, r_i=r_i: e.reciprocal(out=rec[r_i][vr, :], in_=acc[sr, :]),
             reads=[acck], writes=[("rec", r_i)])
        S.op("dve", lambda e, acc=acc, r_i=r_i, qb=qb: e.tensor_tensor(out=out_ap(qb), in0=acc[vr, :], in1=rec[r_i][vr, :], op=ALU.mult),
             reads=[acck, ("rec", r_i)], writes=[okey_fn(qb)])


def mixer_A(C, l):
    S, nc, dr, hT, vec, cstb, psb, sbt = C.S, C.nc, C.dr, C.hT, C.vec, C.cstb, C.psb, C.sbt
    CI, TB, epsb = C.CI, C.TB, C.epsb
    yaT = C.yT[0]
    PJ = C.PJ
    with ExitStack() as ph:
        wA = sbt(ph, "wA", [128, 8, 576], BF16)
        qT = sbt(ph, "qTa", [128, 2, T], BF16)
        kT = sbt(ph, "kTa", [128, 2, T], BF16)
        Va = [sbt(ph, "Va", [128, NT, 128], BF16) for _ in range(2)]
        PT = [sbt(ph, "PT", [128, 512], BF16) for _ in range(4)]
        rec = [sbt(ph, "rec", [128, 512], F32) for _ in range(1)]
        rope = [sbt(ph, "ropeA", [128, 2, 512], F32) for _ in range(1)]
        sqb = [sbt(ph, "sqb", [128, 512], BF16) for _ in range(1)]
        ub = [sbt(ph, "ub", [128, 512], BF16) for _ in range(1)]
        rsb = [sbt(ph, "rsb", [128, 512], F32) for _ in range(1)]
        t1 = [sbt(ph, "t1", [128, 512], F32) for _ in range(1)]
        t2 = [sbt(ph, "t2", [128, 512], F32) for _ in range(1)]
        onesA = sbt(ph, "onesA", [128, 512], F32)
        S.op("dve", lambda e: e.memset(onesA[:], 1.0), writes=["onesA"])
        C.pt_rr = [0, 0]
        blk = cstb[:, CI["blk64"], :]
        perm = cstb[:, CI["permA"], :]
        for g in range(2):
            S.op("pool", lambda e, g=g: e.dma_start(out=wA[:], in_=dr[f"wA{l}"][g]), writes=["wA"], dma="wA")
            S.op("pool", lambda e: e.memset(Va[0][:], 1.0), writes=["Va"])
            S.op("pool", lambda e: e.memset(Va[1][:], 1.0), writes=["Va1"])
            bi = 0
            for tb in range(NB):
                rp = rope[0]
                S.op("sp", lambda e, rp=rp, tb=tb: e.dma_start(out=rp[:], in_=dr["ropeA"][:, :, TB(tb)]),
                     writes=[("ropeA", 0)], dma="ropeA0")
                for blkid in range(4):
                    col0 = blkid * 128
                    gcol = vec[l][:, V_GAQ:V_GAQ + 1] if blkid < 2 else vec[l][:, V_GAK:V_GAK + 1]
                    i = 0
                    p1, p1k = psb("pj", PJ)
                    C.mm(p1[:], [(wA[:, k, col0:col0 + 128], hT[:, k, TB(tb)]) for k in range(8)],
                         reads=["wA"] + C.h_reads(tb), writes=[p1k])
                    S.op("act", lambda e, p1=p1, i=i: e.activation(out=sqb[i][:], in_=p1[:], func=AF.Square),
                         reads=[p1k], writes=[("sqb", i)])
                    S.op("dve", lambda e, p1=p1, i=i, gcol=gcol: e.scalar_tensor_tensor(out=ub[i][:], in0=p1[:], scalar=gcol, in1=onesA[:], op0=ALU.mult, op1=ALU.mult),
                         reads=[p1k, ("vec", l), "onesA"], writes=[("ub", i)])
                    p2, p2k = psb("pj", PJ)
                    C.mm(p2[:], [(blk, sqb[i][:])], reads=["cstb", ("sqb", i)], writes=[p2k])
                    p3, p3k = psb("pj", PJ)
                    C.mm(p3[:], [(perm, ub[i][:])], reads=["cstb", ("ub", i)], writes=[p3k])
                    S.op("act", lambda e, p2=p2, i=i: e.activation(out=rsb[i][:], in_=p2[:], func=AF.Ln, bias=epsb[:, 0:1], scale=1.0),
                         reads=[p2k, "eps"], writes=[("rsb", i)])
                    S.op("act", lambda e, i=i: e.activation(out=rsb[i][:], in_=rsb[i][:], func=AF.Exp, scale=-0.5), reads=[("rsb", i)], writes=[("rsb", i)])
                    S.op("dve", lambda e, i=i, rp=rp: e.tensor_tensor(out=t1[i][:], in0=ub[i][:], in1=rp[:, 0, :], op=ALU.mult),
                         reads=[("ub", i), ("ropeA", 0)], writes=[("t1", i)])
                    S.op("dve", lambda e, i=i, rp=rp, p3=p3: e.tensor_tensor(out=t2[i][:], in0=p3[:], in1=rp[:, 1, :], op=ALU.mult),
                         reads=[p3k, ("ropeA", 0)], writes=[("t2", i)])
                    S.op("pool", lambda e, i=i: e.tensor_tensor(out=t1[i][:], in0=t1[i][:], in1=t2[i][:], op=ALU.add),
                         reads=[("t1", i), ("t2", i)], writes=[("t1", i)])
                    dst = qT[:, blkid, TB(tb)] if blkid < 2 else kT[:, blkid - 2, TB(tb)]
                    dkey = ("qTa", blkid, tb) if blkid < 2 else ("kTa", blkid - 2, tb)
                    S.op("dve", lambda e, i=i, dst=dst: e.tensor_tensor(out=dst, in0=t1[i][:], in1=rsb[i][:], op=ALU.mult),
                         reads=[("t1", i), ("rsb", i)], writes=[dkey])
            for tg in range(4):
                pv, pvk = psb("pj", PJ)

                def vfn(e, tg=tg, pv=pv):
                    ins = None
                    for j in range(4):
                        tt = tg * 4 + j
                        for k in range(8):
                            ins = e.matmul(pv[:, j * 64:(j + 1) * 64], hT[:, k, tt * 128:(tt + 1) * 128], wA[:, k, 512:576],
                                           start=(k == 0), stop=(k == 7))
                    return ins
                S.op("pe", vfn, reads=["wA"] + C.h_reads(tg), writes=[pvk])
                S.op("dve", lambda e, tg=tg, pv=pv: e.tensor_copy(out=Va[0][:, tg * 4:(tg + 1) * 4, 0:64],
                                                                  in_=pv[:, 0:256].rearrange("p (j d) -> p j d", j=4)),
                     reads=[pvk, "Va"], writes=[("Vav", tg)])
                S.op("act", lambda e, tg=tg, pv=pv: e.copy(out=Va[1][:, tg * 4:(tg + 1) * 4, 64:128],
                                                          in_=pv[:, 0:256].rearrange("p (j d) -> p j d", j=4)),
                     reads=[pvk, "Va1"], writes=[("Vav1", tg)])
            vkeys = [("Vav", tg) for tg in range(4)] + ["Va", "Va1"] + [("Vav1", tg) for tg in range(4)]
            for hl in HLS:
                cl, par = hl // 2, hl % 2
                pr = slice(par * 64, par * 64 + 64)
                chunk = 2 * g + cl
                attn_core(C,
                          k_ap=lambda kt, par=par: kT[:, par, kt * 128:(kt + 1) * 128],
                          q_ap=lambda qb, cl=cl: qT[:, cl, TB(qb)],
                          v_ap=(lambda kt: Va[0][:, kt, :]) if par == 0 else (lambda kt: Va[1][:, kt, :]),
                          par=par,
                          out_ap=lambda qb, pr=pr, chunk=chunk: yaT[pr, chunk, TB(qb)],
                          scale=64.0 ** -0.5,
                          kkeys=[("kTa", par, tb) for tb in range(NB)], qkeys=[("qTa", cl, tb) for tb in range(NB)], vkeys=vkeys,
                          okey_fn=lambda qb, chunk=chunk, par=par: ("y0T", chunk, par, qb), PT=PT, rec=rec)
        S.barrier()
    C.dump(f"ya{l}", yaT[:])


def mixer_B(C, l):
    S, nc, dr, hT, vec, cstb, psb, sbt = C.S, C.nc, C.dr, C.hT, C.vec, C.cstb, C.psb, C.sbt
    CI, TB, epsb, ones256 = C.CI, C.TB, C.epsb, C.ones256
    ybT = C.yT[1]
    PJ = C.PJ
    with ExitStack() as ph:
        cqT = sbt(ph, "cqT", [128, 2, T], BF16)
        ckvT = sbt(ph, "ckvT", [128, 2, T], BF16)
        kh = sbt(ph, "kh", [128, T], BF16)
        rope = [sbt(ph, "ropeB", [128, 2, 512], F32) for _ in range(2)]
        t1 = [sbt(ph, "t1b", [128, 512], F32) for _ in range(1)]
        t2 = [sbt(ph, "t2b", [128, 512], F32) for _ in range(1)]
        with ExitStack() as ph1:
            w1 = sbt(ph1, "wB1", [128, 8, 704], BF16)
            sqb = [sbt(ph1, "sqbB", [128, 512], BF16) for _ in range(4)]
            rsb = [sbt(ph1, "rsbB", [128, 512], F32) for _ in range(2)]
            S.op("pool", lambda e: e.dma_start(out=w1[:], in_=dr[f"wB1{l}"][:, :, :]), writes=["wB1"], dma="wB1")
            ni = 0
            for tb in range(NB):
                rp = rope[0]
                S.op("sp", lambda e, rp=rp, tb=tb: e.dma_start(out=rp[:], in_=dr["ropeB"][:, :, TB(tb)]),
                     writes=[("ropeB", 0)], dma="ropeB0")
                for which in range(2):
                    dstT = cqT if which == 0 else ckvT
                    gbase = V_GBQ if which == 0 else V_GBKV
                    pp = []
                    for c in range(2):
                        p1, p1k = psb("pj", PJ)
                        col0 = which * 256 + c * 128
                        C.mm(p1[:], [(w1[:, k, col0:col0 + 128], hT[:, k, TB(tb)]) for k in range(8)],
                             reads=["wB1"] + C.h_reads(tb), writes=[p1k])
                        si = (ni * 2 + c) % 4
                        S.op("act", lambda e, p1=p1, si=si: e.activation(out=sqb[si][:], in_=p1[:], func=AF.Square),
                             reads=[p1k], writes=[("sqbB", si)])
                        pp.append((p1, p1k, si))
                    p2, p2k = psb("pj", PJ)
                    C.mm(p2[:], [(ones256[:], sqb[pp[0][2]][:]), (ones256[:], sqb[pp[1][2]][:])],
                         reads=["ones256", ("sqbB", pp[0][2]), ("sqbB", pp[1][2])], writes=[p2k])
                    ri = ni % 2
                    ni += 1
                    S.op("act", lambda e, p2=p2, ri=ri: e.activation(out=rsb[ri][:], in_=p2[:], func=AF.Ln, bias=epsb[:, 0:1], scale=1.0),
                         reads=[p2k, "eps"], writes=[("rsbB", ri)])
                    S.op("act", lambda e, ri=ri: e.activation(out=rsb[ri][:], in_=rsb[ri][:], func=AF.Exp, scale=-0.5), reads=[("rsbB", ri)], writes=[("rsbB", ri)])
                    for c in range(2):
                        p1, p1k, si = pp[c]
                        S.op("dve", lambda e, p1=p1, c=c, ri=ri, dstT=dstT, gbase=gbase, tb=tb: e.scalar_tensor_tensor(
                            out=dstT[:, c, TB(tb)], in0=p1[:], scalar=vec[l][:, gbase + c:gbase + c + 1], in1=rsb[ri][:], op0=ALU.mult, op1=ALU.mult),
                            reads=[p1k, ("rsbB", ri), ("vec", l)], writes=[("lat", which, c, tb)])
                pa, pak = psb("pj", PJ)
                C.mm(pa[0:96, :], [(w1[:, k, 512:608], hT[:, k, TB(tb)]) for k in range(8)], reads=["wB1"] + C.h_reads(tb), writes=[pak])
                pb, pbk = psb("pj", PJ)
                C.mm(pb[0:96, :], [(w1[:, k, 608:704], hT[:, k, TB(tb)]) for k in range(8)], reads=["wB1"] + C.h_reads(tb), writes=[pbk])
                i = 0
                rr_ = slice(64, 96)
                S.op("dve", lambda e, pa=pa, rp=rp, i=i: e.tensor_tensor(out=t1[i][rr_, :], in0=pa[rr_, :], in1=rp[rr_, 0, :], op=ALU.mult),
                     reads=[pak, ("ropeB", 0)], writes=[("t1b", i)])
                S.op("dve", lambda e, pb=pb, rp=rp, i=i: e.tensor_tensor(out=t2[i][rr_, :], in0=pb[rr_, :], in1=rp[rr_, 1, :], op=ALU.mult),
                     reads=[pbk, ("ropeB", 0)], writes=[("t2b", i)])
                S.op("pool", lambda e, i=i, tb=tb: e.tensor_tensor(out=kh[rr_, TB(tb)], in0=t1[i][rr_, :], in1=t2[i][rr_, :], op=ALU.add),
                     reads=[("t1b", i), ("t2b", i)], writes=[("krT", tb)])
            S.barrier()
        C.dump(f"cq{l}", cqT[:])
        w2 = sbt(ph, "wB2", [128, 5120], BF16)
        qh = sbt(ph, "qh", [128, T], BF16)
        Vb = sbt(ph, "Vb", [128, NT, 128], BF16)
        PT = [sbt(ph, "PTb", [128, 512], BF16) for _ in range(4)]
        rec = [sbt(ph, "recb", [128, 512], F32) for _ in range(1)]
        C.pt_rr = [0, 0]
        S.op("pool", lambda e: e.dma_start(out=w2[:], in_=dr[f"wB2{l}"][:, :]), writes=["wB2"], dma="wB2")
        wq = w2[:, 0:3072].rearrange("p (k h v m) -> p k h v m", k=2, h=8, v=2)
        wk = w2[:, 3072:4096].rearrange("p (k h m) -> p k h m", k=2, h=8)
        wv = w2[:, 4096:5120].rearrange("p (k h m) -> p k h m", k=2, h=8)
        latq = [("lat", 0, c, tb) for c in range(2) for tb in range(NB)]
        latkv = [("lat", 1, c, tb) for c in range(2) for tb in range(NB)]
        for h in range(8):
            par = h % 2
            for tb in range(NB):
                ri_ = (h * NB + tb) % 2
                rp = rope[ri_]
                S.op("sp", lambda e, rp=rp, tb=tb: e.dma_start(out=rp[:], in_=dr["ropeB"][:, :, TB(tb)]),
                     writes=[("ropeB", ri_)], dma=f"ropeB{ri_}")
                pa, pak = psb("pj", PJ)
                C.mm(pa[0:96, :], [(wq[:, k, h, 0, :], cqT[:, k, TB(tb)]) for k in range(2)], reads=["wB2"] + latq, writes=[pak])
                pb, pbk = psb("pj", PJ)
                C.mm(pb[0:96, :], [(wq[:, k, h, 1, :], cqT[:, k, TB(tb)]) for k in range(2)], reads=["wB2"] + latq, writes=[pbk])
                i = 0
                r96 = slice(0, 96)
                S.op("dve", lambda e, pa=pa, rp=rp, i=i: e.tensor_tensor(out=t1[i][r96, :], in0=pa[r96, :], in1=rp[r96, 0, :], op=ALU.mult),
                     reads=[pak, ("ropeB", ri_)], writes=[("t1b", i)])
                S.op("dve", lambda e, pb=pb, rp=rp, i=i: e.tensor_tensor(out=t2[i][r96, :], in0=pb[r96, :], in1=rp[r96, 1, :], op=ALU.mult),
                     reads=[pbk, ("ropeB", ri_)], writes=[("t2b", i)])
                S.op("pool", lambda e, i=i, tb=tb: e.tensor_tensor(out=qh[r96, TB(tb)], in0=t1[i][r96, :], in1=t2[i][r96, :], op=ALU.add),
                     reads=[("t1b", i), ("t2b", i)], writes=[("qh", tb)])
                pc, pck = psb("pj", PJ)
                C.mm(pc[0:64, :], [(wk[:, k, h, :], ckvT[:, k, TB(tb)]) for k in range(2)], reads=["wB2"] + latkv, writes=[pck])
                S.op("act", lambda e, pc=pc, tb=tb: e.copy(out=kh[0:64, TB(tb)], in_=pc[0:64, :]), reads=[pck], writes=[("kh", tb)])
            voff = 0 if par == 0 else 64
            S.op("pool", lambda e, ooff=64 - voff: e.memset(Vb[:, :, ooff:ooff + 64], 1.0), writes=["Vb1"])
            for tg in range(4):
                pv, pvk = psb("pj", PJ)

                def vfn(e, tg=tg, pv=pv, h=h):
                    ins = None
                    for j in range(4):
                        tt = tg * 4 + j
                        for k in range(2):
                            ins = e.matmul(pv[:, j * 64:(j + 1) * 64], ckvT[:, k, tt * 128:(tt + 1) * 128], wv[:, k, h, :],
                                           start=(k == 0), stop=(k == 1))
                    return ins
                S.op("pe", vfn, reads=["wB2"] + latkv, writes=[pvk])
                S.op("dve", lambda e, tg=tg, pv=pv, voff=voff: e.tensor_copy(out=Vb[:, tg * 4:(tg + 1) * 4, voff:voff + 64],
                                                                            in_=pv[:, 0:256].rearrange("p (j d) -> p j d", j=4)),
                     reads=[pvk], writes=[("Vbv", tg)])
            pr = slice(par * 64, par * 64 + 64)
            chunk = h // 2
            attn_core(C,
                      k_ap=lambda kt: kh[0:96, kt * 128:(kt + 1) * 128],
                      q_ap=lambda qb: qh[0:96, TB(qb)],
                      v_ap=lambda kt: Vb[:, kt, :],
                      par=par,
                      out_ap=lambda qb, pr=pr, chunk=chunk: ybT[pr, chunk, TB(qb)],
                      scale=96.0 ** -0.5,
                      kkeys=[("kh", tb) for tb in range(NB)], qkeys=[("qh", tb) for tb in range(NB)],
                      vkeys=[("Vbv", tg) for tg in range(4)] + ["Vb1"],
                      okey_fn=lambda qb, chunk=chunk, par=par: ("y1T", chunk, par, qb), PT=PT, rec=rec)
        S.barrier()
    C.dump(f"yb{l}", ybT[:])


def mixer_C(C, l):
    S, nc, dr, hT, vec, cst, cstb, sbt, ps = C.S, C.nc, C.dr, C.hT, C.vec, C.cst, C.cstb, C.sbt, C.ps
    CI, TB, epsb, ones128, lnsc = C.CI, C.TB, C.epsb, C.ones128, C.lnsc
    ycT = C.yT[2]
    with ExitStack() as ph:
        cT = sbt(ph, "cT", [128, T], BF16)
        with ExitStack() as ph0:
            wg_ = sbt(ph0, "wCg", [128, 8, 64], BF16)
            S.op("pool", lambda e: e.dma_start(out=wg_[:], in_=dr[f"wCg{l}"][:, :, :]), writes=["wCg"], dma="wCg")
            S.op("dve", lambda e: e.memset(cT[:], 1.0), writes=["cT"])
            for tb in range(NB):
                p, pk = C.psb("all", C.ALLB)
                C.mm(p[0:64, :], [(wg_[:, k, :], hT[:, k, TB(tb)]) for k in range(8)], reads=["wCg"] + C.h_reads(tb), writes=[pk])
                S.op("act", lambda e, p=p, tb=tb: e.copy(out=cT[0:48, TB(tb)], in_=p[0:48, :]), reads=[pk, "cT"], writes=[("cTv", tb)])
            S.barrier()
        wup = sbt(ph, "wup", [128, 2, 128], BF16)
        wh = sbt(ph, "wCh", [128, 5120], BF16)
        qT = sbt(ph, "qTc", [128, T], BF16)
        kT = sbt(ph, "kTc", [128, T], BF16)
        ktok = sbt(ph, "ktok", [128, NT, 128], BF16)
        vtok = sbt(ph, "vtok", [128, NT, 128], BF16)
        oT = sbt(ph, "oT", [128, T], F32)
        sp_ = sbt(ph, "sp", [128, 4, 128], F32)
        e1 = sp_[:].rearrange("p a b -> p (a b)")
        eb = sbt(ph, "eb", [128, 512], F32)
        enb = eb
        eg = sbt(ph, "eg", [128, 4, 128], F32)
        kt_ = sbt(ph, "kt", [128, 512], BF16)
        qt2 = [sbt(ph, "qt", [128, 512], BF16) for _ in range(2)]
        kd2 = [[sbt(ph, "kd", [128, 4, 128], BF16) for _ in range(2)] for _ in range(2)]
        ms2 = [sbt(ph, "ms", [128, 4, 128], BF16) for _ in range(2)]
        dec2 = [sbt(ph, "dec", [128, 8], F32) for _ in range(2)]
        Sst = [sbt(ph, "Sst", [128, 128], F32) for _ in range(2)]
        Sbf = [sbt(ph, "Sbf", [128, 128], BF16) for _ in range(4)]
        sqo = sp_[:].rearrange("p a b -> p (a b)")
        rso = eb
        sgo = eg[:].rearrange("p a b -> p (a b)")
        ckeys = []
        tri = {0: cst[:, CI["trif"], :], 1: cst[:, CI["trib"], :]}
        trib16 = {0: cstb[:, CI["trif"], :], 1: cstb[:, CI["trib"], :]}
        uu = {0: cst[:, CI["uf"], :], 1: cst[:, CI["ub"], :]}
        wl = wh[:, 0:3072].rearrange("p (k v m) -> p k v m", k=8, v=3)
        wr = wh[:, 3072:5120].rearrange("p (k m) -> p k m", k=8)
        si = [0]
        for h in range(4):
            S.op("pool", lambda e, h=h: e.dma_start(out=wh[:], in_=dr[f"wC{l}"][h]), writes=["wCh"], dma="wCh")
            S.op("pool", lambda e, h=h: e.dma_start(out=wup[:].rearrange("p b c -> p (b c)"), in_=dr[f"wCup{l}"][:, h * 256:(h + 1) * 256]),
                 writes=["wup"], dma="wCup")
            for gi in range(NB):
                for which, dst, dk in ((0, qT, "qTc"), (1, kT, "kTc")):
                    p, pk = C.psb("all", C.ALLB)
                    C.mm(p[:], [(wl[:, k, which, :], hT[:, k, TB(gi)]) for k in range(8)], reads=["wCh"] + C.h_reads(gi), writes=[pk])
                    S.op("act", lambda e, p=p, dst=dst, gi=gi: e.copy(out=dst[:, TB(gi)], in_=p[:]), reads=[pk], writes=[(dk, gi)])
                for half in range(2):
                    p, pk = C.psb("all", C.ALLB)

                    def kvfn(e, p=p, gi=gi, half=half):
                        ins = None
                        for j in range(2):
                            tt = gi * 4 + half * 2 + j
                            for k in range(8):
                                ins = e.matmul(p[:, j * 256:(j + 1) * 256], hT[:, k, tt * 128:(tt + 1) * 128], wr[:, k, :], start=(k == 0), stop=(k == 7))
                        return ins
                    S.op("pe", kvfn, reads=["wCh"] + C.h_reads(gi), writes=[pk])
                    t0 = gi * 4 + half * 2
                    pv = p[:].rearrange("p (j w) -> p j w", j=2)
                    S.op("dve", lambda e, pv=pv, t0=t0: e.tensor_copy(out=ktok[:, t0:t0 + 2, :], in_=pv[:, :, 0:128]), reads=[pk], writes=[("ktok", t0 // 2)])
                    S.op("act", lambda e, pv=pv, t0=t0: e.copy(out=vtok[:, t0:t0 + 2, :], in_=pv[:, :, 128:256]), reads=[pk], writes=[("vtok", t0 // 2)])
            def prologue(d, gi, pb_, part):
                qt, kd, ms, dec = qt2[pb_], kd2[pb_], ms2[pb_], dec2[pb_]
                pspk = ("ps", 0)
                if part == 0:
                    def spfn(e, gi=gi, d=d):
                        ins = None
                        for j in range(4):
                            tt = gi * 4 + j
                            ins = e.matmul(ps[0][:, j * 128:(j + 1) * 128], cT[:, tt * 128:(tt + 1) * 128], wup[:, d, :], start=True, stop=True)
                        return ins
                    S.op("pe", spfn, reads=ckeys + ["wup"], writes=[pspk])
                    S.op("act", lambda e: e.activation(out=e1, in_=ps[0][:], func=AF.Exp, scale=-1.0), reads=[pspk], writes=["sp"])
                    S.op("act", lambda e: e.activation(out=sp_[:].rearrange("p a b -> p (a b)"), in_=e1, func=AF.Ln, bias=C.ones1[:, 0:1], scale=1.0),
                         reads=["sp", "ones1"], writes=["sp"])

                if part == 1:
                    def cfn(e, d=d):
                        ins = None
                        for j in range(4):
                            ins = e.matmul(ps[1][:, j * 128:(j + 1) * 128], sp_[:, j, :], tri[d], start=True, stop=True)
                        return ins
                    S.op("pe", cfn, reads=["sp", "cst"], writes=[("ps", 1)])
                    S.op("act", lambda e: e.activation(out=eb[:], in_=ps[1][:], func=AF.Exp, scale=-1.0 / 16, bias=lnsc[:, 0:1]),
                         reads=[("ps", 1), "lnsc"], writes=["eb"])
                    S.op("dve", lambda e, gi=gi: e.tensor_tensor(out=qt[:], in0=qT[:, TB(gi)], in1=eb[:], op=ALU.mult), reads=[("qTc", gi), "eb"], writes=[("qt", pb_)])
                    S.op("act", lambda e: e.activation(out=enb[:], in_=ps[1][:], func=AF.Exp, scale=1.0 / 16), reads=[("ps", 1)], writes=["eb"])
                    S.op("pool", lambda e, gi=gi: e.tensor_tensor(out=kt_[:], in0=kT[:, TB(gi)], in1=enb[:], op=ALU.mult), reads=[("kTc", gi), "eb"], writes=["kt"])
                    pb3 = ps[1][:].rearrange("p (c s) -> p c s", c=8)
                    col = 63 if d == 0 else 0
                    S.op("act", lambda e, pb3=pb3, col=col: e.activation(out=dec[:], in_=pb3[:, :, col], func=AF.Exp, scale=-1.0 / 16),
                         reads=[("ps", 1)], writes=[("dec", pb_)])

                    def gfn(e, d=d):
                        ins = None
                        for j in range(4):
                            ins = e.matmul(ps[2][:, j * 128:(j + 1) * 128], uu[d], sp_[:, j, :], start=True, stop=True)
                        return ins
                    S.op("pe", gfn, reads=["sp", "cst"], writes=[("ps", 2)])
                    S.op("act", lambda e: e.activation(out=eg[:].rearrange("p a b -> p (a b)"), in_=ps[2][:], func=AF.Exp, scale=-1.0 / 16),
                         reads=[("ps", 2)], writes=["eg"])
                if part == 2:
                    for ch_ in range(2):
                        rmask = cst[:, CI["trif"], 63:64] if ch_ == 0 else cst[:, CI["trib"], 64:65]
                        S.op("dve", lambda e, gi=gi, ch_=ch_, rmask=rmask: e.scalar_tensor_tensor(out=kd[ch_][:], in0=eg[:], scalar=rmask, in1=ktok[:, gi * 4:gi * 4 + 4, :],
                                                                                              op0=ALU.mult, op1=ALU.mult),
                             reads=[("ktok", gi * 2), ("ktok", gi * 2 + 1), "eg", "cst"], writes=[("kd", pb_, ch_)])

                    def sfn(e):
                        ins = None
                        for j in range(4):
                            ins = e.matmul(ps[3][:, j * 128:(j + 1) * 128], kt_[:, j * 128:(j + 1) * 128], qt[:, j * 128:(j + 1) * 128], start=True, stop=True)
                        return ins
                    S.op("pe", sfn, reads=["kt", ("qt", pb_)], writes=[("ps", 3)])
                if part == 3:
                    S.op("dve", lambda e, d=d: e.tensor_tensor(out=ms[:], in0=ps[3][:].rearrange("p (j c) -> p j c", j=4),
                                                              in1=trib16[d].unsqueeze(1).to_broadcast([128, 4, 128]), op=ALU.mult),
                         reads=[("ps", 3), "cstb"], writes=[("ms", pb_)])

            def chain(d, gi, pb_, first, after_tile=None):
                qt, kd, ms, dec = qt2[pb_], kd2[pb_], ms2[pb_], dec2[pb_]
                if first:
                    cur = si[0] % 2
                    S.op("dve", lambda e, cur=cur: e.memset(Sst[cur][:], 0.0), writes=[("Sst", cur)])
                    sb_i = si[0] % 4
                    S.op("pool", lambda e, sb_i=sb_i: e.memset(Sbf[sb_i][:], 0.0), writes=[("Sbf", sb_i)])
                tiles = list(range(gi * 4, gi * 4 + 4))
                ob = 4 + (si[0] % 2)
                po, pok = ps[ob], ("ps", ob)
                torder = tiles if d == 0 else tiles[::-1]
                corder = (0, 1) if d == 0 else (1, 0)
                chunks = [(tt, tt - gi * 4, ch, ci) for tt in torder for ci, ch in enumerate(corder)]
                si0 = si[0]

                def emit_kv(n):
                    tt, j, ch, ci = chunks[n]
                    kb = 6 + ((si0 + n) % 2)
                    S.op("pe", lambda e, kb=kb, ch=ch, j=j, tt=tt: e.matmul(ps[kb][:, 0:128], kd[ch][:, j, :], vtok[:, tt, :], start=True, stop=True),
                         reads=[("kd", pb_, ch), ("vtok", tt // 2)], writes=[("ps", kb)])
                emit_kv(0)
                emit_kv(1)
                for n, (tt, j, ch, ci) in enumerate(chunks):
                    if ci == 0:
                        S.op("pe", lambda e, tt=tt, j=j, po=po: e.matmul(po[:, j * 128:(j + 1) * 128], vtok[:, tt, :], ms[:, j, :], start=True, stop=False),
                             reads=[("vtok", tt // 2), ("ms", pb_)], writes=[pok])
                    cur = si[0] % 2
                    sb_i = si[0] % 4
                    csl = slice(j * 128 + ch * 64, j * 128 + ch * 64 + 64)
                    S.op("pe", lambda e, po=po, csl=csl, sb_i=sb_i, last=(ci == 1): e.matmul(po[:, csl], Sbf[sb_i][:], qt[:, csl], start=False, stop=last),
                         reads=[("Sbf", sb_i), ("qt", pb_)], writes=[pok])
                    kb = 6 + (si[0] % 2)
                    nxt = 1 - cur
                    dcol = j * 2 + ch
                    nb_ = (si[0] + 1) % 4
                    S.op("dve", lambda e, kb=kb, cur=cur, nb_=nb_, dcol=dcol: e.scalar_tensor_tensor(
                        out=Sbf[nb_][:], in0=Sst[cur][:], scalar=dec[:, dcol:dcol + 1], in1=ps[kb][:, 0:128], op0=ALU.mult, op1=ALU.add),
                        reads=[("Sst", cur), ("dec", pb_), ("ps", kb)], writes=[("Sbf", nb_)])
                    S.op("dve", lambda e, kb=kb, cur=cur, nxt=nxt, dcol=dcol: e.scalar_tensor_tensor(
                        out=Sst[nxt][:], in0=Sst[cur][:], scalar=dec[:, dcol:dcol + 1], in1=ps[kb][:, 0:128], op0=ALU.mult, op1=ALU.add),
                        reads=[("Sst", cur), ("dec", pb_), ("ps", kb)], writes=[("Sst", nxt)])
                    si[0] += 1
                    if n + 2 < len(chunks):
                        emit_kv(n + 2)
                    if ci == 1 and after_tile is not None:
                        after_tile(torder.index(tt))
                if d == 0:
                    S.op("act", lambda e, po=po, gi=gi: e.copy(out=oT[:, TB(gi)], in_=po[:]), reads=[pok], writes=[("oT", gi)])
                else:
                    S.op("dve", lambda e, po=po, gi=gi: e.tensor_tensor(out=oT[:, TB(gi)], in0=oT[:, TB(gi)], in1=po[:], op=ALU.add),
                         reads=[pok, ("oT", gi)], writes=[("oT", gi)])

            steps = [(0, gi, gi == 0) for gi in range(NB)] + [(1, gi, gi == NB - 1) for gi in range(NB - 1, -1, -1)]
            for part in range(4):
                prologue(steps[0][0], steps[0][1], 0, part)
            for k_, (d, gi, first) in enumerate(steps):
                if k_ + 1 < len(steps):
                    nd, ngi, npb = steps[k_ + 1][0], steps[k_ + 1][1], (k_ + 1) % 2
                    prologue(nd, ngi, npb, 0)
                    chain(d, gi, k_ % 2, first, after_tile=lambda ti, nd=nd, ngi=ngi, npb=npb: prologue(nd, ngi, npb, ti + 1) if ti < 3 else None)
                else:
                    chain(d, gi, k_ % 2, first)
            for gi in range(NB):
                S.op("act", lambda e, gi=gi: e.activation(out=sqo, in_=oT[:, TB(gi)], func=AF.Square), reads=[("oT", gi)], writes=["sp"])
                C.mm(ps[0][:], [(ones128[:], sqo)], reads=["ones128", "sp"], writes=[("ps", 0)])
                S.op("act", lambda e: e.activation(out=rso[:], in_=ps[0][:], func=AF.Ln, bias=epsb[:, 0:1], scale=1.0), reads=[("ps", 0), "eps"], writes=["eb"])
                S.op("act", lambda e: e.activation(out=rso[:], in_=rso[:], func=AF.Exp, scale=-0.5), reads=["eb"], writes=["eb"])
                C.mm(ps[1][:], [(wl[:, k, 2, :], hT[:, k, TB(gi)]) for k in range(8)], reads=["wCh"] + C.h_reads(gi), writes=[("ps", 1)])
                S.op("act", lambda e: e.activation(out=sgo, in_=ps[1][:], func=AF.Silu), reads=[("ps", 1)], writes=["eg"])
                S.op("dve", lambda e, gi=gi: e.scalar_tensor_tensor(out=oT[:, TB(gi)], in0=oT[:, TB(gi)], scalar=vec[l][:, V_GCO:V_GCO + 1], in1=rso[:],
                                                                 op0=ALU.mult, op1=ALU.mult), reads=[("oT", gi), "eb", ("vec", l)], writes=[("oT", gi)])
                S.op("dve", lambda e, gi=gi, h=h: e.tensor_tensor(out=ycT[:, h, TB(gi)], in0=oT[:, TB(gi)], in1=sgo, op=ALU.mult),
                     reads=[("oT", gi), "eg"], writes=[("y2T", h, gi)])
            if h == 0:
                C.dump(f"oc{l}", oT[:])
        S.barrier()
    C.dump(f"yc{l}", ycT[:])


def merge_out(C, l):
    S, dr, hT, vec, sbt, psb, TB, xT = C.S, C.dr, C.hT, C.vec, C.sbt, C.psb, C.TB, C.xT
    yT = C.yT
    with ExitStack() as ph:
        mT = sbt(ph, "mT", [128, 8, T], BF16)
        with ExitStack() as ph1:
            wm = [sbt(ph1, "wm", [128, 1536], BF16) for _ in range(3)]
            sg = [sbt(ph1, "sg", [128, 512], BF16) for _ in range(2)]
            acc = sbt(ph1, "macc", [128, NB, 512], F32)
            tm = [sbt(ph1, "tm", [128, 512], F32) for _ in range(2)]
            ykeys = [[k_ for k_ in S.last_writer if isinstance(k_, tuple) and k_[0] == f"y{br}T"] for br in range(3)]
            pi = 0
            si = 0

            def piece_dma(n):
                S.op("pool", lambda e, n=n: e.dma_start(out=wm[n % 3][:], in_=dr[f"wM{l}"][n]),
                     writes=[("wm", n % 3)], dma=f"wm{n % 3}")
            piece_dma(0)
            piece_dma(1)
            for j in range(8):
                for br in range(3):
                    w_i = pi % 3
                    pi += 1
                    wp = wm[w_i][:, 0:512].rearrange("p (k m) -> p k m", k=4)
                    wgt = wm[w_i][:, 512:1536].rearrange("p (k m) -> p k m", k=8)
                    bcol = V_BG + br * 8 + j
                    for tb in range(NB):
                        pg, pgk = psb("mg", [0, 1, 2, 3])
                        C.mm(pg[:], [(wgt[:, k, :], hT[:, k, TB(tb)]) for k in range(8)], reads=[("wm", w_i)] + C.h_reads(tb), writes=[pgk])
                        pp, ppk = psb("mp", [4, 5, 6, 7])
                        C.mm(pp[:], [(wp[:, k, :], yT[br][:, k, TB(tb)]) for k in range(4)], reads=[("wm", w_i)] + ykeys[br], writes=[ppk])
                        s_i = si % 2
                        si += 1
                        S.op("act", lambda e, pg=pg, s_i=s_i, bcol=bcol: e.activation(out=sg[s_i][:], in_=pg[:], func=AF.Sigmoid,
                                                                                     bias=vec[l][:, bcol:bcol + 1], scale=1.0),
                             reads=[pgk, ("vec", l)], writes=[("sg", s_i)])
                        if br == 0:
                            S.op("dve", lambda e, pp=pp, s_i=s_i, tb=tb: e.tensor_tensor(out=acc[:, tb, :], in0=pp[:], in1=sg[s_i][:], op=ALU.mult),
                                 reads=[ppk, ("sg", s_i)], writes=[("macc", tb)])
                        else:
                            S.op("dve", lambda e, pp=pp, s_i=s_i: e.tensor_tensor(out=tm[s_i][:], in0=pp[:], in1=sg[s_i][:], op=ALU.mult),
                                 reads=[ppk, ("sg", s_i)], writes=[("tm", s_i)])
                            if br == 1:
                                S.op("pool", lambda e, s_i=s_i, tb=tb: e.tensor_tensor(out=acc[:, tb, :], in0=acc[:, tb, :], in1=tm[s_i][:], op=ALU.add),
                                     reads=[("macc", tb), ("tm", s_i)], writes=[("macc", tb)])
                            else:
                                S.op("pool", lambda e, s_i=s_i, tb=tb, j=j: e.tensor_tensor(out=mT[:, j, TB(tb)], in0=acc[:, tb, :], in1=tm[s_i][:], op=ALU.add),
                                     reads=[("macc", tb), ("tm", s_i)], writes=[("mT", j, tb)])
                    if pi + 1 < 24:
                        piece_dma(pi + 1)
            S.barrier()
        wo = [sbt(ph, "wo", [128, 1024], BF16) for _ in range(2)]
        for j in range(8):
            w_i = j % 2
            S.op("pool", lambda e, w_i=w_i, j=j: e.dma_start(out=wo[w_i][:], in_=dr[f"wO{l}"][j]), writes=[("wo", w_i)], dma=f"wo{w_i}")
            wov = wo[w_i][:].rearrange("p (k m) -> p k m", k=8)
            for tb in range(NB):
                po, pok = psb("all", C.ALLB)
                C.mm(po[:], [(wov[:, k, :], mT[:, k, TB(tb)]) for k in range(8)], reads=[("wo", w_i)], writes=[pok])
                S.op("dve", lambda e, po=po, j=j, tb=tb: e.tensor_tensor(out=xT[:, j, TB(tb)], in0=xT[:, j, TB(tb)], in1=po[:], op=ALU.add),
                     reads=[pok, C.xk(j, tb)], writes=[C.xk(j, tb)])
        S.barrier()
    C.dump(f"xmix{l}", xT[:])


def ffn_groups(C, wname, g0, ng, gbc=None, gkey=None):
    S, dr, hT, psb, TB, xT = C.S, C.dr, C.hT, C.psb, C.TB, C.xT
    wf, abuf, sgf = C.wf, C.abuf, C.sgf
    GU = [0, 1, 2, 3]
    OB = [4, 5, 6, 7]
    g = 0
    while g < ng:
        n_in = min(2, ng - g)
        a_i = C.ffi[1] % 2
        C.ffi[1] += 1
        ab = abuf[a_i]
        wviews = []
        w_is = []
        for q in range(n_in):
            w_i = C.ffi[0] % 4
            C.ffi[0] += 1
            w_is.append(w_i)
            S.op("pool", lambda e, w_i=w_i, gg=g0 + g + q: e.dma_start(out=wf[w_i][:], in_=dr[wname][gg]), writes=[("wf", w_i)], dma=f"wf{w_i}")
        for q in range(n_in):
            w_i = w_is[q]
            wgv = wf[w_i][:, 0:2048].rearrange("p (c k m) -> p c k m", c=2, k=8)
            wuv = wf[w_i][:, 2048:4096].rearrange("p (c k m) -> p c k m", c=2, k=8)
            wdv = wf[w_i][:, 4096:6144].rearrange("p (c j m) -> p c j m", c=2, j=8)
            wviews.append((w_i, wdv))
            for c in range(2):
                cc = q * 2 + c
                for tb in range(NB):
                    pg, pgk = psb("gu", GU)
                    C.mm(pg[:], [(wgv[:, c, k, :], hT[:, k, TB(tb)]) for k in range(8)], reads=[("wf", w_i)] + C.h_reads(tb), writes=[pgk])
                    pu, puk = psb("gu", GU)
                    C.mm(pu[:], [(wuv[:, c, k, :], hT[:, k, TB(tb)]) for k in range(8)], reads=[("wf", w_i)] + C.h_reads(tb), writes=[puk])
                    s_i = C.ffi[2] % 2
                    C.ffi[2] += 1
                    S.op("act", lambda e, pg=pg, s_i=s_i: e.activation(out=sgf[s_i][:], in_=pg[:], func=AF.Silu), reads=[pgk], writes=[("sgf", s_i)])
                    if gbc is not None:
                        S.op("pool", lambda e, s_i=s_i, tb=tb: e.tensor_tensor(out=sgf[s_i][:], in0=sgf[s_i][:], in1=gbc[:, TB(tb)], op=ALU.mult),
                             reads=[("sgf", s_i), gkey], writes=[("sgf", s_i)])
                    S.op("dve", lambda e, pu=pu, s_i=s_i, ab=ab, cc=cc, tb=tb: e.tensor_tensor(out=ab[:, cc, TB(tb)], in0=pu[:], in1=sgf[s_i][:], op=ALU.mult),
                         reads=[puk, ("sgf", s_i)], writes=[("ab", a_i, cc, tb)])
        for tb in range(NB):
            for j in range(8):
                po, pok = psb("ob", OB)
                pairs = [(wdv[:, c, j, :], ab[:, q * 2 + c, TB(tb)]) for q, (w_i, wdv) in enumerate(wviews) for c in range(2)]
                C.mm(po[:], pairs,
                     reads=[("wf", w_i) for (w_i, _) in wviews] + [("ab", a_i, cc, tb) for cc in range(2 * n_in)], writes=[pok])
                S.op("dve", lambda e, po=po, j=j, tb=tb: e.tensor_tensor(out=xT[:, j, TB(tb)], in0=xT[:, j, TB(tb)], in1=po[:], op=ALU.add),
                     reads=[pok, C.xk(j, tb)], writes=[C.xk(j, tb)])
        g += n_in


def ffn_alloc(C, ph):
    sbt = C.sbt
    C.wf = [sbt(ph, "wf", [128, 6144], BF16) for _ in range(4)]
    C.abuf = [sbt(ph, "abuf", [128, 4, T], BF16) for _ in range(2)]
    C.sgf = [sbt(ph, "sgf", [128, 512], F32) for _ in range(2)]
    C.ffi = [0, 0, 0]


def ffn_dense(C):
    with ExitStack() as ph:
        ffn_alloc(C, ph)
        ffn_groups(C, "wF0", 0, DFF // 256)
        C.S.barrier()
    C.dump("xffn0", C.xT[:])


def moe(C, l):
    S, dr, sbt, vec, cst, CI, ps, xT = C.S, C.dr, C.sbt, C.vec, C.cst, C.CI, C.ps, C.xT
    with ExitStack() as ph0:
        gate = sbt(ph0, "gate", [128, NT, NEXP], F32)
        with ExitStack() as ph:
            wr = sbt(ph, "wR", [128, 8, 8], F32)
            lg = sbt(ph, "lg", [128, NT, NEXP], F32)
            lg2 = sbt(ph, "lg2", [128, NT, NEXP], F32)
            mk1 = sbt(ph, "mk1", [128, NT, NEXP], F32)
            mk2 = sbt(ph, "mk2", [128, NT, NEXP], F32)
            m1 = sbt(ph, "m1", [128, NT], F32)
            m2 = sbt(ph, "m2", [128, NT], F32)
            w1 = sbt(ph, "w1", [128, NT], F32)
            w2 = sbt(ph, "w2", [128, NT], F32)
            S.op("sp", lambda e: e.dma_start(out=wr[:], in_=dr["wR"][:, :, :]), writes=["wR"], dma="wR")
            plg = ps[7]

            def router(tb, h32):
                def rfn(e):
                    ins = None
                    for jt in range(4):
                        tt = tb * 4 + jt
                        for c in range(8):
                            ins = e.matmul(plg[:, tt * 8:(tt + 1) * 8], h32[:, c, jt * 128:(jt + 1) * 128], wr[:, c, :], start=(c == 0), stop=(c == 7))
                    return ins
                S.op("pe", rfn, reads=[("h32", c) for c in range(8)] + ["wR"], writes=[("plg", tb)])
            C.rmsnorm_to_h(lambda c: vec[l][:, V_GFFN + c:V_GFFN + c + 1], ("vec", l), router=router)
            bc = lambda a: a[:].unsqueeze(2).to_broadcast([128, NT, NEXP])
            S.op("dve", lambda e: e.tensor_copy(out=lg[:].rearrange("p a b -> p (a b)"), in_=plg[:, 0:NT * NEXP]), writes=["lg"])
            S.op("dve", lambda e: e.tensor_reduce(out=m1[:], in_=lg[:], axis=AX.X, op=ALU.max), reads=["lg"], writes=["m1"])
            S.op("dve", lambda e: e.tensor_tensor(out=mk1[:], in0=lg[:], in1=bc(m1), op=ALU.is_equal), reads=["lg", "m1"], writes=["mk1"])
            S.op("dve", lambda e: e.scalar_tensor_tensor(out=lg2[:], in0=mk1[:], scalar=-1e30, in1=lg[:], op0=ALU.mult, op1=ALU.add),
                 reads=["mk1", "lg"], writes=["lg2"])
            S.op("dve", lambda e: e.tensor_reduce(out=m2[:], in_=lg2[:], axis=AX.X, op=ALU.max), reads=["lg2"], writes=["m2"])
            S.op("dve", lambda e: e.tensor_tensor(out=mk2[:], in0=lg2[:], in1=bc(m2), op=ALU.is_equal), reads=["lg2", "m2"], writes=["mk2"])
            S.op("dve", lambda e: e.tensor_tensor(out=w2[:], in0=m2[:], in1=m1[:], op=ALU.subtract), reads=["m1", "m2"], writes=["w2"])
            S.op("act", lambda e: e.activation(out=w2[:], in_=w2[:], func=AF.Exp), reads=["w2"], writes=["w2"])
            S.op("dve", lambda e: e.tensor_tensor(out=w1[:], in0=w2[:], in1=C.ones1[:, 0:NT], op=ALU.add), reads=["w2", "ones1"], writes=["w1"])
            S.op("dve", lambda e: e.reciprocal(out=w1[:], in_=w1[:]), reads=["w1"], writes=["w1"])
            S.op("dve", lambda e: e.tensor_tensor(out=w2[:], in0=w2[:], in1=w1[:], op=ALU.mult), reads=["w1", "w2"], writes=["w2"])
            S.op("dve", lambda e: e.tensor_tensor(out=mk1[:], in0=mk1[:], in1=bc(w1), op=ALU.mult), reads=["mk1", "w1"], writes=["mk1"])
            S.op("dve", lambda e: e.tensor_tensor(out=mk2[:], in0=mk2[:], in1=bc(w2), op=ALU.mult), reads=["mk2", "w2"], writes=["mk2"])
            S.op("dve", lambda e: e.tensor_tensor(out=gate[:], in0=mk1[:], in1=mk2[:], op=ALU.add), reads=["mk1", "mk2"], writes=["gate"])
            S.barrier()
        C.dump("gate", gate[:].rearrange("p a b -> p (a b)"))
        with ExitStack() as ph:
            ffn_alloc(C, ph)
            gbc = [sbt(ph, "gbc", [128, T], F32) for _ in range(2)]
            dg = [sbt(ph, "dg", [128, 128], F32) for _ in range(2)]
            ident = cst[:, CI["ident"], :]
            di = 0
            for ex in range(NEXP):
                gb = gbc[ex % 2]
                for tg in range(4):
                    pb_, pbk = C.psb("ob", [4, 5, 6, 7])
                    for jt in range(4):
                        tt = tg * 4 + jt
                        d_i = di % 2
                        di += 1
                        S.op("dve", lambda e, d_i=d_i, tt=tt, ex=ex: e.scalar_tensor_tensor(out=dg[d_i][:], in0=ident, scalar=gate[:, tt, ex:ex + 1], in1=C.ones1[:], op0=ALU.mult, op1=ALU.mult),
                             reads=["gate", "cst", "ones1"], writes=[("dg", d_i)])
                        S.op("pe", lambda e, d_i=d_i, jt=jt, pb_=pb_: e.matmul(pb_[:, jt * 128:(jt + 1) * 128], C.ones1[:], dg[d_i][:], start=True, stop=True),
                             reads=[("dg", d_i), "ones1"], writes=[pbk])
                    S.op("act", lambda e, pb_=pb_, gb=gb, tg=tg: e.copy(out=gb[:, tg * 512:(tg + 1) * 512], in_=pb_[:]), reads=[pbk], writes=[("gbc", ex % 2)])
                ffn_groups(C, "wE", ex * (DFFE // 256), DFFE // 256, gbc=gb, gkey=("gbc", ex % 2))
            S.barrier()
    C.dump("xffn1", xT[:])


def final_out(C):
    S, sbt, xT, TB, gfin, epsb, onesm, psb, ident = C.S, C.sbt, C.xT, C.TB, C.gfin, C.epsb, C.onesm, C.psb, C.ident
    with ExitStack() as ph:
        sq = [sbt(ph, "sqf", [128, 512], F32) for _ in range(2)]
        rs = sbt(ph, "rsf", [128, T], F32)
        yt = [sbt(ph, "ytf", [128, 128], F32) for _ in range(4)]
        stage = [sbt(ph, "stgo", [128, D], F32) for _ in range(2)]
        for tb in range(NB):
            p, pk = psb("all", C.ALLB)
            for c in range(8):
                s_ = sq[c % 2]
                S.op("act", lambda e, s_=s_, c=c, tb=tb: e.activation(out=s_[:], in_=xT[:, c, TB(tb)], func=AF.Square),
                     reads=[C.xk(c, tb)], writes=[("sqf", c % 2)])
                S.op("pe", lambda e, s_=s_, c=c, p=p: e.matmul(p[:], onesm[:], s_[:], start=(c == 0), stop=(c == 7)),
                     reads=[("sqf", c % 2), "onesm"], writes=[pk])
            S.op("act", lambda e, p=p, tb=tb: e.activation(out=rs[:, TB(tb)], in_=p[:], func=AF.Ln, bias=epsb[:, 0:1], scale=1.0),
                 reads=[pk, "eps"], writes=[("rsf", tb)])
            S.op("act", lambda e, tb=tb: e.activation(out=rs[:, TB(tb)], in_=rs[:, TB(tb)], func=AF.Exp, scale=-0.5), reads=[("rsf", tb)], writes=[("rsf", tb)])
        yi = 0
        for tt in range(NT):
            tb = tt // 4
            st = stage[tt % 2]
            for half in range(2):
                p, pk = psb("all", C.ALLB)
                for j in range(4):
                    c = half * 4 + j
                    y_i = yi % 4
                    yi += 1
                    S.op("dve", lambda e, y_i=y_i, c=c, tt=tt: e.scalar_tensor_tensor(out=yt[y_i][:], in0=xT[:, c, tt * 128:(tt + 1) * 128], scalar=gfin[:, c:c + 1],
                                                                                 in1=rs[:, tt * 128:(tt + 1) * 128], op0=ALU.mult, op1=ALU.mult),
                         reads=[C.xk(c, tb), ("rsf", tb), "gfin"], writes=[("ytf", y_i)])
                    S.op("pe", lambda e, y_i=y_i, j=j, p=p: e.transpose(p[:, j * 128:(j + 1) * 128], yt[y_i][:], ident),
                         reads=[("ytf", y_i), "cst"], writes=[pk])
                S.op("act", lambda e, p=p, st=st, half=half: e.copy(out=st[:, half * 512:(half + 1) * 512], in_=p[:]),
                     reads=[pk], writes=[("stgo", tt % 2, half)])
            S.op("sp", lambda e, st=st, tt=tt: e.dma_start(out=C.y_out[tt * 128:(tt + 1) * 128, :], in_=st[:]),
                 reads=[("stgo", tt % 2, 0), ("stgo", tt % 2, 1)], dma=f"yout{tt % 2}")


_CACHE = {}


def kernel(**inputs):
    x = np.asarray(inputs["x"], dtype=np.float32)
    packed = pack_inputs(inputs)
    shapes = {k: v.shape for k, v in packed.items()}
    shapes["x"] = (T, D)
    nc = build_program(shapes)
    in_maps = []
    for b in range(8):
        m = dict(packed)
        m["x"] = np.ascontiguousarray(x[b])
        in_maps.append(m)
    res = run_bass_kernel_spmd(nc, in_maps, core_ids=list(range(8)))
    return np.stack([np.asarray(res.results[b]["y"], dtype=np.float32) for b in range(8)], 0)
```
